# Optimizing a Trainium2 kernel written in Bass

```python
import math
import jax, jax.numpy as jnp
from jax import lax
import numpy as np

D_MODEL = 1024
BATCH = 1
SEQ = 16384
DEPTH = 2
DEC_BATCH = 4
DEC_SEQ = 8192
PAST_LEN = 128

HEAD_DIM = 64
GRID_W = 64
A_Q_HEADS = 8
A_KV_HEADS = 2
A_GROUP = A_Q_HEADS // A_KV_HEADS
A_HALF = 128
A_BLOCK = 128
B_PAIRS = ((128, 1), (512, 4), (2048, 16))
B_GROUPS = 3
B_HEADS = 4
B_BLOCK = 64
A_QW = A_Q_HEADS * HEAD_DIM
A_KVW = A_KV_HEADS * HEAD_DIM
A_IN = A_QW + 2 * A_KVW
B_IN = 3 * B_GROUPS * B_HEADS * HEAD_DIM
AB_IN = A_IN + B_IN
B_OUT = B_HEADS * HEAD_DIM
AB_OUT = A_QW + B_OUT
C_HEADS = 16
C_W = C_HEADS * HEAD_DIM
NA_KH = 8
NA_KW = 16
NA_CB = 16
N_EXPERTS = 32
TOP_K = 4
D_FF = 1024
SWIGLU_LIMIT = 7.0
SWIGLU_ALPHA = 1.702
MOE_BLOCK = 128
RMS_EPS = 1e-6
NEG_INF = -1e30

kernel_name = 'hybrid_bidir_alibi_natten_moe_encoder'


def rms_norm(x, g):
    x32 = x.astype(jnp.float32)
    y = x32 * lax.rsqrt(jnp.mean(x32 * x32, axis=-1, keepdims=True) + RMS_EPS)
    return (y * g.astype(jnp.float32)).astype(x.dtype)


def alibi_slopes(n):
    return jnp.asarray(2.0 ** (-8.0 * np.arange(1, n + 1) / n), dtype=jnp.float32)


def banded_attn(q, k, v, half, blk, slopes, step, sink):
    N, L, Hk, G, hd = q.shape
    nb = L // blk
    W = blk + 2 * half
    kp = jnp.pad(k, ((0, 0), (half, half), (0, 0), (0, 0)))
    vp = jnp.pad(v, ((0, 0), (half, half), (0, 0), (0, 0)))
    starts = np.arange(nb) * blk
    kidx = starts[:, None] + np.arange(W)[None, :]
    kb = kp[:, kidx]
    vb = vp[:, kidx]
    qb = q.reshape(N, nb, blk, Hk, G, hd)
    s = jnp.einsum('nbqhgd,nbkhd->nbhgqk', qb.astype(jnp.float32), kb.astype(jnp.float32)) * (hd ** -0.5)
    kpos = kidx - half
    qpos = starts[:, None] + np.arange(blk)[None, :]
    rel = kpos[:, None, :] - qpos[:, :, None]
    valid = (np.abs(rel) <= half) & (kpos >= 0)[:, None, :] & (kpos < L)[:, None, :]
    dist = jnp.asarray(step * np.abs(rel), dtype=jnp.float32)
    s = s - slopes.astype(jnp.float32)[None, None, :, :, None, None] * dist[None, :, None, None, :, :]
    s = jnp.where(jnp.asarray(valid)[None, :, None, None], s, NEG_INF)
    m = jnp.max(s, axis=-1, keepdims=True)
    if sink is not None:
        sink_b = sink.astype(jnp.float32)[None, None, :, :, None, None]
        m = jnp.maximum(m, sink_b)
    e = jnp.exp(s - m)
    den = jnp.sum(e, axis=-1, keepdims=True)
    if sink is not None:
        den = den + jnp.exp(sink_b - m)
    out = jnp.einsum('nbhgqk,nbkhd->nbqhgd', e / den, vb.astype(jnp.float32))
    out = out.reshape(N, L, Hk, G, hd).astype(q.dtype)
    lse = (m + jnp.log(den))[..., 0]
    lse = lse.transpose(0, 1, 4, 2, 3).reshape(N, L, Hk, G)
    return out, lse


def dilated_attn(q, k, v, dil, half, slopes):
    N, L, H, hd = q.shape
    Ld = L // dil

    def to_strided(t):
        return t.reshape(N, Ld, dil, H, hd).transpose(0, 2, 1, 3, 4).reshape(N * dil, Ld, H, hd)

    blk = math.gcd(Ld, B_BLOCK)
    o, lse = banded_attn(to_strided(q)[:, :, :, None], to_strided(k), to_strided(v),
                         half, blk, slopes[:, None], dil, None)
    o = o.reshape(N, dil, Ld, H, hd).transpose(0, 2, 1, 3, 4).reshape(N, L, H, hd)
    lse = lse.reshape(N, dil, Ld, H).transpose(0, 2, 1, 3).reshape(N, L, H)
    return o, lse


def mixer_ab(h, w_in, q_norm_a, k_norm_a, sink_a, q_norm_b, k_norm_b, w_out):
    N, L, _ = h.shape
    proj = h @ w_in
    qa = rms_norm(proj[..., :A_QW].reshape(N, L, A_KV_HEADS, A_GROUP, HEAD_DIM), q_norm_a)
    ka = rms_norm(proj[..., A_QW:A_QW + A_KVW].reshape(N, L, A_KV_HEADS, HEAD_DIM), k_norm_a)
    va = proj[..., A_QW + A_KVW:A_IN].reshape(N, L, A_KV_HEADS, HEAD_DIM)
    oa, _ = banded_attn(qa, ka, va, A_HALF, A_BLOCK,
                        alibi_slopes(A_Q_HEADS).reshape(A_KV_HEADS, A_GROUP), 1,
                        sink_a.reshape(A_KV_HEADS, A_GROUP))
    qkv_b = proj[..., A_IN:].reshape(N, L, 3, B_GROUPS, B_HEADS, HEAD_DIM)
    slopes_b = alibi_slopes(B_GROUPS * B_HEADS).reshape(B_GROUPS, B_HEADS)
    outs, lses = [], []
    for g, (window, dil) in enumerate(B_PAIRS):
        qg = rms_norm(qkv_b[:, :, 0, g], q_norm_b)
        kg = rms_norm(qkv_b[:, :, 1, g], k_norm_b)
        vg = qkv_b[:, :, 2, g]
        og, lg = dilated_attn(qg, kg, vg, dil, window // (2 * dil), slopes_b[g])
        outs.append(og.astype(jnp.float32))
        lses.append(lg)
    wts = jax.nn.softmax(jnp.stack(lses, axis=0), axis=0)
    ob = jnp.sum(wts[..., None] * jnp.stack(outs, axis=0), axis=0).astype(h.dtype)
    o = jnp.concatenate([oa.reshape(N, L, A_QW), ob.reshape(N, L, B_OUT)], axis=-1)
    return o @ w_out


def neighborhood_attn(q, k, v, rpb):
    N, L, H, hd = q.shape
    rows = L // GRID_W
    kh = min(NA_KH, rows)
    ncb = GRID_W // NA_CB
    kcw = NA_CB + NA_KW
    qc = np.arange(GRID_W).reshape(ncb, NA_CB)
    cs_q = np.clip(qc - NA_KW // 2, 0, GRID_W - NA_KW)
    cs_b = np.clip(np.arange(ncb) * NA_CB - NA_KW // 2, 0, GRID_W - kcw)
    kc = cs_b[:, None] + np.arange(kcw)[None, :]
    col_valid = jnp.asarray((kc[:, None, :] >= cs_q[:, :, None]) & (kc[:, None, :] < cs_q[:, :, None] + NA_KW))
    col_idx = np.clip(kc[:, None, :] - qc[:, :, None] + NA_KW - 1, 0, 2 * NA_KW - 2)
    rpb32 = rpb.astype(jnp.float32)
    q_rows = q.reshape(N, rows, ncb, NA_CB, H, hd).transpose(1, 0, 2, 3, 4, 5)

    def row_step(args):
        r, q_row = args
        rs = jnp.clip(r - kh // 2, 0, rows - kh)
        k_win = lax.dynamic_slice_in_dim(k, rs * GRID_W, kh * GRID_W, axis=1).reshape(N, kh, GRID_W, H, hd)[:, :, kc]
        v_win = lax.dynamic_slice_in_dim(v, rs * GRID_W, kh * GRID_W, axis=1).reshape(N, kh, GRID_W, H, hd)[:, :, kc]
        s = jnp.einsum('njqhd,nrjkhd->njhqrk', q_row.astype(jnp.float32), k_win.astype(jnp.float32)) * (hd ** -0.5)
        row_idx = rs + jnp.arange(kh) - r + NA_KH - 1
        bias = rpb32[:, row_idx][:, :, col_idx]
        s = s + bias.transpose(2, 0, 3, 1, 4)[None]
        s = jnp.where(col_valid[None, :, None, :, None, :], s, NEG_INF)
        p = jax.nn.softmax(s.reshape(N, ncb, H, NA_CB, kh * kcw), axis=-1).reshape(s.shape)
        o = jnp.einsum('njhqrk,nrjkhd->njqhd', p, v_win.astype(jnp.float32))
        return o.astype(q.dtype)

    o = lax.map(row_step, (jnp.arange(rows), q_rows))
    return o.transpose(1, 0, 2, 3, 4, 5).reshape(N, L, H, hd)


def mixer_c(h, w_in, q_norm_c, k_norm_c, rpb_c, w_out):
    N, L, _ = h.shape
    qkv = (h @ w_in).reshape(N, L, 3, C_HEADS, HEAD_DIM)
    q = rms_norm(qkv[:, :, 0], q_norm_c)
    k = rms_norm(qkv[:, :, 1], k_norm_c)
    o = neighborhood_attn(q, k, qkv[:, :, 2], rpb_c)
    return o.reshape(N, L, C_W) @ w_out


def moe_ffn(h, w_router, b_router, w_gate_up, b_gate_up, w_down, b_down):
    N, L, D = h.shape
    T = N * L
    xt = h.reshape(T, D)
    logits = (xt @ w_router + b_router).astype(jnp.float32)
    top_val, top_idx = lax.top_k(logits, TOP_K)
    gates = jax.nn.softmax(top_val, axis=-1)
    n_assign = T * TOP_K
    expert = top_idx.reshape(n_assign).astype(jnp.int32)
    token = jnp.arange(n_assign, dtype=jnp.int32) // TOP_K
    weight = gates.reshape(n_assign)
    order = jnp.argsort(expert)
    e_s, tok_s, w_s = expert[order], token[order], weight[order]
    counts = jnp.bincount(expert, length=N_EXPERTS)
    padded = (counts + MOE_BLOCK - 1) // MOE_BLOCK * MOE_BLOCK
    pad_end = jnp.cumsum(padded)
    pad_start = pad_end - padded
    srt_start = jnp.cumsum(counts) - counts
    slot = pad_start[e_s] + jnp.arange(n_assign, dtype=jnp.int32) - srt_start[e_s]
    n_blocks = -(-n_assign // MOE_BLOCK) + N_EXPERTS
    n_slots = n_blocks * MOE_BLOCK
    slot_tok = jnp.full((n_slots,), T, jnp.int32).at[slot].set(tok_s)
    slot_w = jnp.zeros((n_slots,), jnp.float32).at[slot].set(w_s)
    blk_expert = jnp.minimum(jnp.searchsorted(pad_end, jnp.arange(n_blocks, dtype=jnp.int32) * MOE_BLOCK, side='right'), N_EXPERTS - 1)
    x_pad = jnp.concatenate([xt, jnp.zeros((1, D), xt.dtype)], axis=0)

    def expert_block(args):
        tok_b, e_b, w_b = args
        xb = x_pad[tok_b]
        gu = xb @ w_gate_up[e_b] + b_gate_up[e_b]
        gate = jnp.minimum(gu[:, :D_FF], SWIGLU_LIMIT)
        up = jnp.clip(gu[:, D_FF:], -SWIGLU_LIMIT, SWIGLU_LIMIT)
        act = (up + 1) * gate * jax.nn.sigmoid(SWIGLU_ALPHA * gate)
        y = act @ w_down[e_b] + b_down[e_b]
        return y * w_b[:, None].astype(y.dtype)

    ys = lax.map(expert_block, (slot_tok.reshape(n_blocks, MOE_BLOCK), blk_expert,
                                slot_w.reshape(n_blocks, MOE_BLOCK)))
    out = jnp.zeros((T + 1, D), h.dtype).at[slot_tok].add(ys.reshape(n_slots, D).astype(h.dtype))
    return out[:T].reshape(N, L, D)


def trunk(x, c, layers):
    for i in range(DEPTH):
        ada_w, ada_b, norm1, mixer_fn, mixer_params, norm2, moe_params = layers[i]
        mod = jax.nn.silu(c) @ ada_w + ada_b
        shift1, scale1, gate1, shift2, scale2, gate2 = jnp.split(mod[:, None, :], 6, axis=-1)
        h = rms_norm(x, norm1) * (1 + scale1) + shift1
        x = x + gate1 * mixer_fn(h, *mixer_params)
        h = rms_norm(x, norm2) * (1 + scale2) + shift2
        x = x + gate2 * moe_ffn(h, *moe_params)
    return x


def setup_inputs(seed: int = 0) -> dict:
    key = jax.random.key(seed)
    ks = iter(jax.random.split(key, 64))

    def nrm(shape, scale):
        return scale * jax.random.normal(next(ks), shape, jnp.float32)

    def gain(n):
        return 1.0 + 0.02 * jax.random.normal(next(ks), (n,), jnp.float32)

    D = D_MODEL
    inp = {}
    inp['x_prompt'] = nrm((BATCH, SEQ, D), 1.0)
    inp['x_sample'] = nrm((DEC_BATCH, DEC_SEQ, D), 1.0)
    inp['c_prompt'] = nrm((BATCH, D), 1.0)
    inp['c_sample'] = nrm((DEC_BATCH, D), 1.0)

    def add_moe(p):
        inp[p + 'norm2'] = gain(D)
        inp[p + 'w_router'] = nrm((D, N_EXPERTS), D ** -0.5)
        inp[p + 'b_router'] = nrm((N_EXPERTS,), 0.01)
        inp[p + 'w_gate_up'] = nrm((N_EXPERTS, D, 2 * D_FF), D ** -0.5)
        inp[p + 'b_gate_up'] = nrm((N_EXPERTS, 2 * D_FF), 0.01)
        inp[p + 'w_down'] = nrm((N_EXPERTS, D_FF, D), D_FF ** -0.5)
        inp[p + 'b_down'] = nrm((N_EXPERTS, D), 0.01)

    inp['l0_ada_w'] = nrm((D, 6 * D), 0.5 * D ** -0.5)
    inp['l0_ada_b'] = nrm((6 * D,), 0.01)
    inp['l0_norm1'] = gain(D)
    inp['l0_w_in'] = nrm((D, AB_IN), D ** -0.5)
    inp['l0_q_norm_a'] = gain(HEAD_DIM)
    inp['l0_k_norm_a'] = gain(HEAD_DIM)
    inp['l0_sink_a'] = nrm((A_Q_HEADS,), 0.5)
    inp['l0_q_norm_b'] = gain(HEAD_DIM)
    inp['l0_k_norm_b'] = gain(HEAD_DIM)
    inp['l0_w_out'] = nrm((AB_OUT, D), AB_OUT ** -0.5)
    add_moe('l0_')
    inp['l1_ada_w'] = nrm((D, 6 * D), 0.5 * D ** -0.5)
    inp['l1_ada_b'] = nrm((6 * D,), 0.01)
    inp['l1_norm1'] = gain(D)
    inp['l1_w_in'] = nrm((D, 3 * C_W), D ** -0.5)
    inp['l1_q_norm_c'] = gain(HEAD_DIM)
    inp['l1_k_norm_c'] = gain(HEAD_DIM)
    inp['l1_rpb_c'] = nrm((C_HEADS, 2 * NA_KH - 1, 2 * NA_KW - 1), 0.1)
    inp['l1_w_out'] = nrm((C_W, D), C_W ** -0.5)
    add_moe('l1_')
    return inp


def reference(x_prompt, x_sample, c_prompt, c_sample,
              l0_ada_w, l0_ada_b, l0_norm1, l0_w_in, l0_q_norm_a, l0_k_norm_a, l0_sink_a,
              l0_q_norm_b, l0_k_norm_b, l0_w_out, l0_norm2, l0_w_router, l0_b_router,
              l0_w_gate_up, l0_b_gate_up, l0_w_down, l0_b_down,
              l1_ada_w, l1_ada_b, l1_norm1, l1_w_in, l1_q_norm_c, l1_k_norm_c, l1_rpb_c,
              l1_w_out, l1_norm2, l1_w_router, l1_b_router,
              l1_w_gate_up, l1_b_gate_up, l1_w_down, l1_b_down):
    layers = (
        (l0_ada_w, l0_ada_b, l0_norm1, mixer_ab,
         (l0_w_in, l0_q_norm_a, l0_k_norm_a, l0_sink_a, l0_q_norm_b, l0_k_norm_b, l0_w_out),
         l0_norm2,
         (l0_w_router, l0_b_router, l0_w_gate_up, l0_b_gate_up, l0_w_down, l0_b_down)),
        (l1_ada_w, l1_ada_b, l1_norm1, mixer_c,
         (l1_w_in, l1_q_norm_c, l1_k_norm_c, l1_rpb_c, l1_w_out),
         l1_norm2,
         (l1_w_router, l1_b_router, l1_w_gate_up, l1_b_gate_up, l1_w_down, l1_b_down)),
    )
    y_prompt = trunk(x_prompt, c_prompt, layers)
    y_sample = trunk(x_sample, c_sample, layers)
    return (y_prompt, y_sample)
```

```python
import contextlib
import numpy as np
import concourse.bass as bass
import concourse.mybir as mybir
from concourse.bass_utils import run_bass_kernel_spmd

F32 = mybir.dt.float32; BF16 = mybir.dt.bfloat16; I32 = mybir.dt.int32
AF = mybir.ActivationFunctionType; ALU = mybir.AluOpType
D = 1024; NEG = -30000.0; H0C = 10; HQC = 2; TOPK = 4


class _Stop(Exception):
    pass


def _stoprange(cfg):
    return range(2)


class Cfg:
    def __init__(s, ncores, own, E, C):
        s.ncores = ncores; s.own = own; s.E = E; s.C = C
        s.R0 = [o + 2 * H0C for o in own]; s.Q0 = [o + 2 * HQC for o in own]
        s.R0off = [sum(s.R0[:i]) for i in range(len(own))]; s.Q0off = [sum(s.Q0[:i]) for i in range(len(own))]
        s.Ooff = [sum(own[:i]) for i in range(len(own))]
        s.NR0 = sum(s.R0); s.NQ0 = sum(s.Q0); s.NO = sum(own)


def full_cfg():
    return Cfg(8, [16, 32], 32, [3072, 3072])


def layer_specs():
    L0 = dict(nqb=10, nkb=7, nvh=14, nko=6)
    L0["qcols"] = [b * 128 for b in range(4)] + [768 + g * 256 + j * 128 for g in range(3) for j in range(2)]
    L0["kcols"] = [512] + [1536 + g * 256 + j * 128 for g in range(3) for j in range(2)]
    L0["vsets"] = [(640, 128, 0), (2304, 512, 2), (2816, 256, 10)]
    L0["qgain"] = [0] * 4 + [2] * 6; L0["kgain"] = [1] + [3] * 6
    L0["wgroups"] = [(0, 6, 1), (6, 10, 2), (10, 14, 8)]
    quads = []
    for j in range(2):
        heads = [(4 * j + i, j) for i in range(4)]
        quads.append(dict(units=[(heads, d, ("A", j, d)) for d in (-1, 0, 1)], sink=True))
    bun = []
    for g, rng in enumerate([range(-1, 2), range(-2, 3), range(-8, 9)]):
        heads = [(8 + 4 * g + h, 2 + 4 * g + h) for h in range(4)]
        bun += [(heads, d, ("B", g, d)) for d in rng]
    quads.append(dict(units=bun, sink=False))
    L0["quads"] = quads
    L1 = dict(nqb=8, nkb=8, nvh=16, nko=8)
    L1["qcols"] = [b * 128 for b in range(8)]; L1["kcols"] = [1024 + b * 128 for b in range(8)]
    L1["vsets"] = [(2048, 512, 0), (2560, 512, 8)]
    L1["qgain"] = [0] * 8; L1["kgain"] = [1] * 8
    L1["wgroups"] = [(0, 16, 3)]
    quads = []
    for qd in range(4):
        heads = [(4 * qd + h, 4 * qd + h) for h in range(4)]
        quads.append(dict(units=[(heads, d, ("C", qd, d)) for d in range(-3, 4)], sink=False))
    L1["quads"] = quads
    return L0, L1


def l0_bias_list():
    L0, _ = layer_specs()
    return [u[2] for q in L0["quads"] for u in q["units"]]


def host_bias_l0():
    sl_a = 2.0 ** (-8.0 * np.arange(1, 9) / 8); sl_b = (2.0 ** (-8.0 * np.arange(1, 13) / 12)).reshape(3, 4)
    dil = [1, 4, 16]
    kp = np.arange(128)[:, None]; qp = np.arange(128)[None, :]
    out = []
    for (kind, j, d) in l0_bias_list():
        rel = d * 128 + kp - qp
        t = np.empty((128, 4, 128), np.float32)
        for h in range(4):
            if kind == "A":
                ok = np.abs(rel) <= 128; s = sl_a[4 * j + h]
            else:
                ok = (np.abs(rel) <= 64 * dil[j]) & (rel % dil[j] == 0); s = sl_b[j, h]
            t[:, h, :] = np.where(ok, -s * np.abs(rel), NEG)
        out.append(t.reshape(128, 512))
    return np.stack(out).astype(np.float32)


def host_bias_l1(rpb, r0, nrows):
    key = np.arange(128); q = np.arange(128)
    krl = (key // 64)[:, None]; kc = (key % 64)[:, None]; qrl = (q // 64)[None, :]; c = (q % 64)[None, :]
    cs = np.clip(c - 8, 0, 48); colok = (kc >= cs) & (kc < cs + 16); cidx = np.clip(kc - c + 15, 0, 30)
    out = np.empty((4, 7, 128, 4, 128), np.float32)
    for di, d in enumerate(range(-3, 4)):
        dr = 2 * d + krl - qrl
        if r0 is None:
            rowok = (dr >= -4) & (dr <= 3)
        else:
            r = r0 + qrl; rs = np.clip(r - 4, 0, nrows - 8); kr = r + dr
            rowok = (kr >= rs) & (kr < rs + 8)
        ridx = np.clip(dr + 7, 0, 14); ok = rowok & colok
        for a in range(16):
            out[a // 4, di, :, a % 4, :] = np.where(ok, rpb[a][ridx, cidx], NEG)
    return out.reshape(28, 128, 512)


def build(cfg):
    nc = bass.Bass("TRN2", target_bir_lowering=False)
    nseg = len(cfg.own); E = cfg.E
    L0, L1 = layer_specs(); LS = [L0, L1]
    NU0 = len(l0_bias_list())

    def din(name, shape, dt=F32): return nc.dram_tensor(name, list(shape), dt, kind="ExternalInput")
    def dsc(name, shape, dt): return nc.dram_tensor(name, list(shape), dt)
    xe = [din(f"xe{s}", [cfg.R0[s] * 128, D]) for s in range(nseg)]
    cin = din("c", [nseg, D])
    kval0 = din("kval0", [128, cfg.NR0]); kval1 = din("kval1", [128, cfg.NQ0]); tval0 = din("tval0", [128, cfg.NQ0])
    ident_in = din("ident", [128, 128]); ustrict_in = din("ustrict", [128, 128]); bones_in = din("bones", [128, 128])
    ecol_in = [din(f"ecol{l}", [128, E]) for l in range(2)]
    bias0_in = din("bias0", [NU0, 128, 512]); bias1_in = din("bias1", [1 + 4 * nseg, 28, 128, 512])
    W = []
    for l in range(2):
        w = dict(ada_w=din(f"l{l}_ada_w", [D, 6 * D]), ada_b=din(f"l{l}_ada_b", [1, 6 * D]), norm1=din(f"l{l}_norm1", [1, D]),
                 w_in=din(f"l{l}_w_in", [D, 3072]), gains=din(f"l{l}_gains", [128, 4]), w_out=din(f"l{l}_w_out", [LS[l]["nko"] * 128, D]),
                 norm2=din(f"l{l}_norm2", [1, D]), w_router=din(f"l{l}_w_router", [D, E]), b_router=din(f"l{l}_b_router", [1, E]),
                 w_gu=din(f"l{l}_w_gate_up", [E, D, 2048]), b_gu=din(f"l{l}_b_gate_up", [E, 2048]),
                 w_d=din(f"l{l}_w_down", [E, 1024, D]), b_d=din(f"l{l}_b_down", [E, D]))
        W.append(w)
    sink_in = din("sink", [1, 8])
    youts = [nc.dram_tensor(f"y{s}", [cfg.own[s] * 128, D], F32, kind="ExternalOutput") for s in range(nseg)]
    cnt_out = nc.dram_tensor("cnt_out", [2, E], F32, kind="ExternalOutput") if getattr(cfg, 'dbg', {}).get('cnt_out') else None
    idx_out = nc.dram_tensor("idx_out", [2, 128, cfg.NQ0 * 4], I32, kind="ExternalOutput") if getattr(cfg, 'dbg', {}).get('idx_out') else None
    modd = dsc("modd", [nseg, 6 * D], F32)
    NRl = [cfg.NR0, cfg.NQ0]; NQl = [cfg.NQ0, cfg.NO]
    qT = [dsc(f"qT{l}", [2 * LS[l]["nqb"], 64, NQl[l] * 128], BF16) for l in range(2)]
    kT = [dsc(f"kT{l}", [2 * LS[l]["nkb"], 64, NRl[l] * 128], BF16) for l in range(2)]
    Vd = [dsc(f"V{l}", [NRl[l] * 128, LS[l]["nvh"] * 65], BF16) for l in range(2)]
    x1d = [dsc(f"x1_{l}", [NQl[l] * 128, D], F32) for l in range(2)]
    x2d = dsc("x2", [cfg.NQ0 * 128, D], F32)
    Xg = [dsc(f"Xg{l}", [E * cfg.C[l], D], BF16) for l in range(2)]
    EH = E // 2
    YgH = [[dsc(f"Yg{l}_{hh}", [EH * cfg.C[l], D], F32) for hh in range(2)] for l in range(2)]
    winb = [dsc(f"winb{l}", [D, 3072], BF16) for l in range(2)]
    woutb = [dsc(f"woutb{l}", [LS[l]["nko"] * 128, D], BF16) for l in range(2)]

    es = contextlib.ExitStack()
    nsem = [0]
    class Sem:
        def __init__(s, name):
            nsem[0] += 1; s.h = es.enter_context(nc.semaphore(f"{name}_{nsem[0]}")); s.n = 0
    waited = {}
    def WAIT(eng, *toks):
        for t in toks:
            if t is None: continue
            if isinstance(t, list): WAIT(eng, *t); continue
            sem, v = t; key = (id(eng), id(sem))
            if hasattr(sem, "dq"): sem.dq.closed = True
            if waited.get(key, 0) >= v: continue
            waited[key] = v; eng.wait_ge(sem.h, v)
    def SIG(instr, sem, by=1):
        instr.then_inc(sem.h, by); sem.n += by; return (sem, sem.n)
    PE, ACT, DVE, POOL, SP = nc.tensor, nc.scalar, nc.vector, nc.gpsimd, nc.sync
    sPE = Sem("pe"); sACT = Sem("act"); sDVE = Sem("dve"); sPOOL = Sem("pool")
    def pe(i): return SIG(i, sPE)
    def act(i): return SIG(i, sACT)
    def dve(i): return SIG(i, sDVE)
    def pool(i): return SIG(i, sPOOL)
    class DQ:
        def __init__(s, name): s.sem = Sem(name); s.sem.dq = s; s.closed = False
        def go(s, eng, meth, **kw):
            if s.closed:
                WAIT(eng, (s.sem, s.sem.n)); s.closed = False
            return SIG(getattr(eng, meth)(**kw), s.sem, 16)

    bufs = contextlib.ExitStack()
    uid = [0]
    def uname(name):
        uid[0] += 1; return f"{name}_u{uid[0]}"
    def sb(name, shape, dt): return bufs.enter_context(nc.sbuf_tensor(uname(name), list(shape), dt))
    def ps(name, shape, dt): return bufs.enter_context(nc.psum_tensor(uname(name), list(shape), dt))

    with es, contextlib.suppress(_Stop):
        pst = contextlib.ExitStack()
        def psb(name, shape, dt): return pst.enter_context(nc.sbuf_tensor(uname(name), list(shape), dt))
        ident = psb("ident", [128, 128], F32); identb = psb("identb", [128, 128], BF16)
        ustrict = psb("ustrict", [128, 128], BF16); onesb = psb("onesb", [128, 128], BF16); bones = psb("bonesb", [128, 128], BF16)
        ones1 = psb("ones1", [1, 128], F32); ctmp = psb("ctmp", [128, 128], F32)
        S4 = psb("S4", [128, cfg.NQ0, 4], I32); S4b = psb("S4b", [128, cfg.NQ0, 4], I32); G4 = psb("G4", [128, cfg.NQ0, 4], F32); G4B = psb("G4B", [128, cfg.NQ0, 4], F32)
        dq0 = DQ("c0")
        t = dq0.go(SP, "dma_start", out=ident[:], in_=ident_in[:, :])
        WAIT(DVE, t); dve(DVE.tensor_copy(out=identb[:], in_=ident[:]))
        for src, dst in ((ustrict_in, ustrict), (bones_in, bones)):
            WAIT(SP, (sDVE, sDVE.n)); t = dq0.go(SP, "dma_start", out=ctmp[:], in_=src[:, :]); WAIT(DVE, t)
            dve(DVE.tensor_copy(out=dst[:], in_=ctmp[:]))
        DVE.memset(onesb[:], 1.0); tk_const = dve(DVE.memset(ones1[:], 1.0))
        for e_ in (PE, ACT, DVE, POOL, SP): WAIT(e_, tk_const)

        def barrier():
            toks = [dve(DVE.memset(ctmp[0:1, 0:1], 0.0)), act(ACT.copy(out=ctmp[0:1, 1:2], in_=ones1[0:1, 0:1])),
                    pool(POOL.memset(ctmp[0:1, 2:3], 0.0)), (sPE, sPE.n)]
            for e in (PE, ACT, DVE, POOL, SP): WAIT(e, *toks)

        dqc = DQ("cast"); cast_t = None
        for l in range(2):
            for r in range(0, D, 128):
                cast_t = dqc.go(POOL, "dma_start", out=winb[l][r:r + 128, :], in_=W[l]["w_in"][r:r + 128, :], max_dma_last_dim=4096)
            for r in range(0, LS[l]["nko"] * 128, 128):
                cast_t = dqc.go(POOL, "dma_start", out=woutb[l][r:r + 128, :], in_=W[l]["w_out"][r:r + 128, :], max_dma_last_dim=4096)

        for l in (range(2) if not getattr(cfg, 'stop', None) else _stoprange(cfg)):
            Ls = LS[l]; Wl = W[l]; C = cfg.C[l]
            Rc = cfg.R0 if l == 0 else cfg.Q0; Roff = cfg.R0off if l == 0 else cfg.Q0off
            Qc = cfg.Q0 if l == 0 else cfg.own; Qoff = cfg.Q0off if l == 0 else cfg.Ooff
            q2r = 8 if l == 0 else 2
            xsrc = [xe[s] for s in range(nseg)] if l == 0 else [x2d[cfg.Q0off[s] * 128:(cfg.Q0off[s] + cfg.Q0[s]) * 128, :] for s in range(nseg)]
            kvald = kval0 if l == 0 else kval1

            with contextlib.ExitStack() as bufs_:
                bufs = bufs_
                cT = sb("cT", [128, nseg, 8], F32); scT = sb("scT", [128, 8, 128], F32)
                aw = [sb(f"aw{i}", [128, 8, 512], F32) for i in range(2)]; brow = sb("brow", [1, 6 * D], F32)
                mrow = sb("mrow", [128, 6 * D], F32); pm = [ps(f"pm{i}", [128, 512], F32) for i in range(2)]
                dqa = [DQ("aw0"), DQ("aw1")]; dql = DQ("la")
                for s in range(nseg):
                    t = dql.go(SP, "dma_start", out=cT[:, s, :], in_=cin[s:s + 1, :].rearrange("o (k p) -> p (o k)", p=128), allow_slow_non_contiguous=True)
                t = dql.go(SP, "dma_start", out=brow[:], in_=Wl["ada_b"][:, :])
                WAIT(ACT, t); ta = act(ACT.activation(out=cT[:], in_=cT[:], func=AF.Silu))
                WAIT(DVE, ta, t); half = 128 // nseg
                for s in range(nseg):
                    tsc = dve(DVE.tensor_copy(out=scT[:, :, s * half:(s + 1) * half], in_=cT[:, s, :].unsqueeze(2).to_broadcast([128, 8, half])))
                awfree = [None, None]; pmfree = [None, None]
                for j in range(12):
                    i = j % 2
                    WAIT(SP, awfree[i]); tl = dqa[i].go(SP, "dma_start", out=aw[i][:], in_=Wl["ada_w"][:, j * 512:(j + 1) * 512].rearrange("(k p) n -> p k n", p=128))
                    WAIT(PE, tl, tsc, pmfree[i], tk_const)
                    for k in range(8):
                        PE.matmul(pm[i][:], lhsT=scT[:, k, :], rhs=aw[i][:, k, :], start=(k == 0), stop=False)
                    tp = pe(PE.matmul(pm[i][:], lhsT=ones1[:].to_broadcast([1, 128]) if False else ones1[:], rhs=brow[:, j * 512:(j + 1) * 512], start=False, stop=True))
                    awfree[i] = tp
                    WAIT(DVE, tp); pmfree[i] = dve(DVE.tensor_copy(out=mrow[:, j * 512:(j + 1) * 512], in_=pm[i][:]))
                WAIT(POOL, pmfree[0], pmfree[1]); dqs = DQ("ms")
                for s in range(nseg):
                    t = dqs.go(POOL, "dma_start", out=modd[s:s + 1, :], in_=mrow[s * half:s * half + 1, :])
                WAIT(POOL, t)
            barrier()
            if getattr(cfg, 'stop', None) == (l, 'A'): raise _Stop()

            with contextlib.ExitStack() as bufs_:
                bufs = bufs_
                wsb = sb("wsb", [128, 8, 3072], BF16); gains = sb("gains", [128, 4], F32); g1b = sb("g1b", [128, D], F32)
                Ab = sb("Ab", [128, D], F32); Bb = sb("Bb", [128, D], F32)
                xt = [sb(f"xt{i}", [128, D], F32) for i in range(2)]; junk = sb("junk", [128, D], BF16)
                st = [sb(f"st{i}", [128, 4], F32) for i in range(2)]
                hb = [sb(f"hb{i}", [128, D], BF16) for i in range(2)]; hT = [sb(f"hT{i}", [128, 8, 512], BF16) for i in range(2)]
                sq = [sb(f"sq{i}", [128, 512], BF16) for i in range(2)]; rs = [sb(f"rs{i}", [128, 512], F32) for i in range(2)]
                qk = [sb(f"qk{i}", [128, 512], BF16) for i in range(2)]; vx = [sb(f"vx{i}", [128, Ls["nvh"], 65], BF16) for i in range(2)]
                ptr = ps("ptr", [128, D], BF16); pq = [ps(f"pq{i}", [128, 512], F32) for i in range(2)]
                p2 = [ps(f"p2{i}", [128, 512], F32) for i in range(2)]; pv = [ps(f"pv{i}", [128, 512], F32) for i in range(2)]
                dqw = DQ("w"); dqx = [DQ("x0"), DQ("x1")]; dqm = DQ("m"); dqo = [DQ("qk0"), DQ("qk1")]; dqv = [DQ("v0"), DQ("v1")]
                WAIT(POOL, cast_t)
                tw = dqw.go(POOL, "dma_start", out=wsb[:], in_=winb[l].ap().rearrange("(k p) n -> p k n", p=128))
                dqg_ = DQ("gn"); tg = dqg_.go(SP, "dma_start", out=gains[:], in_=Wl["gains"][:, :])
                WAIT(DVE, tg)
                DVE.tensor_scalar(out=gains[:, 0:1], in0=gains[:, 0:1], scalar1=0.125, scalar2=None, op0=ALU.mult)
                tgain = dve(DVE.tensor_scalar(out=gains[:, 2:3], in0=gains[:, 2:3], scalar1=0.125, scalar2=None, op0=ALU.mult))
                for i in range(2):
                    tvx = dve(DVE.memset(vx[i][:], 1.0))
                xfree = [None, None]; hbfree = [None, None]; hTfree = [None, None]; ptrfree = None
                pqfree = [None, None]; p2free = [None, None]; pvfree = [None, None]; sqfree = [None, None]; rsfree = [None, None]
                qkfree = [None, None]; vxfree = [None, None]; stfree = [None, None]
                ti = 0; gi = 0; bi = 0; vi = 0
                for s in range(nseg):
                    WAIT(SP, (sDVE, sDVE.n), (sPE, sPE.n))
                    t1 = dqm.go(SP, "dma_start", out=g1b[:], in_=Wl["norm1"][0:1, :].to_broadcast([128, D]))
                    t1 = dqm.go(SP, "dma_start", out=Ab[:], in_=modd[s:s + 1, D:2 * D].to_broadcast([128, D]))
                    t1 = dqm.go(SP, "dma_start", out=Bb[:], in_=modd[s:s + 1, 0:D].to_broadcast([128, D]))
                    WAIT(DVE, t1)
                    tAB = dve(DVE.scalar_tensor_tensor(out=Ab[:], in0=Ab[:], scalar=1.0, in1=g1b[:], op0=ALU.add, op1=ALU.mult))
                    ngroups = (Rc[s] + 3) // 4
                    for g in range(ngroups):
                        tiles = list(range(g * 4, min(g * 4 + 4, Rc[s]))); ntok = len(tiles) * 128; gs = gi % 2; gi += 1
                        thT = []
                        for jt, tc in enumerate(tiles):
                            i = ti % 2; ti += 1
                            WAIT(SP, xfree[i]); tx = dqx[i].go(SP, "dma_start", out=xt[i][:], in_=xsrc[s][tc * 128:(tc + 1) * 128, :])
                            WAIT(DVE, tx, stfree[i], tAB)
                            ta_ = dve(DVE.scalar_tensor_tensor(out=junk[:], in0=xt[i][:], scalar=1.0, in1=xt[i][:], op0=ALU.mult, op1=ALU.mult, accum_out=st[i][:, 0:1]))
                            WAIT(DVE, ta_); tb_ = dve(DVE.tensor_scalar(out=st[i][:, 1:2], in0=st[i][:, 0:1], scalar1=1.0 / D, scalar2=1e-6, op0=ALU.mult, op1=ALU.add))
                            WAIT(ACT, tb_); tc_ = act(ACT.activation(out=st[i][:, 2:3], in_=st[i][:, 1:2], func=AF.Sqrt))
                            WAIT(DVE, tc_); td_ = dve(DVE.reciprocal(out=st[i][:, 3:4], in_=st[i][:, 2:3]))
                            WAIT(DVE, td_); te_ = dve(DVE.scalar_tensor_tensor(out=xt[i][:], in0=xt[i][:], scalar=st[i][:, 3:4], in1=Ab[:], op0=ALU.mult, op1=ALU.mult))
                            WAIT(DVE, te_, hbfree[i]); th = dve(DVE.tensor_tensor(out=hb[i][:], in0=xt[i][:], in1=Bb[:], op=ALU.add))
                            xfree[i] = th; stfree[i] = th
                            WAIT(PE, th, ptrfree)
                            for k in range(8):
                                tt = PE.transpose(ptr[:, k * 128:(k + 1) * 128], hb[i][:, k * 128:(k + 1) * 128], identb[:])
                            tt = pe(tt); hbfree[i] = tt
                            WAIT(DVE, tt, hTfree[gs] if jt == 0 else None)
                            ptrfree = dve(DVE.tensor_copy(out=hT[gs][:, :, jt * 128:(jt + 1) * 128], in_=ptr[:].rearrange("p (k n) -> p k n", k=8)))
                            thT.append(ptrfree)
                        qlo = q2r * 128; qhi = (q2r + Qc[s]) * 128; glo = g * 512; ghi = glo + ntok
                        olo = max(glo, qlo); ohi = min(ghi, qhi)
                        blocks = [("k", b) for b in range(Ls["nkb"])] + ([("q", b) for b in range(Ls["nqb"])] if ohi > olo else [])
                        lastmm = None
                        for (kind, b) in blocks:
                            i = bi % 2; bi += 1
                            if kind == "k":
                                c0 = Ls["kcols"][b]; gcol = Ls["kgain"][b]
                            else:
                                c0 = Ls["qcols"][b]; gcol = Ls["qgain"][b]
                            lhs = lambda k, c0=c0: wsb[:, k, c0:c0 + 128]
                            WAIT(PE, thT, tw, pqfree[i])
                            for k in range(8):
                                mm = PE.matmul(pq[i][:, 0:ntok], lhsT=lhs(k), rhs=hT[gs][:, k, 0:ntok], start=(k == 0), stop=(k == 7))
                            tmm = pe(mm); lastmm = tmm
                            WAIT(ACT, tmm, sqfree[i]); tsq = act(ACT.activation(out=sq[i][:, 0:ntok], in_=pq[i][:, 0:ntok], func=AF.Square))
                            WAIT(PE, tsq, p2free[i]); tss = pe(PE.matmul(p2[i][:, 0:ntok], lhsT=bones[:], rhs=sq[i][:, 0:ntok], start=True, stop=True))
                            sqfree[i] = tss
                            WAIT(ACT, tss, rsfree[i]); tsr = act(ACT.activation(out=rs[i][:, 0:ntok], in_=p2[i][:, 0:ntok], func=AF.Sqrt, bias=1e-6, scale=1.0 / 64))
                            p2free[i] = tsr
                            WAIT(DVE, tsr); trc = dve(DVE.reciprocal(out=rs[i][:, 0:ntok], in_=rs[i][:, 0:ntok]))
                            WAIT(DVE, trc, qkfree[i], tgain)
                            tqk = dve(DVE.scalar_tensor_tensor(out=qk[i][:, 0:ntok], in0=pq[i][:, 0:ntok], scalar=gains[:, gcol:gcol + 1], in1=rs[i][:, 0:ntok], op0=ALU.mult, op1=ALU.mult))
                            pqfree[i] = tqk; rsfree[i] = tqk
                            WAIT(POOL, tqk)
                            for hf in range(2):
                                if kind == "k":
                                    qkfree[i] = dqo[i].go(POOL, "dma_start", out=kT[l][2 * b + hf, :, (Roff[s] * 128 + glo):(Roff[s] * 128 + ghi)], in_=qk[i][hf * 64:hf * 64 + 64, 0:ntok])
                                else:
                                    qkfree[i] = dqo[i].go(POOL, "dma_start", out=qT[l][2 * b + hf, :, (Qoff[s] * 128 + olo - qlo):(Qoff[s] * 128 + ohi - qlo)], in_=qk[i][hf * 64:hf * 64 + 64, olo - glo:ohi - glo])
                        for jt, tc in enumerate(tiles):
                            i = vi % 2; vi += 1
                            WAIT(DVE, vxfree[i], tvx)
                            for (c0, ncol, vh0) in Ls["vsets"]:
                                WAIT(PE, thT, tw, pvfree[i])
                                for k in range(8):
                                    mm = PE.matmul(pv[i][:, 0:ncol], lhsT=hT[gs][:, k, jt * 128:(jt + 1) * 128], rhs=wsb[:, k, c0:c0 + ncol], start=(k == 0), stop=(k == 7))
                                tmm = pe(mm); lastmm = tmm
                                WAIT(DVE, tmm)
                                pvfree[i] = dve(DVE.tensor_copy(out=vx[i][:, vh0:vh0 + ncol // 64, 0:64], in_=pv[i][:, 0:ncol].rearrange("p (h e) -> p h e", e=64)))
                            WAIT(POOL, pvfree[i])
                            r0 = (Roff[s] + tc) * 128
                            vxfree[i] = dqv[i].go(POOL, "dma_start", out=Vd[l][r0:r0 + 128, :], in_=vx[i][:].rearrange("p h e -> p (h e)"))
                        hTfree[gs] = lastmm
                WAIT(POOL, qkfree, vxfree)
            barrier()
            if getattr(cfg, 'stop', None) == (l, 'B'): raise _Stop()

            with contextlib.ExitStack() as bufs_:
                bufs = bufs_
                quads = Ls["quads"]; nquad = len(quads); nko = Ls["nko"]
                if l == 0:
                    bl = l0_bias_list(); bidx = {k: i for i, k in enumerate(bl)}; nbias = NU0
                else:
                    bidx = {("C", qd, d): qd * 7 + d + 3 for qd in range(4) for d in range(-3, 4)}; nbias = 28
                biasr = sb("biasr", [128, nbias, 512], BF16)
                biase = [sb(f"biase{i}", [128, 28, 512], BF16) for i in range(1)] if l == 1 else None; biasefree = None
                wo = sb("wo", [128, nko, D], BF16); kv = sb("kv", [128, NRl[l]], F32); gate1 = sb("gate1", [128, D], F32)
                esink = sb("esink", [128, 8], F32)
                dmin = min(u[1] for q in quads for u in q["units"]); dmax = max(u[1] for q in quads for u in q["units"]); nwin = dmax - dmin + 1
                nqh = 2 * Ls["nqb"]; wgr = Ls["wgroups"]
                qs = [sb(f"qs{i}", [64, nqh, 128], BF16) for i in range(2)]
                ks = [[sb(f"ks{i}_{g}", [64, h1 - h0, (2 * r + 1) * 128], BF16) for (h0, h1, r) in wgr] for i in range(2)]
                vs = [[sb(f"vs{i}_{g}", [128, 2 * r + 1, (h1 - h0) * 65], BF16) for (h0, h1, r) in wgr] for i in range(2)]
                def wg_of(kh):
                    for g, (h0, h1, r) in enumerate(wgr):
                        if h0 <= kh < h1: return g, kh - h0, r
                    raise KeyError(kh)
                xq = [sb(f"xq{i}", [128, D], F32) for i in range(2)]
                Eb = [sb(f"E{i}", [128, 512], BF16) for i in range(3)]
                attn = sb("attn", [128, nko * 128], BF16); attnT = sb("attnT", [128, nko, 128], BF16); rden = sb("rden", [128, 16], F32)
                x1t = [sb(f"x1t{i}", [128, D], F32) for i in range(2)]
                pS = [ps(f"pS{i}", [128, 512], F32) for i in range(2)]; pO_ = [ps(f"pO{i}", [128, 512], F32) for i in range(nquad)]; pO = [p_[:, 0:260].rearrange("p (h e) -> p h e", e=65) for p_ in pO_]
                pT = ps("pT", [128, D], BF16); pW = ps("pW", [128, 512], F32)
                dqb = DQ("b"); dqk = [DQ("k0"), DQ("k1")]; dqe = [DQ("e0"), DQ("e1")]; dqx1 = [DQ("x10"), DQ("x11")]
                WAIT(POOL, cast_t)
                t = dqb.go(POOL, "dma_start", out=wo[:], in_=woutb[l].ap().rearrange("(k p) n -> p k n", p=128))
                bsrc = bias0_in if l == 0 else bias1_in[0]
                for u in range(nbias):
                    t = dqb.go(POOL, "dma_start", out=biasr[:, u, :], in_=bsrc[u, :, :])
                dqb2 = DQ("b2"); dqg1 = DQ("g1")
                t2 = dqb2.go(SP, "dma_start", out=kv[:], in_=kvald[:, :])
                t2 = dqb2.go(SP, "dma_start", out=esink[:], in_=sink_in[0:1, :].to_broadcast([128, 8]))
                tres = [t, t2]
                WAIT(ACT, t2); tsink = act(ACT.activation(out=esink[:], in_=esink[:], func=AF.Exp))
                slotfree = [None, None]; xqfree = [None, None]; Sfree = [None, None]; Efree = [None, None, None]
                Ofree = [None] * nquad; pTfree = None; attnfree = None; attnTfree = None; pWfree = None; x1free = [None, None]; rdenfree = None
                qi = 0; ui = 0; ei = 0
                for s in range(nseg):
                    WAIT(SP, (sDVE, sDVE.n)); tg1 = dqg1.go(SP, "dma_start", out=gate1[:], in_=modd[s:s + 1, 2 * D:3 * D].to_broadcast([128, D]))
                    for qc in range(min(Qc[s], getattr(cfg, 'dbg', {}).get('c_blocks', 10**9))):
                        i = qi % 2; qi += 1
                        rc = qc + q2r
                        edge = None
                        if l == 1:
                            if qc < 2: edge = 1 + 4 * s + qc
                            elif qc >= Qc[s] - 2: edge = 1 + 4 * s + 2 + (qc - (Qc[s] - 2))
                        WAIT(SP, slotfree[i], xqfree[i])
                        qcol = (Qoff[s] + qc) * 128
                        dqk[i].go(SP, "dma_start", out=qs[i][:], in_=qT[l][:, :, qcol:qcol + 128].rearrange("b p t -> p b t"))
                        los = []
                        for g, (h0, h1, r) in enumerate(wgr):
                            lo = max(0, rc - r); hi = min(Rc[s] - 1, rc + r); nld = hi - lo + 1; los.append(lo)
                            kcol = (Roff[s] + lo) * 128
                            dqk[i].go(SP, "dma_start", out=ks[i][g][:, :, 0:nld * 128], in_=kT[l][h0:h1, :, kcol:kcol + nld * 128].rearrange("b p t -> p b t"))
                            dqk[i].go(SP, "dma_start", out=vs[i][g][:, 0:nld, :], in_=Vd[l][kcol:kcol + nld * 128, h0 * 65:h1 * 65].rearrange("(c p) f -> p c f", p=128))
                        xrow = (rc * 128) if l == 0 else ((cfg.Q0off[s] + rc) * 128)
                        xs_ = xe[s] if l == 0 else x2d
                        tld = dqk[i].go(SP, "dma_start", out=xq[i][:], in_=xs_[xrow:xrow + 128, :])
                        if edge is not None:
                            eb = 0
                            WAIT(POOL, slotfree[i], biasefree)
                            for u in range(28):
                                tedge = dqe[eb].go(POOL, "dma_start", out=biase[eb][:, u, :], in_=bias1_in[edge, u, :, :])
                        stage = getattr(cfg, 'dbg', {}).get('c_stage', 9)
                        if stage < 1: continue
                        for qd, quad in enumerate(quads):
                            units = quad["units"]
                            if l == 1:
                                units = [u for u in units if (edge is not None) or (-2 <= u[1] <= 2)]
                            def emit_pv(pv):
                                un_, e3_, cidx_, wg_, heads_, tE_ = pv
                                WAIT(PE, tE_, Ofree[qd] if un_ == 0 else None)
                                for h, (qh, kh) in enumerate(heads_):
                                    _, khl, _ = wg_of(kh)
                                    mm_ = PE.matmul(pO[qd][:, h, :], lhsT=Eb[e3_][:, h * 128:(h + 1) * 128], rhs=vs[i][wg_][:, cidx_, khl * 65:(khl + 1) * 65],
                                                    start=(un_ == 0 and h == 0), stop=(un_ == len(units) - 1), skip_group_check=True)
                                Efree[e3_] = pe(mm_); return Efree[e3_]
                            prev = None; tlastpv = None
                            for un, (heads, d, bkey) in enumerate(units):
                                si = ui % 2; e3 = ui % 3; ui += 1
                                wg, _, _ = wg_of(heads[0][1]); lo = los[wg]
                                cidx = min(max(rc + d, 0), Rc[s] - 1) - lo
                                btile = biasr[:, bidx[bkey], :] if edge is None else biase[eb][:, bidx[bkey], :]
                                WAIT(PE, tld, tres, Sfree[si], tedge if edge is not None else None)
                                PE.matmul(pS[si][:], lhsT=identb[:], rhs=btile, start=True, stop=False)
                                for h, (qh, kh) in enumerate(heads):
                                    _, khl, _ = wg_of(kh)
                                    mm = PE.matmul(pS[si][:, h * 128:(h + 1) * 128], lhsT=ks[i][wg][:, khl, cidx * 128:(cidx + 1) * 128],
                                                   rhs=qs[i][:, qh, :], start=False, stop=(h == 3))
                                tS = pe(mm)
                                if edge is not None: biasefree = tS
                                if stage < 2: continue
                                kcolv = Roff[s] + lo + cidx
                                WAIT(ACT, tS, Efree[e3])
                                tE = act(ACT.activation(out=Eb[e3][:], in_=pS[si][:], func=AF.Exp, bias=kv[:, kcolv:kcolv + 1], scale=1.0))
                                Sfree[si] = tE
                                if prev is not None: tlastpv = emit_pv(prev)
                                prev = (un, e3, cidx, wg, heads, tE)
                            if prev is not None: tlastpv = emit_pv(prev)
                            if stage < 4: continue
                            tO = tlastpv
                            WAIT(DVE, tO, rdenfree, tsink)
                            if quad["sink"]:
                                td1 = dve(DVE.tensor_tensor(out=rden[:, qd * 4:qd * 4 + 4], in0=pO[qd][:, :, 64], in1=esink[:, qd * 4:qd * 4 + 4], op=ALU.add))
                            else:
                                td1 = dve(DVE.tensor_copy(out=rden[:, qd * 4:qd * 4 + 4], in_=pO[qd][:, :, 64]))
                            WAIT(DVE, td1); td2 = dve(DVE.reciprocal(out=rden[:, qd * 4:qd * 4 + 4], in_=rden[:, qd * 4:qd * 4 + 4]))
                            WAIT(DVE, td2, attnfree if qd == 0 else None)
                            tat = dve(DVE.tensor_tensor(out=attn[:, qd * 256:(qd + 1) * 256].rearrange("p (h e) -> p h e", e=64), in0=pO[qd][:, :, 0:64],
                                                        in1=rden[:, qd * 4:qd * 4 + 4].unsqueeze(2).to_broadcast([128, 4, 64]), op=ALU.mult))
                            Ofree[qd] = tat
                        if stage < 5: continue
                        slotfree[i] = tlastpv; rdenfree = tat
                        WAIT(PE, tat, pTfree)
                        for k in range(nko):
                            tt = PE.transpose(pT[:, k * 128:(k + 1) * 128], attn[:, k * 128:(k + 1) * 128], identb[:])
                        tt = pe(tt); attnfree = tt
                        WAIT(DVE, tt, attnTfree)
                        tcp = dve(DVE.tensor_copy(out=attnT[:], in_=pT[:, 0:nko * 128].rearrange("p (k n) -> p k n", k=nko))); pTfree = tcp
                        xi = qi % 2
                        WAIT(DVE, x1free[xi], tg1)
                        for hh in range(2):
                            WAIT(PE, tcp, pWfree)
                            for k in range(nko):
                                mm = PE.matmul(pW[:], lhsT=attnT[:, k, :], rhs=wo[:, k, hh * 512:(hh + 1) * 512], start=(k == 0), stop=(k == nko - 1))
                            tw_ = pe(mm)
                            WAIT(DVE, tw_)
                            tm1 = dve(DVE.tensor_tensor(out=x1t[xi][:, hh * 512:(hh + 1) * 512], in0=pW[:], in1=gate1[:, hh * 512:(hh + 1) * 512], op=ALU.mult)); pWfree = tm1
                            WAIT(DVE, tm1)
                            tx1 = dve(DVE.tensor_tensor(out=x1t[xi][:, hh * 512:(hh + 1) * 512], in0=x1t[xi][:, hh * 512:(hh + 1) * 512], in1=xq[i][:, hh * 512:(hh + 1) * 512], op=ALU.add))
                        attnTfree = tw_; xqfree[i] = tx1
                        WAIT(POOL, tx1)
                        r0 = (Qoff[s] + qc) * 128
                        x1free[xi] = dqx1[xi].go(POOL, "dma_start", out=x1d[l][r0:r0 + 128, :], in_=x1t[xi][:])
                WAIT(POOL, x1free)
            barrier()
            if getattr(cfg, 'stop', None) == (l, 'C'): raise _Stop()

            NT = NQl[l]; bc_reg = POOL.to_reg(E * C - 1); bch_reg = POOL.to_reg(EH * C - 1)
            with contextlib.ExitStack() as bufs_:
                bufs = bufs_
                g2b = sb("g2b", [128, D], F32); A2 = sb("A2", [128, D], F32); B2 = sb("B2", [128, D], F32)
                wr = sb("wr", [128, 8, E], F32); br = sb("br", [1, E], F32); ecol = sb("ecol", [128, E], F32); tv = sb("tv", [128, cfg.NQ0], F32)
                cnt = sb("cnt", [128, E], F32)
                xt = [sb(f"dxt{i}", [128, D], F32) for i in range(2)]; junk = sb("djunk", [128, D], BF16); st = [sb(f"dst{i}", [128, 4], F32) for i in range(2)]
                h2 = [sb(f"h2{i}", [128, D], F32) for i in range(2)]; h2b = [sb(f"h2b{i}", [128, D], BF16) for i in range(2)]
                h2T = sb("h2T", [128, 8, 128], F32)
                sm = [sb(f"sm{i}", [128, 8, E], F32) for i in range(2)]
                m8 = [sb(f"m8{i}", [128, 4, 8], F32) for i in range(2)]
                Mb = [sb(f"Mb{i}", [128, E], BF16) for i in range(2)]
                zt = sb("zt", [128, D], BF16)
                pTa = ps("dpTa", [128, 512], F32); pTb = ps("dpTb", [128, 512], F32); pL_ = ps("pL", [128, 512], F32); pL = pL_[:, 0:E]; pR_ = ps("pR", [128, 512], F32); pR = pR_[:, 0:2 * E].rearrange("p (a e) -> p a e", a=2)
                dql = DQ("dl"); dql2 = DQ("dl2"); dqx = [DQ("dx0"), DQ("dx1")]; dqsc = [DQ("sc0"), DQ("sc1")]; dqz = DQ("z")
                t = dql.go(SP, "dma_start", out=wr[:], in_=Wl["w_router"].ap().rearrange("(k p) e -> p k e", p=128))
                t = dql.go(SP, "dma_start", out=br[:], in_=Wl["b_router"][:, :])
                t = dql.go(SP, "dma_start", out=ecol[:], in_=ecol_in[l][:, :])
                if l == 0: t = dql.go(SP, "dma_start", out=tv[:], in_=tval0[:, :])
                tld0 = t
                tz = dve(DVE.memset(zt[:], 0.0)); tcnt = dve(DVE.memset(cnt[:], 0.0))
                if l == 1: tcnt = dve(DVE.memset(tv[:], 1.0))
                WAIT(POOL, tz)
                for r in range(0, E * C, 128):
                    tzero = dqz.go(POOL, "dma_start", out=Xg[l][r:r + 128, :], in_=zt[:])
                WAIT(POOL, tzero)
                xfree = [None, None]; stfree = [None, None]; h2free = [None, None]; h2bfree = [None, None]; smfree = [None, None]
                pTfree = None; h2Tfree = None; pLfree = None; pRfree = None; cntT = tcnt
                ti = 0
                for s in range(nseg):
                    WAIT(SP, (sDVE, sDVE.n))
                    t1 = dql2.go(SP, "dma_start", out=g2b[:], in_=Wl["norm2"][0:1, :].to_broadcast([128, D]))
                    t1 = dql2.go(SP, "dma_start", out=A2[:], in_=modd[s:s + 1, 4 * D:5 * D].to_broadcast([128, D]))
                    t1 = dql2.go(SP, "dma_start", out=B2[:], in_=modd[s:s + 1, 3 * D:4 * D].to_broadcast([128, D]))
                    WAIT(DVE, t1); tAB = dve(DVE.scalar_tensor_tensor(out=A2[:], in0=A2[:], scalar=1.0, in1=g2b[:], op0=ALU.add, op1=ALU.mult))
                    for qc in range(Qc[s]):
                        i = ti % 2; ti += 1; tg = Qoff[s] + qc
                        WAIT(SP, xfree[i]); tx = dqx[i].go(SP, "dma_start", out=xt[i][:], in_=x1d[l][tg * 128:(tg + 1) * 128, :])
                        WAIT(DVE, tx, stfree[i], tAB)
                        ta_ = dve(DVE.scalar_tensor_tensor(out=junk[:], in0=xt[i][:], scalar=1.0, in1=xt[i][:], op0=ALU.mult, op1=ALU.mult, accum_out=st[i][:, 0:1]))
                        WAIT(DVE, ta_); tb_ = dve(DVE.tensor_scalar(out=st[i][:, 1:2], in0=st[i][:, 0:1], scalar1=1.0 / D, scalar2=1e-6, op0=ALU.mult, op1=ALU.add))
                        WAIT(ACT, tb_); tc_ = act(ACT.activation(out=st[i][:, 2:3], in_=st[i][:, 1:2], func=AF.Sqrt))
                        WAIT(DVE, tc_); td_ = dve(DVE.reciprocal(out=st[i][:, 3:4], in_=st[i][:, 2:3]))
                        WAIT(DVE, td_); te_ = dve(DVE.scalar_tensor_tensor(out=xt[i][:], in0=xt[i][:], scalar=st[i][:, 3:4], in1=A2[:], op0=ALU.mult, op1=ALU.mult))
                        WAIT(DVE, te_, h2free[i]); th = dve(DVE.tensor_tensor(out=h2[i][:], in0=xt[i][:], in1=B2[:], op=ALU.add))
                        xfree[i] = th; stfree[i] = th
                        WAIT(ACT, th, h2bfree[i]); thb = act(ACT.copy(out=h2b[i][:], in_=h2[i][:]))
                        WAIT(PE, th, pTfree)
                        for k in range(8):
                            tt = PE.transpose((pTa if k < 4 else pTb)[:, (k % 4) * 128:(k % 4 + 1) * 128], h2[i][:, k * 128:(k + 1) * 128], ident[:])
                        tt = pe(tt)
                        WAIT(DVE, tt, h2Tfree)
                        DVE.tensor_copy(out=h2T[:, 0:4, :], in_=pTa[:].rearrange("p (k n) -> p k n", k=4))
                        tcp = dve(DVE.tensor_copy(out=h2T[:, 4:8, :], in_=pTb[:].rearrange("p (k n) -> p k n", k=4))); pTfree = tcp
                        WAIT(PE, tcp, tld0, pLfree, tk_const)
                        for k in range(8):
                            PE.matmul(pL, lhsT=h2T[:, k, :], rhs=wr[:, k, :], start=(k == 0), stop=False)
                        tlg = pe(PE.matmul(pL, lhsT=ones1[:], rhs=br[:], start=False, stop=True)); h2Tfree = tlg
                        S = sm[i]; M8 = m8[i]
                        WAIT(DVE, tlg, smfree[i], tld0)
                        t_ = dve(DVE.tensor_copy(out=S[:, 0, :], in_=pL)); pLfree = t_
                        WAIT(DVE, t_); t_ = dve(DVE.max(out=M8[:, 0, :], in_=S[:, 0, :]))
                        WAIT(DVE, t_); tM = dve(DVE.tensor_scalar(out=S[:, 1, :], in0=S[:, 0, :], scalar1=M8[:, 0, 3:4], scalar2=None, op0=ALU.is_ge))
                        tn = dve(DVE.tensor_scalar(out=M8[:, 2, 0:1], in0=M8[:, 0, 0:1], scalar1=-1.0, scalar2=None, op0=ALU.mult))
                        WAIT(ACT, tn); tex = act(ACT.activation(out=S[:, 2, :], in_=S[:, 0, :], func=AF.Exp, bias=M8[:, 2, 0:1], scale=1.0))
                        WAIT(DVE, tex, tM); t_ = dve(DVE.scalar_tensor_tensor(out=S[:, 3, :], in0=S[:, 2, :], scalar=1.0, in1=S[:, 1, :], op0=ALU.mult, op1=ALU.mult, accum_out=M8[:, 2, 1:2]))
                        WAIT(DVE, t_); t_ = dve(DVE.reciprocal(out=M8[:, 2, 2:3], in_=M8[:, 2, 1:2]))
                        WAIT(DVE, t_); tG = dve(DVE.tensor_scalar(out=S[:, 4, :], in0=S[:, 3, :], scalar1=M8[:, 2, 2:3], scalar2=None, op0=ALU.mult))
                        tMv = dve(DVE.tensor_scalar(out=S[:, 7, :], in0=S[:, 1, :], scalar1=tv[:, tg:tg + 1], scalar2=None, op0=ALU.mult))
                        WAIT(DVE, tMv); tMb = dve(DVE.tensor_copy(out=Mb[i][:], in_=S[:, 7, :]))
                        WAIT(PE, tMb, pRfree)
                        PE.matmul(pR[:, 0, :], lhsT=ustrict[:], rhs=Mb[i][:], start=True, stop=True)
                        tR = pe(PE.matmul(pR[:, 1, :], lhsT=onesb[:], rhs=Mb[i][:], start=True, stop=True, skip_group_check=True))
                        WAIT(DVE, tR, cntT)
                        t_ = dve(DVE.tensor_tensor(out=S[:, 5, :], in0=pR[:, 0, :], in1=cnt[:], op=ALU.add))
                        WAIT(DVE, t_); cntT = dve(DVE.tensor_tensor(out=cnt[:], in0=pR[:, 1, :], in1=cnt[:], op=ALU.add)); pRfree = cntT
                        WAIT(DVE, t_); tok_ = dve(DVE.tensor_scalar(out=S[:, 6, :], in0=S[:, 5, :], scalar1=float(C), scalar2=None, op0=ALU.is_lt))
                        WAIT(DVE, tok_, tMb); tok_ = dve(DVE.tensor_tensor(out=S[:, 7, :], in0=S[:, 7, :], in1=S[:, 6, :], op=ALU.mult))
                        WAIT(DVE, tok_); t_ = dve(DVE.tensor_tensor(out=S[:, 5, :], in0=S[:, 5, :], in1=ecol[:], op=ALU.add))
                        WAIT(DVE, t_); tsv = dve(DVE.tensor_tensor(out=S[:, 5, :], in0=S[:, 5, :], in1=S[:, 7, :], op=ALU.mult))
                        WAIT(DVE, tsv); t_ = dve(DVE.max(out=M8[:, 1, :], in_=S[:, 5, :]))
                        WAIT(DVE, t_)
                        t_ = dve(DVE.tensor_scalar(out=M8[:, 3, 0:4], in0=M8[:, 1, 0:4], scalar1=0.5, scalar2=float(E * C + 8), op0=ALU.is_lt, op1=ALU.mult))
                        WAIT(DVE, t_); t_ = dve(DVE.scalar_tensor_tensor(out=M8[:, 3, 0:4], in0=M8[:, 1, 0:4], scalar=-1.0, in1=M8[:, 3, 0:4], op0=ALU.add, op1=ALU.add))
                        WAIT(DVE, t_); tidx = dve(DVE.tensor_copy(out=S4[:, tg, :], in_=M8[:, 3, 0:4]))
                        tflag = dve(DVE.tensor_scalar(out=M8[:, 3, 4:8], in0=M8[:, 3, 0:4], scalar1=float(EH * C), scalar2=None, op0=ALU.is_lt))
                        tlow = dve(DVE.tensor_scalar(out=M8[:, 2, 4:8], in0=M8[:, 3, 0:4], scalar1=float(EH * C), scalar2=float(2 * E * C), op0=ALU.is_lt, op1=ALU.mult))
                        WAIT(DVE, tlow); tlow = dve(DVE.scalar_tensor_tensor(out=M8[:, 2, 4:8], in0=M8[:, 3, 0:4], scalar=-float(EH * C), in1=M8[:, 2, 4:8], op0=ALU.add, op1=ALU.add))
                        WAIT(DVE, tlow); tidxb = dve(DVE.tensor_copy(out=S4b[:, tg, :], in_=M8[:, 2, 4:8]))
                        tgk = None
                        for k in range(4):
                            WAIT(DVE, tsv, tG, tgk)
                            t_ = dve(DVE.tensor_scalar(out=S[:, 6, :], in0=S[:, 5, :], scalar1=M8[:, 1, k:k + 1], scalar2=None, op0=ALU.is_equal))
                            WAIT(DVE, t_); tgk = dve(DVE.scalar_tensor_tensor(out=S[:, 7, :] if False else S[:, 6, :], in0=S[:, 6, :], scalar=1.0, in1=S[:, 4, :], op0=ALU.mult, op1=ALU.mult, accum_out=G4[:, tg, k:k + 1]))
                        WAIT(DVE, tgk); tgv = dve(DVE.tensor_scalar(out=G4B[:, tg, :], in0=G4[:, tg, :], scalar1=tv[:, tg:tg + 1], scalar2=None, op0=ALU.mult))
                        WAIT(DVE, tgv, tflag); tgv = dve(DVE.tensor_tensor(out=G4[:, tg, :], in0=G4B[:, tg, :], in1=M8[:, 3, 4:8], op=ALU.mult))
                        WAIT(DVE, tgv); tgv = dve(DVE.tensor_tensor(out=G4B[:, tg, :], in0=G4B[:, tg, :], in1=G4[:, tg, :], op=ALU.subtract))
                        smfree[i] = [tgv, tidxb]; h2free[i] = tlg
                        WAIT(POOL, tidx, thb)
                        for k in range(4):
                            tsc = dqsc[i].go(POOL, "indirect_dma_start", out=Xg[l][:, :], out_offset=bass.IndirectOffsetOnAxis(ap=S4[:, tg, k:k + 1], axis=0),
                                                                  in_=h2b[i][:, :], in_offset=None, bounds_check=bc_reg, oob_is_err=False)
                        h2bfree[i] = tsc
                WAIT(POOL, h2bfree)
                if idx_out is not None and l == 0:
                    dqix = DQ("ix"); WAIT(POOL, (sDVE, sDVE.n))
                    dqix.go(POOL, "dma_start", out=idx_out[0, :, :], in_=S4[:].rearrange("p t k -> p (t k)"))
                    tix = dqix.go(POOL, "dma_start", out=idx_out[1, :, :], in_=S4b[:].rearrange("p t k -> p (t k)")); WAIT(POOL, tix)
                if cnt_out is not None:
                    dqcn = DQ("cn"); WAIT(POOL, cntT)
                    tcn = dqcn.go(POOL, "dma_start", out=cnt_out[l:l + 1, :], in_=cnt[0:1, :]); WAIT(POOL, tcn)
            barrier()
            if getattr(cfg, 'stop', None) == (l, 'D'): raise _Stop()

            with contextlib.ExitStack() as bufs_:
                bufs = bufs_
                wgu = [sb(f"wgu{i}", [128, 8, 2048], BF16) for i in range(2)]; wd = [sb(f"wd{i}", [128, 8, D], BF16) for i in range(2)]
                bgu = [sb(f"bgu{i}", [128, 16], F32) for i in range(2)]; bd = [sb(f"bd{i}", [1, D], F32) for i in range(2)]
                xg = [sb(f"xg{i}", [128, 4, D], BF16) for i in range(2)]; xT = sb("xT", [128, 8, 512], BF16); aT = sb("aT", [128, 8, 512], BF16)
                gt = [sb(f"gt{i}", [128, 512], F32) for i in range(2)]; sg = [sb(f"sg{i}", [128, 512], F32) for i in range(2)]; ut = [sb(f"ut{i}", [128, 512], F32) for i in range(2)]
                yo = [sb(f"yo{i}", [128, D], F32) for i in range(2)]
                pT = ps("epT", [128, D], BF16); pG = [ps(f"pG{i}", [128, 512], F32) for i in range(2)]; pU = [ps(f"pU{i}", [128, 512], F32) for i in range(2)]
                pY = [ps(f"pY{i}", [128, 512], F32) for i in range(2)]
                dqw_ = [DQ("ew0"), DQ("ew1")]; dqx = [DQ("ex0"), DQ("ex1")]; dqy = [DQ("ey0"), DQ("ey1")]
                wfree = [None, None]; xgfree = [None, None]; pTfree = None; xTfree = None; aTfree = None
                pGfree = [None, None]; pUfree = [None, None]; gtfree = [None, None]; sgfree = [None, None]; utfree = [None, None]
                pYfree = [None, None]; yofree = [None, None]
                chunks = [(c0, min(512, C - c0)) for c0 in range(0, C, 512)]
                xi = 0; fi = 0; yi = 0; yoi = 0
                for e in range(E):
                    wi = e % 2
                    WAIT(POOL, wfree[wi])
                    for k in range(8):
                        dqw_[wi].go(POOL, "dma_start", out=wgu[wi][:, k, :], in_=Wl["w_gu"][e, k * 128:(k + 1) * 128, :], max_dma_last_dim=4096)
                        dqw_[wi].go(POOL, "dma_start", out=wd[wi][:, k, :], in_=Wl["w_d"][e, k * 128:(k + 1) * 128, :], max_dma_last_dim=4096)
                    dqw_[wi].go(POOL, "dma_start", out=bgu[wi][:], in_=Wl["b_gu"][e:e + 1, :].rearrange("o (f p) -> p (o f)", p=128), allow_slow_non_contiguous=True)
                    twl = dqw_[wi].go(POOL, "dma_start", out=bd[wi][:], in_=Wl["b_d"][e:e + 1, :])
                    for (c0, nsl) in chunks:
                        nb = nsl // 128; j = xi % 2; xi += 1
                        WAIT(SP, xgfree[j])
                        r0 = e * C + c0
                        txg = dqx[j].go(SP, "dma_start", out=xg[j][:, 0:nb, :], in_=Xg[l][r0:r0 + nsl, :].rearrange("(b p) d -> p b d", p=128))
                        for b in range(nb):
                            WAIT(PE, txg, pTfree)
                            for k in range(8):
                                tt = PE.transpose(pT[:, k * 128:(k + 1) * 128], xg[j][:, b, k * 128:(k + 1) * 128], identb[:])
                            tt = pe(tt)
                            WAIT(DVE, tt, xTfree if b == 0 else None)
                            pTfree = dve(DVE.tensor_copy(out=xT[:, :, b * 128:(b + 1) * 128], in_=pT[:].rearrange("p (k n) -> p k n", k=8)))
                        xgfree[j] = tt; txT = pTfree
                        for fp in range(8):
                            f = fi % 2; fi += 1
                            WAIT(PE, txT, twl, pGfree[f], pUfree[f])
                            for k in range(8):
                                mm = PE.matmul(pG[f][:, 0:nsl], lhsT=wgu[wi][:, k, fp * 128:(fp + 1) * 128], rhs=xT[:, k, 0:nsl], start=(k == 0), stop=(k == 7))
                            tg_ = pe(mm)
                            for k in range(8):
                                mm = PE.matmul(pU[f][:, 0:nsl], lhsT=wgu[wi][:, k, 1024 + fp * 128:1024 + (fp + 1) * 128], rhs=xT[:, k, 0:nsl], start=(k == 0), stop=(k == 7))
                            tu_ = pe(mm)
                            WAIT(DVE, tg_, gtfree[f])
                            t1 = dve(DVE.tensor_scalar(out=gt[f][:, 0:nsl], in0=pG[f][:, 0:nsl], scalar1=bgu[wi][:, fp:fp + 1], scalar2=7.0, op0=ALU.add, op1=ALU.min)); pGfree[f] = t1
                            WAIT(ACT, t1, sgfree[f]); t2 = act(ACT.activation(out=sg[f][:, 0:nsl], in_=gt[f][:, 0:nsl], func=AF.Sigmoid, scale=1.702))
                            WAIT(DVE, tu_, utfree[f])
                            t3 = dve(DVE.tensor_scalar(out=ut[f][:, 0:nsl], in0=pU[f][:, 0:nsl], scalar1=bgu[wi][:, 8 + fp:9 + fp], scalar2=7.0, op0=ALU.add, op1=ALU.min)); pUfree[f] = t3
                            WAIT(DVE, t3); t4 = dve(DVE.tensor_scalar(out=ut[f][:, 0:nsl], in0=ut[f][:, 0:nsl], scalar1=-7.0, scalar2=1.0, op0=ALU.max, op1=ALU.add))
                            WAIT(DVE, t4, t2); t5 = dve(DVE.tensor_tensor(out=gt[f][:, 0:nsl], in0=gt[f][:, 0:nsl], in1=sg[f][:, 0:nsl], op=ALU.mult)); sgfree[f] = t5
                            WAIT(DVE, t5, aTfree if fp == 0 else None)
                            t6 = dve(DVE.tensor_tensor(out=aT[:, fp, 0:nsl], in0=gt[f][:, 0:nsl], in1=ut[f][:, 0:nsl], op=ALU.mult)); gtfree[f] = t6; utfree[f] = t6
                        xTfree = tu_
                        for b in range(nb):
                            yb = yoi % 2; yoi += 1
                            WAIT(DVE, yofree[yb])
                            for hh in range(2):
                                y = yi % 2; yi += 1
                                WAIT(PE, t6, pYfree[y])
                                for k in range(8):
                                    PE.matmul(pY[y][:], lhsT=aT[:, k, b * 128:(b + 1) * 128], rhs=wd[wi][:, k, hh * 512:(hh + 1) * 512], start=(k == 0), stop=False)
                                ty = pe(PE.matmul(pY[y][:], lhsT=ones1[:], rhs=bd[wi][:, hh * 512:(hh + 1) * 512], start=False, stop=True))
                                WAIT(ACT, ty, yofree[yb]); pYfree[y] = act(ACT.copy(out=yo[yb][:, hh * 512:(hh + 1) * 512], in_=pY[y][:]))
                            WAIT(POOL, pYfree[y])
                            rr = (e % EH) * C + c0 + b * 128
                            yofree[yb] = dqy[yb].go(POOL, "dma_start", out=YgH[l][e // EH][rr:rr + 128, :], in_=yo[yb][:])
                        aTfree = ty
                    wfree[wi] = ty
                WAIT(POOL, yofree)
            barrier()
            if getattr(cfg, 'stop', None) == (l, 'E'): raise _Stop()

            with contextlib.ExitStack() as bufs_:
                bufs = bufs_
                gate2 = sb("gate2", [128, D], F32)
                yk = [sb(f"yk{i}", [128, 4, D], F32) for i in range(2)]; ykB = [sb(f"ykB{i}", [128, 4, D], F32) for i in range(2)]; xt = [sb(f"fx{i}", [128, D], F32) for i in range(2)]
                acc = [sb(f"acc{i}", [128, D], F32) for i in range(2)]
                dqg = [DQ("g0"), DQ("g1")]; dqx = [DQ("fx0"), DQ("fx1")]; dqo_ = [DQ("fo0"), DQ("fo1")]; dql = DQ("fl")
                for i in range(2):
                    DVE.memset(ykB[i][:], 0.0); tms = dve(DVE.memset(yk[i][:], 0.0))
                ykfree = [tms, tms]; xfree = [None, None]; accfree = [None, None]
                ti = 0
                for s in range(nseg):
                    WAIT(SP, (sDVE, sDVE.n)); tg2 = dql.go(SP, "dma_start", out=gate2[:], in_=modd[s:s + 1, 5 * D:6 * D].to_broadcast([128, D]))
                    for qc in range(Qc[s]):
                        i = ti % 2; ti += 1; tg = Qoff[s] + qc
                        WAIT(POOL, ykfree[i])
                        for hh, ixt, dst_ in ((0, S4, yk[i]), (1, S4b, ykB[i])):
                            for k in range(4):
                                tgt = dqg[i].go(POOL, "indirect_dma_start", out=dst_[:, k, :], out_offset=None, in_=YgH[l][hh][:, :],
                                                in_offset=bass.IndirectOffsetOnAxis(ap=ixt[:, tg, k:k + 1], axis=0), bounds_check=bch_reg, oob_is_err=False)
                        WAIT(SP, xfree[i]); tx = dqx[i].go(SP, "dma_start", out=xt[i][:], in_=x1d[l][tg * 128:(tg + 1) * 128, :])
                        WAIT(DVE, tgt, accfree[i], tg2)
                        t_ = dve(DVE.tensor_scalar(out=acc[i][:], in0=yk[i][:, 0, :], scalar1=G4[:, tg, 0:1], scalar2=None, op0=ALU.mult))
                        for k in range(1, 4):
                            WAIT(DVE, t_); t_ = dve(DVE.scalar_tensor_tensor(out=acc[i][:], in0=yk[i][:, k, :], scalar=G4[:, tg, k:k + 1], in1=acc[i][:], op0=ALU.mult, op1=ALU.add))
                        for k in range(4):
                            WAIT(DVE, t_); t_ = dve(DVE.scalar_tensor_tensor(out=acc[i][:], in0=ykB[i][:, k, :], scalar=G4B[:, tg, k:k + 1], in1=acc[i][:], op0=ALU.mult, op1=ALU.add))
                        ykfree[i] = t_
                        WAIT(DVE, t_); t_ = dve(DVE.tensor_tensor(out=acc[i][:], in0=acc[i][:], in1=gate2[:], op=ALU.mult))
                        WAIT(DVE, t_, tx); to = dve(DVE.tensor_tensor(out=acc[i][:], in0=acc[i][:], in1=xt[i][:], op=ALU.add)); xfree[i] = to
                        WAIT(POOL, to)
                        if l == 0:
                            accfree[i] = dqo_[i].go(POOL, "dma_start", out=x2d[tg * 128:(tg + 1) * 128, :], in_=acc[i][:])
                        else:
                            accfree[i] = dqo_[i].go(POOL, "dma_start", out=youts[s][qc * 128:(qc + 1) * 128, :], in_=acc[i][:])
                WAIT(POOL, accfree)
            barrier()
            if getattr(cfg, 'stop', None) == (l, 'F'): raise _Stop()
        pst.close()
    return nc


def seg_layout(cfg, core, seqlens):
    raise NotImplementedError


def make_inputs(cfg, core_segs, xs, cs, inp):
    E = cfg.E; m = {}
    kval0 = np.zeros((128, cfg.NR0), np.float32); kval1 = np.zeros((128, cfg.NQ0), np.float32); tval0 = np.zeros((128, cfg.NQ0), np.float32)
    cc = np.zeros((len(core_segs), D), np.float32)
    rpb = inp["l1_rpb_c"]
    b1 = np.empty((1 + 4 * len(core_segs), 28, 128, 512), np.float32); b1[0] = host_bias_l1(rpb, None, 0)
    for s, (xf, crow, start, L) in enumerate(core_segs):
        own = cfg.own[s] * 128
        pos = np.arange(start - H0C * 128, start + own + H0C * 128)
        ok = (pos >= 0) & (pos < L)
        xe = np.zeros((len(pos), D), np.float32); xe[ok] = xf[pos[ok]]
        m[f"xe{s}"] = xe; cc[s] = crow
        kval0[:, cfg.R0off[s]:cfg.R0off[s] + cfg.R0[s]] = np.where(ok, 0.0, NEG).reshape(cfg.R0[s], 128).T
        posq = np.arange(start - HQC * 128, start + own + HQC * 128); okq = (posq >= 0) & (posq < L)
        kval1[:, cfg.Q0off[s]:cfg.Q0off[s] + cfg.Q0[s]] = np.where(okq, 0.0, NEG).reshape(cfg.Q0[s], 128).T
        tval0[:, cfg.Q0off[s]:cfg.Q0off[s] + cfg.Q0[s]] = okq.astype(np.float32).reshape(cfg.Q0[s], 128).T
        nrows = L // 64; r_first = start // 64; nblk = cfg.own[s]
        for j, qc in enumerate([0, 1, nblk - 2, nblk - 1]):
            b1[1 + 4 * s + j] = host_bias_l1(rpb, r_first + 2 * qc, nrows)
    m["c"] = cc; m["kval0"] = kval0; m["kval1"] = kval1; m["tval0"] = tval0
    m["ident"] = np.eye(128, dtype=np.float32)
    m["ustrict"] = np.triu(np.ones((128, 128), np.float32), 1)
    bo = np.zeros((128, 128), np.float32); bo[:64, :64] = 1; bo[64:, 64:] = 1; m["bones"] = bo
    for l in range(2):
        m[f"ecol{l}"] = np.broadcast_to((np.arange(E) * cfg.C[l] + 1).astype(np.float32), (128, E)).copy()
    m["bias0"] = host_bias_l0(); m["bias1"] = b1
    m["sink"] = inp["l0_sink_a"].reshape(1, 8)
    def g2(v): return np.concatenate([v, v]).astype(np.float32)
    m["l0_gains"] = np.stack([g2(inp["l0_q_norm_a"]), g2(inp["l0_k_norm_a"]), g2(inp["l0_q_norm_b"]), g2(inp["l0_k_norm_b"])], 1)
    m["l1_gains"] = np.stack([g2(inp["l1_q_norm_c"]), g2(inp["l1_k_norm_c"]), g2(inp["l1_q_norm_c"]), g2(inp["l1_k_norm_c"])], 1)
    for l in range(2):
        p = f"l{l}_"
        m[p + "ada_w"] = inp[p + "ada_w"]; m[p + "ada_b"] = inp[p + "ada_b"].reshape(1, -1); m[p + "norm1"] = inp[p + "norm1"].reshape(1, -1)
        m[p + "w_in"] = inp[p + "w_in"]; m[p + "w_out"] = inp[p + "w_out"]; m[p + "norm2"] = inp[p + "norm2"].reshape(1, -1)
        m[p + "w_router"] = inp[p + "w_router"]; m[p + "b_router"] = inp[p + "b_router"].reshape(1, -1)
        m[p + "w_gate_up"] = inp[p + "w_gate_up"]; m[p + "b_gate_up"] = inp[p + "b_gate_up"]
        m[p + "w_down"] = inp[p + "w_down"]; m[p + "b_down"] = inp[p + "b_down"]
    return m


def kernel(**inputs):
    inp = {k: np.asarray(v) for k, v in inputs.items()}
    cfg = full_cfg()
    xp = inp["x_prompt"]; xs = inp["x_sample"]; cp = inp["c_prompt"]; cs = inp["c_sample"]
    in_maps = []
    for c in range(8):
        segs = [(xp[0], cp[0], c * 2048, xp.shape[1]), (xs[c // 2], cs[c // 2], (c % 2) * 4096, xs.shape[1])]
        in_maps.append(make_inputs(cfg, segs, None, None, inp))
    nc = build(cfg)
    res = run_bass_kernel_spmd(nc, in_maps, core_ids=list(range(8)))
    yp = np.empty_like(xp); ys = np.empty_like(xs)
    for c in range(8):
        yp[0, c * 2048:(c + 1) * 2048] = res.results[c]["y0"]
        ys[c // 2, (c % 2) * 4096:(c % 2 + 1) * 4096] = res.results[c]["y1"]
    return (yp, ys)
```

```python
import contextlib
import numpy as np
import concourse.bass as bass
import concourse.mybir as mybir
from concourse.bass_utils import run_bass_kernel_spmd

F32 = mybir.dt.float32; BF16 = mybir.dt.bfloat16; I32 = mybir.dt.int32
AF = mybir.ActivationFunctionType; ALU = mybir.AluOpType
D = 1024; NEG = -30000.0; H0C = 10; HQC = 2; TOPK = 4


class _Stop(Exception):
    pass


class _RecInstr:
    def __init__(s, ent): s.ent = ent


class _Rec:
    def __init__(s, log): s._log = log
    def __getattr__(s, name):
        def f(*a, **kw):
            ent = [name, a, kw, []]; s._log.append(ent); return _RecInstr(ent)
        return f


def _stoprange(cfg):
    return range(2)


class Cfg:
    def __init__(s, ncores, own, E, C):
        s.ncores = ncores; s.own = own; s.E = E; s.C = C
        s.R0 = [o + 2 * H0C for o in own]; s.Q0 = [o + 2 * HQC for o in own]
        s.R0off = [sum(s.R0[:i]) for i in range(len(own))]; s.Q0off = [sum(s.Q0[:i]) for i in range(len(own))]
        s.Ooff = [sum(own[:i]) for i in range(len(own))]
        s.NR0 = sum(s.R0); s.NQ0 = sum(s.Q0); s.NO = sum(own)


def full_cfg():
    return Cfg(8, [16, 32], 32, [3072, 3072])


def layer_specs():
    L0 = dict(nqb=10, nkb=7, nvh=14, nko=6)
    L0["qcols"] = [b * 128 for b in range(4)] + [768 + g * 256 + j * 128 for g in range(3) for j in range(2)]
    L0["kcols"] = [512] + [1536 + g * 256 + j * 128 for g in range(3) for j in range(2)]
    L0["vsets"] = [(640, 128, 0), (2304, 512, 2), (2816, 256, 10)]
    L0["qgain"] = [0] * 4 + [2] * 6; L0["kgain"] = [1] + [3] * 6
    L0["wgroups"] = [(0, 6, 1), (6, 10, 2), (10, 14, 8)]
    quads = []
    for j in range(2):
        heads = [(4 * j + i, j) for i in range(4)]
        quads.append(dict(units=[(heads, d, ("A", j, d)) for d in (-1, 0, 1)], sink=True))
    bun = []
    for g, rng in enumerate([range(-1, 2), range(-2, 3), range(-8, 9)]):
        heads = [(8 + 4 * g + h, 2 + 4 * g + h) for h in range(4)]
        bun += [(heads, d, ("B", g, d)) for d in rng]
    quads.append(dict(units=bun, sink=False))
    L0["quads"] = quads
    L1 = dict(nqb=8, nkb=8, nvh=16, nko=8)
    L1["qcols"] = [b * 128 for b in range(8)]; L1["kcols"] = [1024 + b * 128 for b in range(8)]
    L1["vsets"] = [(2048, 512, 0), (2560, 512, 8)]
    L1["qgain"] = [0] * 8; L1["kgain"] = [1] * 8
    L1["wgroups"] = [(0, 16, 3)]
    quads = []
    for qd in range(4):
        heads = [(4 * qd + h, 4 * qd + h) for h in range(4)]
        quads.append(dict(units=[(heads, d, ("C", qd, d)) for d in range(-3, 4)], sink=False))
    L1["quads"] = quads
    return L0, L1


def l0_bias_list():
    L0, _ = layer_specs()
    return [u[2] for q in L0["quads"] for u in q["units"]]


def host_bias_l0():
    sl_a = 2.0 ** (-8.0 * np.arange(1, 9) / 8); sl_b = (2.0 ** (-8.0 * np.arange(1, 13) / 12)).reshape(3, 4)
    dil = [1, 4, 16]
    kp = np.arange(128)[:, None]; qp = np.arange(128)[None, :]
    out = []
    for (kind, j, d) in l0_bias_list():
        rel = d * 128 + kp - qp
        t = np.empty((128, 4, 128), np.float32)
        for h in range(4):
            if kind == "A":
                ok = np.abs(rel) <= 128; s = sl_a[4 * j + h]
            else:
                ok = (np.abs(rel) <= 64 * dil[j]) & (rel % dil[j] == 0); s = sl_b[j, h]
            t[:, h, :] = np.where(ok, -s * np.abs(rel), NEG)
        out.append(t.reshape(128, 512))
    return np.stack(out).astype(np.float32)


def host_bias_l1(rpb, r0, nrows):
    key = np.arange(128); q = np.arange(128)
    krl = (key // 64)[:, None]; kc = (key % 64)[:, None]; qrl = (q // 64)[None, :]; c = (q % 64)[None, :]
    cs = np.clip(c - 8, 0, 48); colok = (kc >= cs) & (kc < cs + 16); cidx = np.clip(kc - c + 15, 0, 30)
    out = np.empty((4, 7, 128, 4, 128), np.float32)
    for di, d in enumerate(range(-3, 4)):
        dr = 2 * d + krl - qrl
        if r0 is None:
            rowok = (dr >= -4) & (dr <= 3)
        else:
            r = r0 + qrl; rs = np.clip(r - 4, 0, nrows - 8); kr = r + dr
            rowok = (kr >= rs) & (kr < rs + 8)
        ridx = np.clip(dr + 7, 0, 14); ok = rowok & colok
        for a in range(16):
            out[a // 4, di, :, a % 4, :] = np.where(ok, rpb[a][ridx, cidx], NEG)
    return out.reshape(28, 128, 512)


def build(cfg):
    nc = bass.Bass("TRN2", target_bir_lowering=False)
    nseg = len(cfg.own); E = cfg.E
    L0, L1 = layer_specs(); LS = [L0, L1]
    NU0 = len(l0_bias_list())

    def din(name, shape, dt=F32): return nc.dram_tensor(name, list(shape), dt, kind="ExternalInput")
    def dsc(name, shape, dt): return nc.dram_tensor(name, list(shape), dt)
    xe = [din(f"xe{s}", [cfg.R0[s] * 128, D]) for s in range(nseg)]
    cin = din("c", [nseg, D])
    kval0 = din("kval0", [128, cfg.NR0]); kval1 = din("kval1", [128, cfg.NQ0]); tval0 = din("tval0", [128, cfg.NQ0])
    ident_in = din("ident", [128, 128]); ustrict_in = din("ustrict", [128, 128]); bones_in = din("bones", [128, 128])
    ecol_in = [din(f"ecol{l}", [128, E]) for l in range(2)]
    bias0_in = din("bias0", [NU0, 128, 512]); bias1_in = din("bias1", [1 + 4 * nseg, 28, 128, 512])
    W = []
    for l in range(2):
        w = dict(ada_w=din(f"l{l}_ada_w", [D, 6 * D]), ada_b=din(f"l{l}_ada_b", [1, 6 * D]), norm1=din(f"l{l}_norm1", [1, D]),
                 w_in=din(f"l{l}_w_in", [D, 3072]), gains=din(f"l{l}_gains", [128, 4]), w_out=din(f"l{l}_w_out", [LS[l]["nko"] * 128, D]),
                 norm2=din(f"l{l}_norm2", [1, D]), w_router=din(f"l{l}_w_router", [D, E]), b_router=din(f"l{l}_b_router", [1, E]),
                 w_gu=din(f"l{l}_w_gate_up", [E, D, 2048]), b_gu=din(f"l{l}_b_gate_up", [E, 2048]),
                 w_d=din(f"l{l}_w_down", [E, 1024, D]), b_d=din(f"l{l}_b_down", [E, D]))
        W.append(w)
    sink_in = din("sink", [1, 8])
    youts = [nc.dram_tensor(f"y{s}", [cfg.own[s] * 128, D], F32, kind="ExternalOutput") for s in range(nseg)]
    cnt_out = nc.dram_tensor("cnt_out", [2, E], F32, kind="ExternalOutput") if getattr(cfg, 'dbg', {}).get('cnt_out') else None
    idx_out = nc.dram_tensor("idx_out", [2, 128, cfg.NQ0 * 4], I32, kind="ExternalOutput") if getattr(cfg, 'dbg', {}).get('idx_out') else None
    modd = dsc("modd", [nseg, 6 * D], F32)
    NRl = [cfg.NR0, cfg.NQ0]; NQl = [cfg.NQ0, cfg.NO]
    qT = [dsc(f"qT{l}", [2 * LS[l]["nqb"], 64, NQl[l] * 128], BF16) for l in range(2)]
    kT = [dsc(f"kT{l}", [2 * LS[l]["nkb"], 64, NRl[l] * 128], BF16) for l in range(2)]
    Vd = [dsc(f"V{l}", [NRl[l] * 128, LS[l]["nvh"] * 65], BF16) for l in range(2)]
    x1d = [dsc(f"x1_{l}", [NQl[l] * 128, D], F32) for l in range(2)]
    x2d = dsc("x2", [cfg.NQ0 * 128, D], F32)
    Xg = [dsc(f"Xg{l}", [E * cfg.C[l], D], BF16) for l in range(2)]
    EH = E // 2
    YgH = [[dsc(f"Yg{l}_{hh}", [EH * cfg.C[l], D], F32) for hh in range(2)] for l in range(2)]
    winb = [dsc(f"winb{l}", [D, 3072], BF16) for l in range(2)]
    ddram = dsc("ddram", [16, 64], F32)
    woutb = [dsc(f"woutb{l}", [LS[l]["nko"] * 128, D], BF16) for l in range(2)]

    es = contextlib.ExitStack()
    nsem = [0]; all_sems = []
    class Sem:
        def __init__(s, name):
            nsem[0] += 1; s.h = es.enter_context(nc.semaphore(f"{name}_{nsem[0]}")); s.n = 0
            all_sems.append(s)
    waited = {}
    def WAIT(eng, *toks):
        for t in toks:
            if t is None: continue
            if isinstance(t, list): WAIT(eng, *t); continue
            sem, v = t; key = (id(eng), id(sem))
            if hasattr(sem, "dq"): sem.dq.closed = True
            if waited.get(key, 0) >= v: continue
            waited[key] = v; eng.wait_ge(sem.h, v)
    def SIG(instr, sem, by=1):
        if isinstance(instr, _RecInstr): instr.ent[3].append((sem, by))
        else: instr.then_inc(sem.h, by)
        sem.n += by; return (sem, sem.n)
    PE, ACT, DVE, POOL, SP = nc.tensor, nc.scalar, nc.vector, nc.gpsimd, nc.sync
    sPE = Sem("pe"); sACT = Sem("act"); sDVE = Sem("dve"); sPOOL = Sem("pool")
    def pe(i): return SIG(i, sPE)
    def act(i): return SIG(i, sACT)
    def dve(i): return SIG(i, sDVE)
    def pool(i): return SIG(i, sPOOL)
    class DQ:
        def __init__(s, name): s.sem = Sem(name); s.sem.dq = s; s.closed = False
        def go(s, eng, meth, **kw):
            if s.closed:
                WAIT(eng, (s.sem, s.sem.n)); s.closed = False
            return SIG(getattr(eng, meth)(**kw), s.sem, 16)

    bufs = contextlib.ExitStack()
    uid = [0]
    def uname(name):
        uid[0] += 1; return f"{name}_u{uid[0]}"
    def sb(name, shape, dt): return bufs.enter_context(nc.sbuf_tensor(uname(name), list(shape), dt))
    def ps(name, shape, dt): return bufs.enter_context(nc.psum_tensor(uname(name), list(shape), dt))

    with es, contextlib.suppress(_Stop):
        pst = contextlib.ExitStack()
        def psb(name, shape, dt): return pst.enter_context(nc.sbuf_tensor(uname(name), list(shape), dt))
        ident = psb("ident", [128, 128], F32); identb = psb("identb", [128, 128], BF16)
        ustrict = psb("ustrict", [128, 128], BF16); onesb = psb("onesb", [128, 128], BF16); bones = psb("bonesb", [128, 128], BF16)
        ones1 = psb("ones1", [1, 128], F32); ctmp = psb("ctmp", [128, 128], F32)
        cnt_i = psb("cnti", [1, E], I32); S4 = psb("S4", [128, cfg.NQ0, 4], I32); S4b = psb("S4b", [128, cfg.NQ0, 4], I32); G4 = psb("G4", [128, cfg.NQ0, 4], F32); G4B = psb("G4B", [128, cfg.NQ0, 4], F32)
        dq0 = DQ("c0")
        t = dq0.go(SP, "dma_start", out=ident[:], in_=ident_in[:, :])
        WAIT(DVE, t); dve(DVE.tensor_copy(out=identb[:], in_=ident[:]))
        for src, dst in ((ustrict_in, ustrict), (bones_in, bones)):
            WAIT(SP, (sDVE, sDVE.n)); t = dq0.go(SP, "dma_start", out=ctmp[:], in_=src[:, :]); WAIT(DVE, t)
            dve(DVE.tensor_copy(out=dst[:], in_=ctmp[:]))
        DVE.memset(onesb[:], 1.0); tk_const = dve(DVE.memset(ones1[:], 1.0))
        for e_ in (PE, ACT, DVE, POOL, SP): WAIT(e_, tk_const)

        ENGS = (("PE", PE), ("ACT", ACT), ("DVE", DVE), ("POOL", POOL), ("SP", SP))
        cregs = {nm: en.alloc_register(f"cntreg_{nm}") for nm, en in ENGS}
        keep_alive = []
        dsb = psb("dsb", [1, 16 * 16], F32); dzero = psb("dzero", [16, 64], F32); dids = {}
        tdz = dve(DVE.memset(dzero[:], 0.0)); WAIT(POOL, tdz)
        dq_dz = DQ("dz"); tdz = dq_dz.go(POOL, "dma_start", out=ddram[:, :], in_=dzero[:])
        for e_ in (POOL, SP): WAIT(e_, tdz)
        def guarded(thr, cnt_word, body, zero_src=None):
            logs = {nm: [] for nm, _ in ENGS}; prox = {nm: _Rec(logs[nm]) for nm, _ in ENGS}; keep_alive.append(prox)
            ret = body(prox["PE"], prox["ACT"], prox["DVE"], prox["POOL"], prox["SP"])
            for nm, en in ENGS:
                tot = {}
                for ent in logs[nm]:
                    for (sm, by) in ent[3]: tot[id(sm)] = (sm, tot.get(id(sm), (sm, 0))[1] + by)
                if not logs[nm]: continue
                en.reg_load(cregs[nm], cnt_word)
                with en.If_lt(cregs[nm], thr):
                    for sm, t_ in tot.values():
                        en.wait_ge(sm.h, sm.n - t_)
                        if hasattr(sm, "dq") and nm == "POOL" and zero_src is not None:
                            for (meth, a, kw, incs) in logs[nm]:
                                if meth == "dma_start" and any(s_ is sm for (s_, _) in incs):
                                    dd = en.dma_start(out=kw["out"], in_=zero_src)
                                    for (s_, by) in incs: dd.then_inc(s_.h, by)
                        elif hasattr(sm, "dq"):
                            did = dids.setdefault(id(sm), len(dids)); assert did < 16 and t_ % 16 == 0 and t_ // 16 <= 8
                            for k_ in range(t_ // 16):
                                if nm == "SP": dd = en.dma_start(out=dsb[0:1, did * 16 + 2 * k_:did * 16 + 2 * k_ + 2], in_=ddram[did:did + 1, 2 * k_:2 * k_ + 2])
                                else: dd = en.dma_start(out=ddram[did:did + 1, 32 + 2 * k_:32 + 2 * k_ + 2], in_=dzero[0:1, 0:2])
                                dd.then_inc(sm.h, 16)
                        else:
                            en.sem_inc(sm.h, t_)
                with en.Else():
                    for (meth, a, kw, incs) in logs[nm]:
                        ins = getattr(en, meth)(*a, **kw)
                        for (sm, by) in incs: ins.then_inc(sm.h, by)
            return ret

        def barrier():
            toks = [dve(DVE.memset(ctmp[0:1, 0:1], 0.0)), act(ACT.copy(out=ctmp[0:1, 1:2], in_=ones1[0:1, 0:1])),
                    pool(POOL.memset(ctmp[0:1, 2:3], 0.0)), (sPE, sPE.n)]
            for e in (PE, ACT, DVE, POOL, SP): WAIT(e, *toks)

        dqc = DQ("cast"); cast_t = None
        for l in range(2):
            for r in range(0, D, 128):
                cast_t = dqc.go(POOL, "dma_start", out=winb[l][r:r + 128, :], in_=W[l]["w_in"][r:r + 128, :], max_dma_last_dim=4096)
            for r in range(0, LS[l]["nko"] * 128, 128):
                cast_t = dqc.go(POOL, "dma_start", out=woutb[l][r:r + 128, :], in_=W[l]["w_out"][r:r + 128, :], max_dma_last_dim=4096)

        for l in (range(2) if not getattr(cfg, 'stop', None) else _stoprange(cfg)):
            Ls = LS[l]; Wl = W[l]; C = cfg.C[l]
            Rc = cfg.R0 if l == 0 else cfg.Q0; Roff = cfg.R0off if l == 0 else cfg.Q0off
            Qc = cfg.Q0 if l == 0 else cfg.own; Qoff = cfg.Q0off if l == 0 else cfg.Ooff
            q2r = 8 if l == 0 else 2
            xsrc = [xe[s] for s in range(nseg)] if l == 0 else [x2d[cfg.Q0off[s] * 128:(cfg.Q0off[s] + cfg.Q0[s]) * 128, :] for s in range(nseg)]
            kvald = kval0 if l == 0 else kval1

            with contextlib.ExitStack() as bufs_:
                bufs = bufs_
                cT = sb("cT", [128, nseg, 8], F32); scT = sb("scT", [128, 8, 128], F32)
                aw = [sb(f"aw{i}", [128, 8, 512], F32) for i in range(2)]; brow = sb("brow", [1, 6 * D], F32)
                mrow = sb("mrow", [128, 6 * D], F32); pm = [ps(f"pm{i}", [128, 512], F32) for i in range(2)]
                dqa = [DQ("aw0"), DQ("aw1")]; dql = DQ("la")
                for s in range(nseg):
                    t = dql.go(SP, "dma_start", out=cT[:, s, :], in_=cin[s:s + 1, :].rearrange("o (k p) -> p (o k)", p=128), allow_slow_non_contiguous=True)
                t = dql.go(SP, "dma_start", out=brow[:], in_=Wl["ada_b"][:, :])
                WAIT(ACT, t); ta = act(ACT.activation(out=cT[:], in_=cT[:], func=AF.Silu))
                WAIT(DVE, ta, t); half = 128 // nseg
                for s in range(nseg):
                    tsc = dve(DVE.tensor_copy(out=scT[:, :, s * half:(s + 1) * half], in_=cT[:, s, :].unsqueeze(2).to_broadcast([128, 8, half])))
                awfree = [None, None]; pmfree = [None, None]
                for j in range(12):
                    i = j % 2
                    WAIT(SP, awfree[i]); tl = dqa[i].go(SP, "dma_start", out=aw[i][:], in_=Wl["ada_w"][:, j * 512:(j + 1) * 512].rearrange("(k p) n -> p k n", p=128))
                    WAIT(PE, tl, tsc, pmfree[i], tk_const)
                    for k in range(8):
                        PE.matmul(pm[i][:], lhsT=scT[:, k, :], rhs=aw[i][:, k, :], start=(k == 0), stop=False)
                    tp = pe(PE.matmul(pm[i][:], lhsT=ones1[:].to_broadcast([1, 128]) if False else ones1[:], rhs=brow[:, j * 512:(j + 1) * 512], start=False, stop=True))
                    awfree[i] = tp
                    WAIT(DVE, tp); pmfree[i] = dve(DVE.tensor_copy(out=mrow[:, j * 512:(j + 1) * 512], in_=pm[i][:]))
                WAIT(POOL, pmfree[0], pmfree[1]); dqs = DQ("ms")
                for s in range(nseg):
                    t = dqs.go(POOL, "dma_start", out=modd[s:s + 1, :], in_=mrow[s * half:s * half + 1, :])
                WAIT(POOL, t)
            barrier()
            if getattr(cfg, 'stop', None) == (l, 'A'): raise _Stop()

            with contextlib.ExitStack() as bufs_:
                bufs = bufs_
                wsb = sb("wsb", [128, 8, 3072], BF16); gains = sb("gains", [128, 4], F32); g1b = sb("g1b", [128, D], F32)
                Ab = sb("Ab", [128, D], F32); Bb = sb("Bb", [128, D], F32)
                xt = [sb(f"xt{i}", [128, D], F32) for i in range(2)]; junk = sb("junk", [128, D], BF16)
                st = [sb(f"st{i}", [128, 4], F32) for i in range(2)]
                hb = [sb(f"hb{i}", [128, D], BF16) for i in range(2)]; hT = [sb(f"hT{i}", [128, 8, 512], BF16) for i in range(2)]
                sq = [sb(f"sq{i}", [128, 512], BF16) for i in range(2)]; rs = [sb(f"rs{i}", [128, 512], F32) for i in range(2)]
                qk = [sb(f"qk{i}", [128, 512], BF16) for i in range(2)]; vx = [sb(f"vx{i}", [128, Ls["nvh"], 65], BF16) for i in range(2)]
                ptr = ps("ptr", [128, D], BF16); pq = [ps(f"pq{i}", [128, 512], F32) for i in range(2)]
                p2 = [ps(f"p2{i}", [128, 512], F32) for i in range(2)]; pv = [ps(f"pv{i}", [128, 512], F32) for i in range(2)]
                dqw = DQ("w"); dqx = [DQ("x0"), DQ("x1")]; dqm = DQ("m"); dqo = [DQ("qk0"), DQ("qk1")]; dqv = [DQ("v0"), DQ("v1")]
                WAIT(POOL, cast_t)
                tw = dqw.go(POOL, "dma_start", out=wsb[:], in_=winb[l].ap().rearrange("(k p) n -> p k n", p=128))
                dqg_ = DQ("gn"); tg = dqg_.go(SP, "dma_start", out=gains[:], in_=Wl["gains"][:, :])
                WAIT(DVE, tg)
                DVE.tensor_scalar(out=gains[:, 0:1], in0=gains[:, 0:1], scalar1=0.125, scalar2=None, op0=ALU.mult)
                tgain = dve(DVE.tensor_scalar(out=gains[:, 2:3], in0=gains[:, 2:3], scalar1=0.125, scalar2=None, op0=ALU.mult))
                for i in range(2):
                    tvx = dve(DVE.memset(vx[i][:], 1.0))
                xfree = [None, None]; hbfree = [None, None]; hTfree = [None, None]; ptrfree = None
                pqfree = [None, None]; p2free = [None, None]; pvfree = [None, None]; sqfree = [None, None]; rsfree = [None, None]
                qkfree = [None, None]; vxfree = [None, None]; stfree = [None, None]
                ti = 0; gi = 0; bi = 0; vi = 0
                for s in range(nseg):
                    WAIT(SP, (sDVE, sDVE.n), (sPE, sPE.n))
                    t1 = dqm.go(SP, "dma_start", out=g1b[:], in_=Wl["norm1"][0:1, :].to_broadcast([128, D]))
                    t1 = dqm.go(SP, "dma_start", out=Ab[:], in_=modd[s:s + 1, D:2 * D].to_broadcast([128, D]))
                    t1 = dqm.go(SP, "dma_start", out=Bb[:], in_=modd[s:s + 1, 0:D].to_broadcast([128, D]))
                    WAIT(DVE, t1)
                    tAB = dve(DVE.scalar_tensor_tensor(out=Ab[:], in0=Ab[:], scalar=1.0, in1=g1b[:], op0=ALU.add, op1=ALU.mult))
                    ngroups = (Rc[s] + 3) // 4
                    for g in range(ngroups):
                        tiles = list(range(g * 4, min(g * 4 + 4, Rc[s]))); ntok = len(tiles) * 128; gs = gi % 2; gi += 1
                        thT = []
                        for jt, tc in enumerate(tiles):
                            i = ti % 2; ti += 1
                            WAIT(SP, xfree[i]); tx = dqx[i].go(SP, "dma_start", out=xt[i][:], in_=xsrc[s][tc * 128:(tc + 1) * 128, :])
                            WAIT(DVE, tx, stfree[i], tAB)
                            ta_ = dve(DVE.scalar_tensor_tensor(out=junk[:], in0=xt[i][:], scalar=1.0, in1=xt[i][:], op0=ALU.mult, op1=ALU.mult, accum_out=st[i][:, 0:1]))
                            WAIT(DVE, ta_); tb_ = dve(DVE.tensor_scalar(out=st[i][:, 1:2], in0=st[i][:, 0:1], scalar1=1.0 / D, scalar2=1e-6, op0=ALU.mult, op1=ALU.add))
                            WAIT(ACT, tb_); tc_ = act(ACT.activation(out=st[i][:, 2:3], in_=st[i][:, 1:2], func=AF.Sqrt))
                            WAIT(DVE, tc_); td_ = dve(DVE.reciprocal(out=st[i][:, 3:4], in_=st[i][:, 2:3]))
                            WAIT(DVE, td_); te_ = dve(DVE.scalar_tensor_tensor(out=xt[i][:], in0=xt[i][:], scalar=st[i][:, 3:4], in1=Ab[:], op0=ALU.mult, op1=ALU.mult))
                            WAIT(DVE, te_, hbfree[i]); th = dve(DVE.tensor_tensor(out=hb[i][:], in0=xt[i][:], in1=Bb[:], op=ALU.add))
                            xfree[i] = th; stfree[i] = th
                            WAIT(PE, th, ptrfree)
                            for k in range(8):
                                tt = PE.transpose(ptr[:, k * 128:(k + 1) * 128], hb[i][:, k * 128:(k + 1) * 128], identb[:])
                            tt = pe(tt); hbfree[i] = tt
                            WAIT(DVE, tt, hTfree[gs] if jt == 0 else None)
                            ptrfree = dve(DVE.tensor_copy(out=hT[gs][:, :, jt * 128:(jt + 1) * 128], in_=ptr[:].rearrange("p (k n) -> p k n", k=8)))
                            thT.append(ptrfree)
                        qlo = q2r * 128; qhi = (q2r + Qc[s]) * 128; glo = g * 512; ghi = glo + ntok
                        olo = max(glo, qlo); ohi = min(ghi, qhi)
                        blocks = [("k", b) for b in range(Ls["nkb"])] + ([("q", b) for b in range(Ls["nqb"])] if ohi > olo else [])
                        lastmm = None
                        for (kind, b) in blocks:
                            i = bi % 2; bi += 1
                            if kind == "k":
                                c0 = Ls["kcols"][b]; gcol = Ls["kgain"][b]
                            else:
                                c0 = Ls["qcols"][b]; gcol = Ls["qgain"][b]
                            lhs = lambda k, c0=c0: wsb[:, k, c0:c0 + 128]
                            WAIT(PE, thT, tw, pqfree[i])
                            for k in range(8):
                                mm = PE.matmul(pq[i][:, 0:ntok], lhsT=lhs(k), rhs=hT[gs][:, k, 0:ntok], start=(k == 0), stop=(k == 7))
                            tmm = pe(mm); lastmm = tmm
                            WAIT(ACT, tmm, sqfree[i]); tsq = act(ACT.activation(out=sq[i][:, 0:ntok], in_=pq[i][:, 0:ntok], func=AF.Square))
                            WAIT(PE, tsq, p2free[i]); tss = pe(PE.matmul(p2[i][:, 0:ntok], lhsT=bones[:], rhs=sq[i][:, 0:ntok], start=True, stop=True))
                            sqfree[i] = tss
                            WAIT(ACT, tss, rsfree[i]); tsr = act(ACT.activation(out=rs[i][:, 0:ntok], in_=p2[i][:, 0:ntok], func=AF.Sqrt, bias=1e-6, scale=1.0 / 64))
                            p2free[i] = tsr
                            WAIT(DVE, tsr); trc = dve(DVE.reciprocal(out=rs[i][:, 0:ntok], in_=rs[i][:, 0:ntok]))
                            WAIT(DVE, trc, qkfree[i], tgain)
                            tqk = dve(DVE.scalar_tensor_tensor(out=qk[i][:, 0:ntok], in0=pq[i][:, 0:ntok], scalar=gains[:, gcol:gcol + 1], in1=rs[i][:, 0:ntok], op0=ALU.mult, op1=ALU.mult))
                            pqfree[i] = tqk; rsfree[i] = tqk
                            WAIT(POOL, tqk)
                            for hf in range(2):
                                if kind == "k":
                                    qkfree[i] = dqo[i].go(POOL, "dma_start", out=kT[l][2 * b + hf, :, (Roff[s] * 128 + glo):(Roff[s] * 128 + ghi)], in_=qk[i][hf * 64:hf * 64 + 64, 0:ntok])
                                else:
                                    qkfree[i] = dqo[i].go(POOL, "dma_start", out=qT[l][2 * b + hf, :, (Qoff[s] * 128 + olo - qlo):(Qoff[s] * 128 + ohi - qlo)], in_=qk[i][hf * 64:hf * 64 + 64, olo - glo:ohi - glo])
                        for jt, tc in enumerate(tiles):
                            i = vi % 2; vi += 1
                            WAIT(DVE, vxfree[i], tvx)
                            for (c0, ncol, vh0) in Ls["vsets"]:
                                WAIT(PE, thT, tw, pvfree[i])
                                for k in range(8):
                                    mm = PE.matmul(pv[i][:, 0:ncol], lhsT=hT[gs][:, k, jt * 128:(jt + 1) * 128], rhs=wsb[:, k, c0:c0 + ncol], start=(k == 0), stop=(k == 7))
                                tmm = pe(mm); lastmm = tmm
                                WAIT(DVE, tmm)
                                pvfree[i] = dve(DVE.tensor_copy(out=vx[i][:, vh0:vh0 + ncol // 64, 0:64], in_=pv[i][:, 0:ncol].rearrange("p (h e) -> p h e", e=64)))
                            WAIT(POOL, pvfree[i])
                            r0 = (Roff[s] + tc) * 128
                            vxfree[i] = dqv[i].go(POOL, "dma_start", out=Vd[l][r0:r0 + 128, :], in_=vx[i][:].rearrange("p h e -> p (h e)"))
                        hTfree[gs] = lastmm
                WAIT(POOL, qkfree, vxfree)
            barrier()
            if getattr(cfg, 'stop', None) == (l, 'B'): raise _Stop()

            with contextlib.ExitStack() as bufs_:
                bufs = bufs_
                quads = Ls["quads"]; nquad = len(quads); nko = Ls["nko"]
                if l == 0:
                    bl = l0_bias_list(); bidx = {k: i for i, k in enumerate(bl)}; nbias = NU0
                else:
                    bidx = {("C", qd, d): qd * 7 + d + 3 for qd in range(4) for d in range(-3, 4)}; nbias = 28
                biasr = sb("biasr", [128, nbias, 512], BF16)
                biase = [sb(f"biase{i}", [128, 28, 512], BF16) for i in range(1)] if l == 1 else None; biasefree = None
                wo = sb("wo", [128, nko, D], BF16); kv = sb("kv", [128, NRl[l]], F32); gate1 = sb("gate1", [128, D], F32)
                esink = sb("esink", [128, 8], F32)
                dmin = min(u[1] for q in quads for u in q["units"]); dmax = max(u[1] for q in quads for u in q["units"]); nwin = dmax - dmin + 1
                nqh = 2 * Ls["nqb"]; wgr = Ls["wgroups"]
                qs = [sb(f"qs{i}", [64, nqh, 128], BF16) for i in range(2)]
                ks = [[sb(f"ks{i}_{g}", [64, h1 - h0, (2 * r + 1) * 128], BF16) for (h0, h1, r) in wgr] for i in range(2)]
                vs = [[sb(f"vs{i}_{g}", [128, 2 * r + 1, (h1 - h0) * 65], BF16) for (h0, h1, r) in wgr] for i in range(2)]
                def wg_of(kh):
                    for g, (h0, h1, r) in enumerate(wgr):
                        if h0 <= kh < h1: return g, kh - h0, r
                    raise KeyError(kh)
                xq = [sb(f"xq{i}", [128, D], F32) for i in range(2)]
                Eb = [sb(f"E{i}", [128, 512], BF16) for i in range(3)]
                attn = sb("attn", [128, nko * 128], BF16); attnT = sb("attnT", [128, nko, 128], BF16); rden = sb("rden", [128, 16], F32)
                x1t = [sb(f"x1t{i}", [128, D], F32) for i in range(2)]
                pS = [ps(f"pS{i}", [128, 512], F32) for i in range(2)]; pO_ = [ps(f"pO{i}", [128, 512], F32) for i in range(nquad)]; pO = [p_[:, 0:260].rearrange("p (h e) -> p h e", e=65) for p_ in pO_]
                pT = ps("pT", [128, D], BF16); pW = ps("pW", [128, 512], F32)
                dqb = DQ("b"); dqk = [DQ("k0"), DQ("k1")]; dqe = [DQ("e0"), DQ("e1")]; dqx1 = [DQ("x10"), DQ("x11")]
                WAIT(POOL, cast_t)
                t = dqb.go(POOL, "dma_start", out=wo[:], in_=woutb[l].ap().rearrange("(k p) n -> p k n", p=128))
                bsrc = bias0_in if l == 0 else bias1_in[0]
                for u in range(nbias):
                    t = dqb.go(POOL, "dma_start", out=biasr[:, u, :], in_=bsrc[u, :, :])
                dqb2 = DQ("b2"); dqg1 = DQ("g1")
                t2 = dqb2.go(SP, "dma_start", out=kv[:], in_=kvald[:, :])
                t2 = dqb2.go(SP, "dma_start", out=esink[:], in_=sink_in[0:1, :].to_broadcast([128, 8]))
                tres = [t, t2]
                WAIT(ACT, t2); tsink = act(ACT.activation(out=esink[:], in_=esink[:], func=AF.Exp))
                slotfree = [None, None]; xqfree = [None, None]; Sfree = [None, None]; Efree = [None, None, None]
                Ofree = [None] * nquad; pTfree = None; attnfree = None; attnTfree = None; pWfree = None; x1free = [None, None]; rdenfree = None
                qi = 0; ui = 0; ei = 0
                for s in range(nseg):
                    WAIT(SP, (sDVE, sDVE.n)); tg1 = dqg1.go(SP, "dma_start", out=gate1[:], in_=modd[s:s + 1, 2 * D:3 * D].to_broadcast([128, D]))
                    for qc in range(min(Qc[s], getattr(cfg, 'dbg', {}).get('c_blocks', 10**9))):
                        i = qi % 2; qi += 1
                        rc = qc + q2r
                        edge = None
                        if l == 1:
                            if qc < 2: edge = 1 + 4 * s + qc
                            elif qc >= Qc[s] - 2: edge = 1 + 4 * s + 2 + (qc - (Qc[s] - 2))
                        WAIT(SP, slotfree[i], xqfree[i])
                        qcol = (Qoff[s] + qc) * 128
                        dqk[i].go(SP, "dma_start", out=qs[i][:], in_=qT[l][:, :, qcol:qcol + 128].rearrange("b p t -> p b t"))
                        los = []
                        for g, (h0, h1, r) in enumerate(wgr):
                            lo = max(0, rc - r); hi = min(Rc[s] - 1, rc + r); nld = hi - lo + 1; los.append(lo)
                            kcol = (Roff[s] + lo) * 128
                            dqk[i].go(SP, "dma_start", out=ks[i][g][:, :, 0:nld * 128], in_=kT[l][h0:h1, :, kcol:kcol + nld * 128].rearrange("b p t -> p b t"))
                            dqk[i].go(SP, "dma_start", out=vs[i][g][:, 0:nld, :], in_=Vd[l][kcol:kcol + nld * 128, h0 * 65:h1 * 65].rearrange("(c p) f -> p c f", p=128))
                        xrow = (rc * 128) if l == 0 else ((cfg.Q0off[s] + rc) * 128)
                        xs_ = xe[s] if l == 0 else x2d
                        tld = dqk[i].go(SP, "dma_start", out=xq[i][:], in_=xs_[xrow:xrow + 128, :])
                        if edge is not None:
                            eb = 0
                            WAIT(POOL, slotfree[i], biasefree)
                            for u in range(28):
                                tedge = dqe[eb].go(POOL, "dma_start", out=biase[eb][:, u, :], in_=bias1_in[edge, u, :, :])
                        stage = getattr(cfg, 'dbg', {}).get('c_stage', 9)
                        if stage < 1: continue
                        for qd, quad in enumerate(quads):
                            units = quad["units"]
                            if l == 1:
                                units = [u for u in units if (edge is not None) or (-2 <= u[1] <= 2)]
                            def emit_pv(pv):
                                un_, e3_, cidx_, wg_, heads_, tE_ = pv
                                WAIT(PE, tE_, Ofree[qd] if un_ == 0 else None)
                                for h, (qh, kh) in enumerate(heads_):
                                    _, khl, _ = wg_of(kh)
                                    mm_ = PE.matmul(pO[qd][:, h, :], lhsT=Eb[e3_][:, h * 128:(h + 1) * 128], rhs=vs[i][wg_][:, cidx_, khl * 65:(khl + 1) * 65],
                                                    start=(un_ == 0 and h == 0), stop=(un_ == len(units) - 1), skip_group_check=True)
                                Efree[e3_] = pe(mm_); return Efree[e3_]
                            prev = None; tlastpv = None
                            for un, (heads, d, bkey) in enumerate(units):
                                si = ui % 2; e3 = ui % 3; ui += 1
                                wg, _, _ = wg_of(heads[0][1]); lo = los[wg]
                                cidx = min(max(rc + d, 0), Rc[s] - 1) - lo
                                btile = biasr[:, bidx[bkey], :] if edge is None else biase[eb][:, bidx[bkey], :]
                                WAIT(PE, tld, tres, Sfree[si], tedge if edge is not None else None)
                                PE.matmul(pS[si][:], lhsT=identb[:], rhs=btile, start=True, stop=False)
                                for h, (qh, kh) in enumerate(heads):
                                    _, khl, _ = wg_of(kh)
                                    mm = PE.matmul(pS[si][:, h * 128:(h + 1) * 128], lhsT=ks[i][wg][:, khl, cidx * 128:(cidx + 1) * 128],
                                                   rhs=qs[i][:, qh, :], start=False, stop=(h == 3))
                                tS = pe(mm)
                                if edge is not None: biasefree = tS
                                if stage < 2: continue
                                kcolv = Roff[s] + lo + cidx
                                WAIT(ACT, tS, Efree[e3])
                                tE = act(ACT.activation(out=Eb[e3][:], in_=pS[si][:], func=AF.Exp, bias=kv[:, kcolv:kcolv + 1], scale=1.0))
                                Sfree[si] = tE
                                if prev is not None: tlastpv = emit_pv(prev)
                                prev = (un, e3, cidx, wg, heads, tE)
                            if prev is not None: tlastpv = emit_pv(prev)
                            if stage < 4: continue
                            tO = tlastpv
                            WAIT(DVE, tO, rdenfree, tsink)
                            if quad["sink"]:
                                td1 = dve(DVE.tensor_tensor(out=rden[:, qd * 4:qd * 4 + 4], in0=pO[qd][:, :, 64], in1=esink[:, qd * 4:qd * 4 + 4], op=ALU.add))
                            else:
                                td1 = dve(DVE.tensor_copy(out=rden[:, qd * 4:qd * 4 + 4], in_=pO[qd][:, :, 64]))
                            WAIT(DVE, td1); td2 = dve(DVE.reciprocal(out=rden[:, qd * 4:qd * 4 + 4], in_=rden[:, qd * 4:qd * 4 + 4]))
                            WAIT(DVE, td2, attnfree if qd == 0 else None)
                            tat = dve(DVE.tensor_tensor(out=attn[:, qd * 256:(qd + 1) * 256].rearrange("p (h e) -> p h e", e=64), in0=pO[qd][:, :, 0:64],
                                                        in1=rden[:, qd * 4:qd * 4 + 4].unsqueeze(2).to_broadcast([128, 4, 64]), op=ALU.mult))
                            Ofree[qd] = tat
                        if stage < 5: continue
                        slotfree[i] = tlastpv; rdenfree = tat
                        WAIT(PE, tat, pTfree)
                        for k in range(nko):
                            tt = PE.transpose(pT[:, k * 128:(k + 1) * 128], attn[:, k * 128:(k + 1) * 128], identb[:])
                        tt = pe(tt); attnfree = tt
                        WAIT(DVE, tt, attnTfree)
                        tcp = dve(DVE.tensor_copy(out=attnT[:], in_=pT[:, 0:nko * 128].rearrange("p (k n) -> p k n", k=nko))); pTfree = tcp
                        xi = qi % 2
                        WAIT(DVE, x1free[xi], tg1)
                        for hh in range(2):
                            WAIT(PE, tcp, pWfree)
                            for k in range(nko):
                                mm = PE.matmul(pW[:], lhsT=attnT[:, k, :], rhs=wo[:, k, hh * 512:(hh + 1) * 512], start=(k == 0), stop=(k == nko - 1))
                            tw_ = pe(mm)
                            WAIT(DVE, tw_)
                            tm1 = dve(DVE.tensor_tensor(out=x1t[xi][:, hh * 512:(hh + 1) * 512], in0=pW[:], in1=gate1[:, hh * 512:(hh + 1) * 512], op=ALU.mult)); pWfree = tm1
                            WAIT(DVE, tm1)
                            tx1 = dve(DVE.tensor_tensor(out=x1t[xi][:, hh * 512:(hh + 1) * 512], in0=x1t[xi][:, hh * 512:(hh + 1) * 512], in1=xq[i][:, hh * 512:(hh + 1) * 512], op=ALU.add))
                        attnTfree = tw_; xqfree[i] = tx1
                        WAIT(POOL, tx1)
                        r0 = (Qoff[s] + qc) * 128
                        x1free[xi] = dqx1[xi].go(POOL, "dma_start", out=x1d[l][r0:r0 + 128, :], in_=x1t[xi][:])
                WAIT(POOL, x1free)
            barrier()
            if getattr(cfg, 'stop', None) == (l, 'C'): raise _Stop()

            NT = NQl[l]; bc_reg = POOL.to_reg(E * C - 1); bch_reg = POOL.to_reg(EH * C - 1)
            with contextlib.ExitStack() as bufs_:
                bufs = bufs_
                g2b = sb("g2b", [128, D], F32); A2 = sb("A2", [128, D], F32); B2 = sb("B2", [128, D], F32)
                wr = sb("wr", [128, 8, E], F32); br = sb("br", [1, E], F32); ecol = sb("ecol", [128, E], F32); tv = sb("tv", [128, cfg.NQ0], F32)
                cnt = sb("cnt", [128, E], F32)
                xt = [sb(f"dxt{i}", [128, D], F32) for i in range(2)]; junk = sb("djunk", [128, D], BF16); st = [sb(f"dst{i}", [128, 4], F32) for i in range(2)]
                h2 = [sb(f"h2{i}", [128, D], F32) for i in range(2)]; h2b = [sb(f"h2b{i}", [128, D], BF16) for i in range(2)]
                h2T = sb("h2T", [128, 8, 128], F32)
                sm = [sb(f"sm{i}", [128, 8, E], F32) for i in range(2)]
                m8 = [sb(f"m8{i}", [128, 4, 8], F32) for i in range(2)]
                Mb = [sb(f"Mb{i}", [128, E], BF16) for i in range(2)]
                zt = sb("zt", [128, D], BF16)
                pTa = ps("dpTa", [128, 512], F32); pTb = ps("dpTb", [128, 512], F32); pL_ = ps("pL", [128, 512], F32); pL = pL_[:, 0:E]; pR_ = ps("pR", [128, 512], F32); pR = pR_[:, 0:2 * E].rearrange("p (a e) -> p a e", a=2)
                dql = DQ("dl"); dql2 = DQ("dl2"); dqx = [DQ("dx0"), DQ("dx1")]; dqsc = [DQ("sc0"), DQ("sc1")]; dqz = DQ("z")
                t = dql.go(SP, "dma_start", out=wr[:], in_=Wl["w_router"].ap().rearrange("(k p) e -> p k e", p=128))
                t = dql.go(SP, "dma_start", out=br[:], in_=Wl["b_router"][:, :])
                t = dql.go(SP, "dma_start", out=ecol[:], in_=ecol_in[l][:, :])
                if l == 0: t = dql.go(SP, "dma_start", out=tv[:], in_=tval0[:, :])
                tld0 = t
                tz = dve(DVE.memset(zt[:], 0.0)); tcnt = dve(DVE.memset(cnt[:], 0.0))
                if l == 1: tcnt = dve(DVE.memset(tv[:], 1.0))
                WAIT(POOL, tz)
                for r in range(0, E * C, 128):
                    tzero = dqz.go(POOL, "dma_start", out=Xg[l][r:r + 128, :], in_=zt[:])
                WAIT(POOL, tzero)
                xfree = [None, None]; stfree = [None, None]; h2free = [None, None]; h2bfree = [None, None]; smfree = [None, None]
                pTfree = None; h2Tfree = None; pLfree = None; pRfree = None; cntT = tcnt
                ti = 0
                for s in range(nseg):
                    WAIT(SP, (sDVE, sDVE.n))
                    t1 = dql2.go(SP, "dma_start", out=g2b[:], in_=Wl["norm2"][0:1, :].to_broadcast([128, D]))
                    t1 = dql2.go(SP, "dma_start", out=A2[:], in_=modd[s:s + 1, 4 * D:5 * D].to_broadcast([128, D]))
                    t1 = dql2.go(SP, "dma_start", out=B2[:], in_=modd[s:s + 1, 3 * D:4 * D].to_broadcast([128, D]))
                    WAIT(DVE, t1); tAB = dve(DVE.scalar_tensor_tensor(out=A2[:], in0=A2[:], scalar=1.0, in1=g2b[:], op0=ALU.add, op1=ALU.mult))
                    for qc in range(Qc[s]):
                        i = ti % 2; ti += 1; tg = Qoff[s] + qc
                        WAIT(SP, xfree[i]); tx = dqx[i].go(SP, "dma_start", out=xt[i][:], in_=x1d[l][tg * 128:(tg + 1) * 128, :])
                        WAIT(DVE, tx, stfree[i], tAB)
                        ta_ = dve(DVE.scalar_tensor_tensor(out=junk[:], in0=xt[i][:], scalar=1.0, in1=xt[i][:], op0=ALU.mult, op1=ALU.mult, accum_out=st[i][:, 0:1]))
                        WAIT(DVE, ta_); tb_ = dve(DVE.tensor_scalar(out=st[i][:, 1:2], in0=st[i][:, 0:1], scalar1=1.0 / D, scalar2=1e-6, op0=ALU.mult, op1=ALU.add))
                        WAIT(ACT, tb_); tc_ = act(ACT.activation(out=st[i][:, 2:3], in_=st[i][:, 1:2], func=AF.Sqrt))
                        WAIT(DVE, tc_); td_ = dve(DVE.reciprocal(out=st[i][:, 3:4], in_=st[i][:, 2:3]))
                        WAIT(DVE, td_); te_ = dve(DVE.scalar_tensor_tensor(out=xt[i][:], in0=xt[i][:], scalar=st[i][:, 3:4], in1=A2[:], op0=ALU.mult, op1=ALU.mult))
                        WAIT(DVE, te_, h2free[i]); th = dve(DVE.tensor_tensor(out=h2[i][:], in0=xt[i][:], in1=B2[:], op=ALU.add))
                        xfree[i] = th; stfree[i] = th
                        WAIT(ACT, th, h2bfree[i]); thb = act(ACT.copy(out=h2b[i][:], in_=h2[i][:]))
                        WAIT(PE, th, pTfree)
                        for k in range(8):
                            tt = PE.transpose((pTa if k < 4 else pTb)[:, (k % 4) * 128:(k % 4 + 1) * 128], h2[i][:, k * 128:(k + 1) * 128], ident[:])
                        tt = pe(tt)
                        WAIT(DVE, tt, h2Tfree)
                        DVE.tensor_copy(out=h2T[:, 0:4, :], in_=pTa[:].rearrange("p (k n) -> p k n", k=4))
                        tcp = dve(DVE.tensor_copy(out=h2T[:, 4:8, :], in_=pTb[:].rearrange("p (k n) -> p k n", k=4))); pTfree = tcp
                        WAIT(PE, tcp, tld0, pLfree, tk_const)
                        for k in range(8):
                            PE.matmul(pL, lhsT=h2T[:, k, :], rhs=wr[:, k, :], start=(k == 0), stop=False)
                        tlg = pe(PE.matmul(pL, lhsT=ones1[:], rhs=br[:], start=False, stop=True)); h2Tfree = tlg
                        S = sm[i]; M8 = m8[i]
                        WAIT(DVE, tlg, smfree[i], tld0)
                        t_ = dve(DVE.tensor_copy(out=S[:, 0, :], in_=pL)); pLfree = t_
                        WAIT(DVE, t_); t_ = dve(DVE.max(out=M8[:, 0, :], in_=S[:, 0, :]))
                        WAIT(DVE, t_); tM = dve(DVE.tensor_scalar(out=S[:, 1, :], in0=S[:, 0, :], scalar1=M8[:, 0, 3:4], scalar2=None, op0=ALU.is_ge))
                        tn = dve(DVE.tensor_scalar(out=M8[:, 2, 0:1], in0=M8[:, 0, 0:1], scalar1=-1.0, scalar2=None, op0=ALU.mult))
                        WAIT(ACT, tn); tex = act(ACT.activation(out=S[:, 2, :], in_=S[:, 0, :], func=AF.Exp, bias=M8[:, 2, 0:1], scale=1.0))
                        WAIT(DVE, tex, tM); t_ = dve(DVE.scalar_tensor_tensor(out=S[:, 3, :], in0=S[:, 2, :], scalar=1.0, in1=S[:, 1, :], op0=ALU.mult, op1=ALU.mult, accum_out=M8[:, 2, 1:2]))
                        WAIT(DVE, t_); t_ = dve(DVE.reciprocal(out=M8[:, 2, 2:3], in_=M8[:, 2, 1:2]))
                        WAIT(DVE, t_); tG = dve(DVE.tensor_scalar(out=S[:, 4, :], in0=S[:, 3, :], scalar1=M8[:, 2, 2:3], scalar2=None, op0=ALU.mult))
                        tMv = dve(DVE.tensor_scalar(out=S[:, 7, :], in0=S[:, 1, :], scalar1=tv[:, tg:tg + 1], scalar2=None, op0=ALU.mult))
                        WAIT(DVE, tMv); tMb = dve(DVE.tensor_copy(out=Mb[i][:], in_=S[:, 7, :]))
                        WAIT(PE, tMb, pRfree)
                        PE.matmul(pR[:, 0, :], lhsT=ustrict[:], rhs=Mb[i][:], start=True, stop=True)
                        tR = pe(PE.matmul(pR[:, 1, :], lhsT=onesb[:], rhs=Mb[i][:], start=True, stop=True, skip_group_check=True))
                        WAIT(DVE, tR, cntT)
                        t_ = dve(DVE.tensor_tensor(out=S[:, 5, :], in0=pR[:, 0, :], in1=cnt[:], op=ALU.add))
                        WAIT(DVE, t_); cntT = dve(DVE.tensor_tensor(out=cnt[:], in0=pR[:, 1, :], in1=cnt[:], op=ALU.add)); pRfree = cntT
                        WAIT(DVE, t_); tok_ = dve(DVE.tensor_scalar(out=S[:, 6, :], in0=S[:, 5, :], scalar1=float(C), scalar2=None, op0=ALU.is_lt))
                        WAIT(DVE, tok_, tMb); tok_ = dve(DVE.tensor_tensor(out=S[:, 7, :], in0=S[:, 7, :], in1=S[:, 6, :], op=ALU.mult))
                        WAIT(DVE, tok_); t_ = dve(DVE.tensor_tensor(out=S[:, 5, :], in0=S[:, 5, :], in1=ecol[:], op=ALU.add))
                        WAIT(DVE, t_); tsv = dve(DVE.tensor_tensor(out=S[:, 5, :], in0=S[:, 5, :], in1=S[:, 7, :], op=ALU.mult))
                        WAIT(DVE, tsv); t_ = dve(DVE.max(out=M8[:, 1, :], in_=S[:, 5, :]))
                        WAIT(DVE, t_)
                        t_ = dve(DVE.tensor_scalar(out=M8[:, 3, 0:4], in0=M8[:, 1, 0:4], scalar1=0.5, scalar2=float(E * C + 8), op0=ALU.is_lt, op1=ALU.mult))
                        WAIT(DVE, t_); t_ = dve(DVE.scalar_tensor_tensor(out=M8[:, 3, 0:4], in0=M8[:, 1, 0:4], scalar=-1.0, in1=M8[:, 3, 0:4], op0=ALU.add, op1=ALU.add))
                        WAIT(DVE, t_); tidx = dve(DVE.tensor_copy(out=S4[:, tg, :], in_=M8[:, 3, 0:4]))
                        tflag = dve(DVE.tensor_scalar(out=M8[:, 3, 4:8], in0=M8[:, 3, 0:4], scalar1=float(EH * C), scalar2=None, op0=ALU.is_lt))
                        tlow = dve(DVE.tensor_scalar(out=M8[:, 2, 4:8], in0=M8[:, 3, 0:4], scalar1=float(EH * C), scalar2=float(2 * E * C), op0=ALU.is_lt, op1=ALU.mult))
                        WAIT(DVE, tlow); tlow = dve(DVE.scalar_tensor_tensor(out=M8[:, 2, 4:8], in0=M8[:, 3, 0:4], scalar=-float(EH * C), in1=M8[:, 2, 4:8], op0=ALU.add, op1=ALU.add))
                        WAIT(DVE, tlow); tidxb = dve(DVE.tensor_copy(out=S4b[:, tg, :], in_=M8[:, 2, 4:8]))
                        tgk = None
                        for k in range(4):
                            WAIT(DVE, tsv, tG, tgk)
                            t_ = dve(DVE.tensor_scalar(out=S[:, 6, :], in0=S[:, 5, :], scalar1=M8[:, 1, k:k + 1], scalar2=None, op0=ALU.is_equal))
                            WAIT(DVE, t_); tgk = dve(DVE.scalar_tensor_tensor(out=S[:, 7, :] if False else S[:, 6, :], in0=S[:, 6, :], scalar=1.0, in1=S[:, 4, :], op0=ALU.mult, op1=ALU.mult, accum_out=G4[:, tg, k:k + 1]))
                        WAIT(DVE, tgk); tgv = dve(DVE.tensor_scalar(out=G4B[:, tg, :], in0=G4[:, tg, :], scalar1=tv[:, tg:tg + 1], scalar2=None, op0=ALU.mult))
                        WAIT(DVE, tgv, tflag); tgv = dve(DVE.tensor_tensor(out=G4[:, tg, :], in0=G4B[:, tg, :], in1=M8[:, 3, 4:8], op=ALU.mult))
                        WAIT(DVE, tgv); tgv = dve(DVE.tensor_tensor(out=G4B[:, tg, :], in0=G4B[:, tg, :], in1=G4[:, tg, :], op=ALU.subtract))
                        smfree[i] = [tgv, tidxb]; h2free[i] = tlg
                        WAIT(POOL, tidx, thb)
                        for k in range(4):
                            tsc = dqsc[i].go(POOL, "indirect_dma_start", out=Xg[l][:, :], out_offset=bass.IndirectOffsetOnAxis(ap=S4[:, tg, k:k + 1], axis=0),
                                                                  in_=h2b[i][:, :], in_offset=None, bounds_check=bc_reg, oob_is_err=False)
                        h2bfree[i] = tsc
                WAIT(POOL, h2bfree)
                WAIT(DVE, cntT); dve(DVE.tensor_copy(out=cnt_i[0:1, :], in_=cnt[0:1, :]))
                if idx_out is not None and l == 0:
                    dqix = DQ("ix"); WAIT(POOL, (sDVE, sDVE.n))
                    dqix.go(POOL, "dma_start", out=idx_out[0, :, :], in_=S4[:].rearrange("p t k -> p (t k)"))
                    tix = dqix.go(POOL, "dma_start", out=idx_out[1, :, :], in_=S4b[:].rearrange("p t k -> p (t k)")); WAIT(POOL, tix)
                if cnt_out is not None:
                    dqcn = DQ("cn"); WAIT(POOL, cntT)
                    tcn = dqcn.go(POOL, "dma_start", out=cnt_out[l:l + 1, :], in_=cnt[0:1, :]); WAIT(POOL, tcn)
            barrier()
            if getattr(cfg, 'stop', None) == (l, 'D'): raise _Stop()

            with contextlib.ExitStack() as bufs_:
                bufs = bufs_
                wgu = [sb(f"wgu{i}", [128, 8, 2048], BF16) for i in range(2)]; wd = [sb(f"wd{i}", [128, 8, D], BF16) for i in range(2)]
                bgu = [sb(f"bgu{i}", [128, 16], F32) for i in range(2)]; bd = [sb(f"bd{i}", [1, D], F32) for i in range(2)]
                xg = [sb(f"xg{i}", [128, 4, D], BF16) for i in range(2)]; xT = sb("xT", [128, 8, 512], BF16); aT = sb("aT", [128, 8, 512], BF16)
                gt = [sb(f"gt{i}", [128, 512], F32) for i in range(2)]; sg = [sb(f"sg{i}", [128, 512], F32) for i in range(2)]; ut = [sb(f"ut{i}", [128, 512], F32) for i in range(2)]
                yo = [sb(f"yo{i}", [128, D], F32) for i in range(2)]; yzero = sb("yzero", [128, D], F32)
                tyz = dve(DVE.memset(yzero[:], 0.0)); WAIT(POOL, tyz)
                pT = ps("epT", [128, D], BF16); pG = [ps(f"pG{i}", [128, 512], F32) for i in range(2)]; pU = [ps(f"pU{i}", [128, 512], F32) for i in range(2)]
                pY = [ps(f"pY{i}", [128, 512], F32) for i in range(2)]
                dqw_ = [DQ("ew0"), DQ("ew1")]; dqx = [DQ("ex0"), DQ("ex1")]; dqy = [DQ("ey0"), DQ("ey1")]
                wfree = [None, None]; xgfree = [None, None]; pTfree = None; xTfree = None; aTfree = None
                pGfree = [None, None]; pUfree = [None, None]; gtfree = [None, None]; sgfree = [None, None]; utfree = [None, None]
                pYfree = [None, None]; yofree = [None, None]
                chunks = [(c0, min(512, C - c0)) for c0 in range(0, C, 512)]
                stE = dict(xi=0, fi=0, yi=0, yoi=0, pTfree=None, xTfree=None, aTfree=None)
                def chunk_body(PE, ACT, DVE, POOL, SP, e, wi, c0, nsl, twl):
                    nb = nsl // 128; j = stE["xi"] % 2; stE["xi"] += 1
                    WAIT(SP, xgfree[j])
                    r0 = e * C + c0
                    txg = dqx[j].go(SP, "dma_start", out=xg[j][:, 0:nb, :], in_=Xg[l][r0:r0 + nsl, :].rearrange("(b p) d -> p b d", p=128))
                    for b in range(nb):
                        WAIT(PE, txg, stE["pTfree"])
                        for k in range(8):
                            tt = PE.transpose(pT[:, k * 128:(k + 1) * 128], xg[j][:, b, k * 128:(k + 1) * 128], identb[:])
                        tt = pe(tt)
                        WAIT(DVE, tt, stE["xTfree"] if b == 0 else None)
                        stE["pTfree"] = dve(DVE.tensor_copy(out=xT[:, :, b * 128:(b + 1) * 128], in_=pT[:].rearrange("p (k n) -> p k n", k=8)))
                    xgfree[j] = tt; txT = stE["pTfree"]
                    for fp in range(8):
                        f = stE["fi"] % 2; stE["fi"] += 1
                        WAIT(PE, txT, twl, pGfree[f], pUfree[f])
                        for k in range(8):
                            mm = PE.matmul(pG[f][:, 0:nsl], lhsT=wgu[wi][:, k, fp * 128:(fp + 1) * 128], rhs=xT[:, k, 0:nsl], start=(k == 0), stop=(k == 7))
                        tg_ = pe(mm)
                        for k in range(8):
                            mm = PE.matmul(pU[f][:, 0:nsl], lhsT=wgu[wi][:, k, 1024 + fp * 128:1024 + (fp + 1) * 128], rhs=xT[:, k, 0:nsl], start=(k == 0), stop=(k == 7))
                        tu_ = pe(mm)
                        WAIT(DVE, tg_, gtfree[f])
                        t1 = dve(DVE.tensor_scalar(out=gt[f][:, 0:nsl], in0=pG[f][:, 0:nsl], scalar1=bgu[wi][:, fp:fp + 1], scalar2=7.0, op0=ALU.add, op1=ALU.min)); pGfree[f] = t1
                        WAIT(ACT, t1, sgfree[f]); t2 = act(ACT.activation(out=sg[f][:, 0:nsl], in_=gt[f][:, 0:nsl], func=AF.Sigmoid, scale=1.702))
                        WAIT(DVE, tu_, utfree[f])
                        t3 = dve(DVE.tensor_scalar(out=ut[f][:, 0:nsl], in0=pU[f][:, 0:nsl], scalar1=bgu[wi][:, 8 + fp:9 + fp], scalar2=7.0, op0=ALU.add, op1=ALU.min)); pUfree[f] = t3
                        WAIT(DVE, t3); t4 = dve(DVE.tensor_scalar(out=ut[f][:, 0:nsl], in0=ut[f][:, 0:nsl], scalar1=-7.0, scalar2=1.0, op0=ALU.max, op1=ALU.add))
                        WAIT(DVE, t4, t2); t5 = dve(DVE.tensor_tensor(out=gt[f][:, 0:nsl], in0=gt[f][:, 0:nsl], in1=sg[f][:, 0:nsl], op=ALU.mult)); sgfree[f] = t5
                        WAIT(DVE, t5, stE["aTfree"] if fp == 0 else None)
                        t6 = dve(DVE.tensor_tensor(out=aT[:, fp, 0:nsl], in0=gt[f][:, 0:nsl], in1=ut[f][:, 0:nsl], op=ALU.mult)); gtfree[f] = t6; utfree[f] = t6
                    stE["xTfree"] = tu_
                    for b in range(nb):
                        yb = stE["yoi"] % 2; stE["yoi"] += 1
                        for hh in range(2):
                            y = stE["yi"] % 2; stE["yi"] += 1
                            WAIT(PE, t6, pYfree[y])
                            for k in range(8):
                                PE.matmul(pY[y][:], lhsT=aT[:, k, b * 128:(b + 1) * 128], rhs=wd[wi][:, k, hh * 512:(hh + 1) * 512], start=(k == 0), stop=False)
                            ty = pe(PE.matmul(pY[y][:], lhsT=ones1[:], rhs=bd[wi][:, hh * 512:(hh + 1) * 512], start=False, stop=True))
                            WAIT(ACT, ty, yofree[yb]); pYfree[y] = act(ACT.copy(out=yo[yb][:, hh * 512:(hh + 1) * 512], in_=pY[y][:]))
                        WAIT(POOL, pYfree[y])
                        rr = (e % EH) * C + c0 + b * 128
                        yofree[yb] = dqy[yb].go(POOL, "dma_start", out=YgH[l][e // EH][rr:rr + 128, :], in_=yo[yb][:])
                    stE["aTfree"] = ty
                    return ty
                dyn = getattr(cfg, "dyn", True)
                for e in range(E):
                    wi = e % 2
                    WAIT(POOL, wfree[wi])
                    for k in range(8):
                        dqw_[wi].go(POOL, "dma_start", out=wgu[wi][:, k, :], in_=Wl["w_gu"][e, k * 128:(k + 1) * 128, :], max_dma_last_dim=4096)
                        dqw_[wi].go(POOL, "dma_start", out=wd[wi][:, k, :], in_=Wl["w_d"][e, k * 128:(k + 1) * 128, :], max_dma_last_dim=4096)
                    dqw_[wi].go(POOL, "dma_start", out=bgu[wi][:], in_=Wl["b_gu"][e:e + 1, :].rearrange("o (f p) -> p (o f)", p=128), allow_slow_non_contiguous=True)
                    twl = dqw_[wi].go(POOL, "dma_start", out=bd[wi][:], in_=Wl["b_d"][e:e + 1, :])
                    for (c0, nsl) in chunks:
                        if c0 == 0 or not dyn:
                            ty = chunk_body(PE, ACT, DVE, POOL, SP, e, wi, c0, nsl, twl)
                        else:
                            ty = guarded(c0 + 1, cnt_i[0:1, e:e + 1], lambda PE_, ACT_, DVE_, POOL_, SP_: chunk_body(PE_, ACT_, DVE_, POOL_, SP_, e, wi, c0, nsl, twl), zero_src=yzero[:])
                    wfree[wi] = ty
                WAIT(POOL, yofree)
            barrier()
            if getattr(cfg, 'stop', None) == (l, 'E'): raise _Stop()

            with contextlib.ExitStack() as bufs_:
                bufs = bufs_
                gate2 = sb("gate2", [128, D], F32)
                yk = [sb(f"yk{i}", [128, 4, D], F32) for i in range(2)]; ykB = [sb(f"ykB{i}", [128, 4, D], F32) for i in range(2)]; xt = [sb(f"fx{i}", [128, D], F32) for i in range(2)]
                acc = [sb(f"acc{i}", [128, D], F32) for i in range(2)]
                dqg = [DQ("g0"), DQ("g1")]; dqx = [DQ("fx0"), DQ("fx1")]; dqo_ = [DQ("fo0"), DQ("fo1")]; dql = DQ("fl")
                for i in range(2):
                    DVE.memset(ykB[i][:], 0.0); tms = dve(DVE.memset(yk[i][:], 0.0))
                ykfree = [tms, tms]; xfree = [None, None]; accfree = [None, None]
                ti = 0
                for s in range(nseg):
                    WAIT(SP, (sDVE, sDVE.n)); tg2 = dql.go(SP, "dma_start", out=gate2[:], in_=modd[s:s + 1, 5 * D:6 * D].to_broadcast([128, D]))
                    for qc in range(Qc[s]):
                        i = ti % 2; ti += 1; tg = Qoff[s] + qc
                        WAIT(POOL, ykfree[i])
                        for hh, ixt, dst_ in ((0, S4, yk[i]), (1, S4b, ykB[i])):
                            for k in range(4):
                                tgt = dqg[i].go(POOL, "indirect_dma_start", out=dst_[:, k, :], out_offset=None, in_=YgH[l][hh][:, :],
                                                in_offset=bass.IndirectOffsetOnAxis(ap=ixt[:, tg, k:k + 1], axis=0), bounds_check=bch_reg, oob_is_err=False)
                        WAIT(SP, xfree[i]); tx = dqx[i].go(SP, "dma_start", out=xt[i][:], in_=x1d[l][tg * 128:(tg + 1) * 128, :])
                        WAIT(DVE, tgt, accfree[i], tg2)
                        t_ = dve(DVE.tensor_scalar(out=acc[i][:], in0=yk[i][:, 0, :], scalar1=G4[:, tg, 0:1], scalar2=None, op0=ALU.mult))
                        for k in range(1, 4):
                            WAIT(DVE, t_); t_ = dve(DVE.scalar_tensor_tensor(out=acc[i][:], in0=yk[i][:, k, :], scalar=G4[:, tg, k:k + 1], in1=acc[i][:], op0=ALU.mult, op1=ALU.add))
                        for k in range(4):
                            WAIT(DVE, t_); t_ = dve(DVE.scalar_tensor_tensor(out=acc[i][:], in0=ykB[i][:, k, :], scalar=G4B[:, tg, k:k + 1], in1=acc[i][:], op0=ALU.mult, op1=ALU.add))
                        ykfree[i] = t_
                        WAIT(DVE, t_); t_ = dve(DVE.tensor_tensor(out=acc[i][:], in0=acc[i][:], in1=gate2[:], op=ALU.mult))
                        WAIT(DVE, t_, tx); to = dve(DVE.tensor_tensor(out=acc[i][:], in0=acc[i][:], in1=xt[i][:], op=ALU.add)); xfree[i] = to
                        WAIT(POOL, to)
                        if l == 0:
                            accfree[i] = dqo_[i].go(POOL, "dma_start", out=x2d[tg * 128:(tg + 1) * 128, :], in_=acc[i][:])
                        else:
                            accfree[i] = dqo_[i].go(POOL, "dma_start", out=youts[s][qc * 128:(qc + 1) * 128, :], in_=acc[i][:])
                WAIT(POOL, accfree)
            barrier()
            if getattr(cfg, 'stop', None) == (l, 'F'): raise _Stop()
        pst.close()
    return nc


def seg_layout(cfg, core, seqlens):
    raise NotImplementedError


def make_inputs(cfg, core_segs, xs, cs, inp):
    E = cfg.E; m = {}
    kval0 = np.zeros((128, cfg.NR0), np.float32); kval1 = np.zeros((128, cfg.NQ0), np.float32); tval0 = np.zeros((128, cfg.NQ0), np.float32)
    cc = np.zeros((len(core_segs), D), np.float32)
    rpb = inp["l1_rpb_c"]
    b1 = np.empty((1 + 4 * len(core_segs), 28, 128, 512), np.float32); b1[0] = host_bias_l1(rpb, None, 0)
    for s, (xf, crow, start, L) in enumerate(core_segs):
        own = cfg.own[s] * 128
        pos = np.arange(start - H0C * 128, start + own + H0C * 128)
        ok = (pos >= 0) & (pos < L)
        xe = np.zeros((len(pos), D), np.float32); xe[ok] = xf[pos[ok]]
        m[f"xe{s}"] = xe; cc[s] = crow
        kval0[:, cfg.R0off[s]:cfg.R0off[s] + cfg.R0[s]] = np.where(ok, 0.0, NEG).reshape(cfg.R0[s], 128).T
        posq = np.arange(start - HQC * 128, start + own + HQC * 128); okq = (posq >= 0) & (posq < L)
        kval1[:, cfg.Q0off[s]:cfg.Q0off[s] + cfg.Q0[s]] = np.where(okq, 0.0, NEG).reshape(cfg.Q0[s], 128).T
        tval0[:, cfg.Q0off[s]:cfg.Q0off[s] + cfg.Q0[s]] = okq.astype(np.float32).reshape(cfg.Q0[s], 128).T
        nrows = L // 64; r_first = start // 64; nblk = cfg.own[s]
        for j, qc in enumerate([0, 1, nblk - 2, nblk - 1]):
            b1[1 + 4 * s + j] = host_bias_l1(rpb, r_first + 2 * qc, nrows)
    m["c"] = cc; m["kval0"] = kval0; m["kval1"] = kval1; m["tval0"] = tval0
    m["ident"] = np.eye(128, dtype=np.float32)
    m["ustrict"] = np.triu(np.ones((128, 128), np.float32), 1)
    bo = np.zeros((128, 128), np.float32); bo[:64, :64] = 1; bo[64:, 64:] = 1; m["bones"] = bo
    for l in range(2):
        m[f"ecol{l}"] = np.broadcast_to((np.arange(E) * cfg.C[l] + 1).astype(np.float32), (128, E)).copy()
    m["bias0"] = host_bias_l0(); m["bias1"] = b1
    m["sink"] = inp["l0_sink_a"].reshape(1, 8)
    def g2(v): return np.concatenate([v, v]).astype(np.float32)
    m["l0_gains"] = np.stack([g2(inp["l0_q_norm_a"]), g2(inp["l0_k_norm_a"]), g2(inp["l0_q_norm_b"]), g2(inp["l0_k_norm_b"])], 1)
    m["l1_gains"] = np.stack([g2(inp["l1_q_norm_c"]), g2(inp["l1_k_norm_c"]), g2(inp["l1_q_norm_c"]), g2(inp["l1_k_norm_c"])], 1)
    for l in range(2):
        p = f"l{l}_"
        m[p + "ada_w"] = inp[p + "ada_w"]; m[p + "ada_b"] = inp[p + "ada_b"].reshape(1, -1); m[p + "norm1"] = inp[p + "norm1"].reshape(1, -1)
        m[p + "w_in"] = inp[p + "w_in"]; m[p + "w_out"] = inp[p + "w_out"]; m[p + "norm2"] = inp[p + "norm2"].reshape(1, -1)
        m[p + "w_router"] = inp[p + "w_router"]; m[p + "b_router"] = inp[p + "b_router"].reshape(1, -1)
        m[p + "w_gate_up"] = inp[p + "w_gate_up"]; m[p + "b_gate_up"] = inp[p + "b_gate_up"]
        m[p + "w_down"] = inp[p + "w_down"]; m[p + "b_down"] = inp[p + "b_down"]
    return m


def kernel(**inputs):
    inp = {k: np.asarray(v) for k, v in inputs.items()}
    cfg = full_cfg()
    xp = inp["x_prompt"]; xs = inp["x_sample"]; cp = inp["c_prompt"]; cs = inp["c_sample"]
    in_maps = []
    for c in range(8):
        segs = [(xp[0], cp[0], c * 2048, xp.shape[1]), (xs[c // 2], cs[c // 2], (c % 2) * 4096, xs.shape[1])]
        in_maps.append(make_inputs(cfg, segs, None, None, inp))
    nc = build(cfg)
    res = run_bass_kernel_spmd(nc, in_maps, core_ids=list(range(8)))
    yp = np.empty_like(xp); ys = np.empty_like(xs)
    for c in range(8):
        yp[0, c * 2048:(c + 1) * 2048] = res.results[c]["y0"]
        ys[c // 2, (c % 2) * 4096:(c % 2 + 1) * 4096] = res.results[c]["y1"]
    return (yp, ys)
```

```python
import contextlib
import numpy as np
import concourse.bass as bass
import concourse.mybir as mybir
from concourse.bass_utils import run_bass_kernel_spmd

F32 = mybir.dt.float32; BF16 = mybir.dt.bfloat16; I32 = mybir.dt.int32
AF = mybir.ActivationFunctionType; ALU = mybir.AluOpType
D = 1024; NEG = -30000.0; H0C = 10; HQC = 2; TOPK = 4


class _Stop(Exception):
    pass


class _RecInstr:
    def __init__(s, ent): s.ent = ent


class _Rec:
    def __init__(s, log): s._log = log
    def __getattr__(s, name):
        def f(*a, **kw):
            ent = [name, a, kw, []]; s._log.append(ent); return _RecInstr(ent)
        return f


def _stoprange(cfg):
    return range(2)


class Cfg:
    def __init__(s, ncores, own, E, C):
        s.ncores = ncores; s.own = own; s.E = E; s.C = C
        s.R0 = [o + 2 * H0C for o in own]; s.Q0 = [o + 2 * HQC for o in own]
        s.R0off = [sum(s.R0[:i]) for i in range(len(own))]; s.Q0off = [sum(s.Q0[:i]) for i in range(len(own))]
        s.Ooff = [sum(own[:i]) for i in range(len(own))]
        s.NR0 = sum(s.R0); s.NQ0 = sum(s.Q0); s.NO = sum(own)


def full_cfg():
    return Cfg(8, [16, 32], 32, [3584, 3584])


def layer_specs():
    L0 = dict(nqb=10, nkb=7, nvh=14, nko=6)
    L0["qcols"] = [b * 128 for b in range(4)] + [768 + g * 256 + j * 128 for g in range(3) for j in range(2)]
    L0["kcols"] = [512] + [1536 + g * 256 + j * 128 for g in range(3) for j in range(2)]
    L0["vsets"] = [(640, 128, 0), (2304, 512, 2), (2816, 256, 10)]
    L0["qgain"] = [0] * 4 + [2] * 6; L0["kgain"] = [1] + [3] * 6
    L0["wgroups"] = [(0, 6, 1), (6, 10, 2), (10, 14, 8)]
    quads = []
    for j in range(2):
        heads = [(4 * j + i, j) for i in range(4)]
        quads.append(dict(units=[(heads, d, ("A", j, d)) for d in (-1, 0, 1)], sink=True))
    bun = []
    for g, rng in enumerate([range(-1, 2), range(-2, 3), range(-8, 9)]):
        heads = [(8 + 4 * g + h, 2 + 4 * g + h) for h in range(4)]
        bun += [(heads, d, ("B", g, d)) for d in rng]
    quads.append(dict(units=bun, sink=False))
    L0["quads"] = quads
    L1 = dict(nqb=8, nkb=8, nvh=16, nko=8)
    L1["qcols"] = [b * 128 for b in range(8)]; L1["kcols"] = [1024 + b * 128 for b in range(8)]
    L1["vsets"] = [(2048, 512, 0), (2560, 512, 8)]
    L1["qgain"] = [0] * 8; L1["kgain"] = [1] * 8
    L1["wgroups"] = [(0, 16, 3)]
    quads = []
    for qd in range(4):
        heads = [(4 * qd + h, 4 * qd + h) for h in range(4)]
        quads.append(dict(units=[(heads, d, ("C", qd, d)) for d in range(-3, 4)], sink=False))
    L1["quads"] = quads
    return L0, L1


def l0_bias_list():
    L0, _ = layer_specs()
    return [u[2] for q in L0["quads"] for u in q["units"]]


def host_bias_l0():
    sl_a = 2.0 ** (-8.0 * np.arange(1, 9) / 8); sl_b = (2.0 ** (-8.0 * np.arange(1, 13) / 12)).reshape(3, 4)
    dil = [1, 4, 16]
    kp = np.arange(128)[:, None]; qp = np.arange(128)[None, :]
    out = []
    for (kind, j, d) in l0_bias_list():
        rel = d * 128 + kp - qp
        t = np.empty((128, 4, 128), np.float32)
        for h in range(4):
            if kind == "A":
                ok = np.abs(rel) <= 128; s = sl_a[4 * j + h]
            else:
                ok = (np.abs(rel) <= 64 * dil[j]) & (rel % dil[j] == 0); s = sl_b[j, h]
            t[:, h, :] = np.where(ok, -s * np.abs(rel), NEG)
        out.append(t.reshape(128, 512))
    return np.stack(out).astype(np.float32)


def host_bias_l1(rpb, r0, nrows):
    key = np.arange(128); q = np.arange(128)
    krl = (key // 64)[:, None]; kc = (key % 64)[:, None]; qrl = (q // 64)[None, :]; c = (q % 64)[None, :]
    cs = np.clip(c - 8, 0, 48); colok = (kc >= cs) & (kc < cs + 16); cidx = np.clip(kc - c + 15, 0, 30)
    out = np.empty((4, 7, 128, 4, 128), np.float32)
    for di, d in enumerate(range(-3, 4)):
        dr = 2 * d + krl - qrl
        if r0 is None:
            rowok = (dr >= -4) & (dr <= 3)
        else:
            r = r0 + qrl; rs = np.clip(r - 4, 0, nrows - 8); kr = r + dr
            rowok = (kr >= rs) & (kr < rs + 8)
        ridx = np.clip(dr + 7, 0, 14); ok = rowok & colok
        for a in range(16):
            out[a // 4, di, :, a % 4, :] = np.where(ok, rpb[a][ridx, cidx], NEG)
    return out.reshape(28, 128, 512)


def build(cfg):
    nc = bass.Bass("TRN2", target_bir_lowering=False)
    nseg = len(cfg.own); E = cfg.E
    L0, L1 = layer_specs(); LS = [L0, L1]
    NU0 = len(l0_bias_list())

    def din(name, shape, dt=F32): return nc.dram_tensor(name, list(shape), dt, kind="ExternalInput")
    def dsc(name, shape, dt): return nc.dram_tensor(name, list(shape), dt)
    xe = [din(f"xe{s}", [cfg.R0[s] * 128, D]) for s in range(nseg)]
    cin = din("c", [nseg, D])
    kval0 = din("kval0", [128, cfg.NR0]); kval1 = din("kval1", [128, cfg.NQ0]); tval0 = din("tval0", [128, cfg.NQ0])
    ident_in = din("ident", [128, 128]); ustrict_in = din("ustrict", [128, 128]); bones_in = din("bones", [128, 128])
    ecol_in = [din(f"ecol{l}", [128, E]) for l in range(2)]
    bias0_in = din("bias0", [NU0, 128, 512]); bias1_in = din("bias1", [1 + 4 * nseg, 28, 128, 512])
    W = []
    for l in range(2):
        w = dict(ada_w=din(f"l{l}_ada_w", [D, 6 * D]), ada_b=din(f"l{l}_ada_b", [1, 6 * D]), norm1=din(f"l{l}_norm1", [1, D]),
                 w_in=din(f"l{l}_w_in", [D, 3072]), gains=din(f"l{l}_gains", [128, 4]), w_out=din(f"l{l}_w_out", [LS[l]["nko"] * 128, D]),
                 norm2=din(f"l{l}_norm2", [1, D]), w_router=din(f"l{l}_w_router", [D, E]), b_router=din(f"l{l}_b_router", [1, E]),
                 w_gu=din(f"l{l}_w_gate_up", [E, D, 2048]), b_gu=din(f"l{l}_b_gate_up", [E, 2048]),
                 w_d=din(f"l{l}_w_down", [E, 1024, D]), b_d=din(f"l{l}_b_down", [E, D]))
        W.append(w)
    sink_in = din("sink", [1, 8])
    youts = [nc.dram_tensor(f"y{s}", [cfg.own[s] * 128, D], F32, kind="ExternalOutput") for s in range(nseg)]
    cnt_out = nc.dram_tensor("cnt_out", [2, E], F32, kind="ExternalOutput") if getattr(cfg, 'dbg', {}).get('cnt_out') else None
    idx_out = nc.dram_tensor("idx_out", [2, 128, cfg.NQ0 * 4], I32, kind="ExternalOutput") if getattr(cfg, 'dbg', {}).get('idx_out') else None
    modd = dsc("modd", [nseg, 6 * D], F32)
    NRl = [cfg.NR0, cfg.NQ0]; NQl = [cfg.NQ0, cfg.NO]
    qT = [dsc(f"qT{l}", [2 * LS[l]["nqb"], 64, NQl[l] * 128], BF16) for l in range(2)]
    kT = [dsc(f"kT{l}", [2 * LS[l]["nkb"], 64, NRl[l] * 128], BF16) for l in range(2)]
    Vd = [dsc(f"V{l}", [NRl[l] * 128, LS[l]["nvh"] * 65], BF16) for l in range(2)]
    x1d = [dsc(f"x1_{l}", [NQl[l] * 128, D], F32) for l in range(2)]
    x2d = dsc("x2", [cfg.NQ0 * 128, D], F32)
    Xg = [dsc(f"Xg{l}", [E * cfg.C[l], D], BF16) for l in range(2)]
    EH = E // 2
    YgH = [[dsc(f"Yg{l}_{hh}", [EH * cfg.C[l], D], F32) for hh in range(2)] for l in range(2)]
    winb = [dsc(f"winb{l}", [D, 3072], BF16) for l in range(2)]
    ddram = dsc("ddram", [16, 64], F32)
    woutb = [dsc(f"woutb{l}", [LS[l]["nko"] * 128, D], BF16) for l in range(2)]

    es = contextlib.ExitStack()
    nsem = [0]; all_sems = []
    class Sem:
        def __init__(s, name):
            nsem[0] += 1; s.h = es.enter_context(nc.semaphore(f"{name}_{nsem[0]}")); s.n = 0
            all_sems.append(s)
    waited = {}
    def WAIT(eng, *toks):
        for t in toks:
            if t is None: continue
            if isinstance(t, list): WAIT(eng, *t); continue
            sem, v = t; key = (id(eng), id(sem))
            if hasattr(sem, "dq"): sem.dq.closed = True
            if waited.get(key, 0) >= v: continue
            waited[key] = v; eng.wait_ge(sem.h, v)
    def SIG(instr, sem, by=1):
        if isinstance(instr, _RecInstr): instr.ent[3].append((sem, by))
        else: instr.then_inc(sem.h, by)
        sem.n += by; return (sem, sem.n)
    PE, ACT, DVE, POOL, SP = nc.tensor, nc.scalar, nc.vector, nc.gpsimd, nc.sync
    sPE = Sem("pe"); sACT = Sem("act"); sDVE = Sem("dve"); sPOOL = Sem("pool")
    def pe(i): return SIG(i, sPE)
    def act(i): return SIG(i, sACT)
    def dve(i): return SIG(i, sDVE)
    def pool(i): return SIG(i, sPOOL)
    class DQ:
        def __init__(s, name): s.sem = Sem(name); s.sem.dq = s; s.closed = False
        def go(s, eng, meth, **kw):
            if s.closed:
                WAIT(eng, (s.sem, s.sem.n)); s.closed = False
            return SIG(getattr(eng, meth)(**kw), s.sem, 16)

    bufs = contextlib.ExitStack()
    uid = [0]
    def uname(name):
        uid[0] += 1; return f"{name}_u{uid[0]}"
    def sb(name, shape, dt): return bufs.enter_context(nc.sbuf_tensor(uname(name), list(shape), dt))
    def ps(name, shape, dt): return bufs.enter_context(nc.psum_tensor(uname(name), list(shape), dt))

    with es, contextlib.suppress(_Stop):
        pst = contextlib.ExitStack()
        def psb(name, shape, dt): return pst.enter_context(nc.sbuf_tensor(uname(name), list(shape), dt))
        ident = psb("ident", [128, 128], F32); identb = psb("identb", [128, 128], BF16)
        ustrict = psb("ustrict", [128, 128], BF16); onesb = psb("onesb", [128, 128], BF16); bones = psb("bonesb", [128, 128], BF16)
        ones1 = psb("ones1", [1, 128], F32); ctmp = psb("ctmp", [128, 128], F32)
        cnt_i = psb("cnti", [1, E], I32); S4 = psb("S4", [128, cfg.NQ0, 4], I32); S4b = psb("S4b", [128, cfg.NQ0, 4], I32); G4 = psb("G4", [128, cfg.NQ0, 4], F32); G4B = psb("G4B", [128, cfg.NQ0, 4], F32)
        dq0 = DQ("c0")
        t = dq0.go(SP, "dma_start", out=ident[:], in_=ident_in[:, :])
        WAIT(DVE, t); dve(DVE.tensor_copy(out=identb[:], in_=ident[:]))
        for src, dst in ((ustrict_in, ustrict), (bones_in, bones)):
            WAIT(SP, (sDVE, sDVE.n)); t = dq0.go(SP, "dma_start", out=ctmp[:], in_=src[:, :]); WAIT(DVE, t)
            dve(DVE.tensor_copy(out=dst[:], in_=ctmp[:]))
        DVE.memset(onesb[:], 1.0); tk_const = dve(DVE.memset(ones1[:], 1.0))
        for e_ in (PE, ACT, DVE, POOL, SP): WAIT(e_, tk_const)

        ENGS = (("PE", PE), ("ACT", ACT), ("DVE", DVE), ("POOL", POOL), ("SP", SP))
        cregs = {nm: en.alloc_register(f"cntreg_{nm}") for nm, en in ENGS}
        keep_alive = []; tzf = [None]
        ztp = psb("ztp", [128, D], BF16); tztp = dve(DVE.memset(ztp[:], 0.0))
        dsb = psb("dsb", [1, 16 * 16], F32); dzero = psb("dzero", [16, 64], F32); dids = {}
        tdz = dve(DVE.memset(dzero[:], 0.0)); WAIT(POOL, tdz)
        dq_dz = DQ("dz"); tdz = dq_dz.go(POOL, "dma_start", out=ddram[:, :], in_=dzero[:])
        for e_ in (POOL, SP): WAIT(e_, tdz)
        def guarded(thr, cnt_word, body, zero_src=None):
            logs = {nm: [] for nm, _ in ENGS}; prox = {nm: _Rec(logs[nm]) for nm, _ in ENGS}; keep_alive.append(prox)
            ret = body(prox["PE"], prox["ACT"], prox["DVE"], prox["POOL"], prox["SP"])
            for nm, en in ENGS:
                tot = {}
                for ent in logs[nm]:
                    for (sm, by) in ent[3]: tot[id(sm)] = (sm, tot.get(id(sm), (sm, 0))[1] + by)
                if not logs[nm]: continue
                en.reg_load(cregs[nm], cnt_word)
                with en.If_lt(cregs[nm], thr):
                    for sm, t_ in tot.values():
                        en.wait_ge(sm.h, sm.n - t_)
                        if hasattr(sm, "dq") and nm == "POOL" and zero_src is not None:
                            for (meth, a, kw, incs) in logs[nm]:
                                if meth == "dma_start" and any(s_ is sm for (s_, _) in incs):
                                    dd = en.dma_start(out=kw["out"], in_=zero_src)
                                    for (s_, by) in incs: dd.then_inc(s_.h, by)
                        elif hasattr(sm, "dq"):
                            did = dids.setdefault(id(sm), len(dids)); assert did < 16 and t_ % 16 == 0 and t_ // 16 <= 8
                            for k_ in range(t_ // 16):
                                if nm == "SP": dd = en.dma_start(out=dsb[0:1, did * 16 + 2 * k_:did * 16 + 2 * k_ + 2], in_=ddram[did:did + 1, 2 * k_:2 * k_ + 2])
                                else: dd = en.dma_start(out=ddram[did:did + 1, 32 + 2 * k_:32 + 2 * k_ + 2], in_=dzero[0:1, 0:2])
                                dd.then_inc(sm.h, 16)
                        else:
                            en.sem_inc(sm.h, t_)
                with en.Else():
                    for (meth, a, kw, incs) in logs[nm]:
                        ins = getattr(en, meth)(*a, **kw)
                        for (sm, by) in incs: ins.then_inc(sm.h, by)
            return ret

        def barrier():
            toks = [dve(DVE.memset(ctmp[0:1, 0:1], 0.0)), act(ACT.copy(out=ctmp[0:1, 1:2], in_=ones1[0:1, 0:1])),
                    pool(POOL.memset(ctmp[0:1, 2:3], 0.0)), (sPE, sPE.n)]
            for e in (PE, ACT, DVE, POOL, SP): WAIT(e, *toks)

        dqc = DQ("cast"); cast_t = None
        for l in range(2):
            for r in range(0, D, 128):
                cast_t = dqc.go(POOL, "dma_start", out=winb[l][r:r + 128, :], in_=W[l]["w_in"][r:r + 128, :], max_dma_last_dim=4096)
            for r in range(0, LS[l]["nko"] * 128, 128):
                cast_t = dqc.go(POOL, "dma_start", out=woutb[l][r:r + 128, :], in_=W[l]["w_out"][r:r + 128, :], max_dma_last_dim=4096)

        for l in (range(2) if not getattr(cfg, 'stop', None) else _stoprange(cfg)):
            Ls = LS[l]; Wl = W[l]; C = cfg.C[l]
            Rc = cfg.R0 if l == 0 else cfg.Q0; Roff = cfg.R0off if l == 0 else cfg.Q0off
            Qc = cfg.Q0 if l == 0 else cfg.own; Qoff = cfg.Q0off if l == 0 else cfg.Ooff
            q2r = 8 if l == 0 else 2
            xsrc = [xe[s] for s in range(nseg)] if l == 0 else [x2d[cfg.Q0off[s] * 128:(cfg.Q0off[s] + cfg.Q0[s]) * 128, :] for s in range(nseg)]
            kvald = kval0 if l == 0 else kval1

            with contextlib.ExitStack() as bufs_:
                bufs = bufs_
                cT = sb("cT", [128, nseg, 8], F32); scT = sb("scT", [128, 8, 128], F32)
                aw = [sb(f"aw{i}", [128, 8, 512], F32) for i in range(2)]; brow = sb("brow", [1, 6 * D], F32)
                mrow = sb("mrow", [128, 6 * D], F32); pm = [ps(f"pm{i}", [128, 512], F32) for i in range(2)]
                dqa = [DQ("aw0"), DQ("aw1")]; dql = DQ("la")
                for s in range(nseg):
                    t = dql.go(SP, "dma_start", out=cT[:, s, :], in_=cin[s:s + 1, :].rearrange("o (k p) -> p (o k)", p=128), allow_slow_non_contiguous=True)
                t = dql.go(SP, "dma_start", out=brow[:], in_=Wl["ada_b"][:, :])
                WAIT(ACT, t); ta = act(ACT.activation(out=cT[:], in_=cT[:], func=AF.Silu))
                WAIT(DVE, ta, t); half = 128 // nseg
                for s in range(nseg):
                    tsc = dve(DVE.tensor_copy(out=scT[:, :, s * half:(s + 1) * half], in_=cT[:, s, :].unsqueeze(2).to_broadcast([128, 8, half])))
                awfree = [None, None]; pmfree = [None, None]
                for j in range(12):
                    i = j % 2
                    WAIT(SP, awfree[i]); tl = dqa[i].go(SP, "dma_start", out=aw[i][:], in_=Wl["ada_w"][:, j * 512:(j + 1) * 512].rearrange("(k p) n -> p k n", p=128))
                    WAIT(PE, tl, tsc, pmfree[i], tk_const)
                    for k in range(8):
                        PE.matmul(pm[i][:], lhsT=scT[:, k, :], rhs=aw[i][:, k, :], start=(k == 0), stop=False)
                    tp = pe(PE.matmul(pm[i][:], lhsT=ones1[:].to_broadcast([1, 128]) if False else ones1[:], rhs=brow[:, j * 512:(j + 1) * 512], start=False, stop=True))
                    awfree[i] = tp
                    WAIT(DVE, tp); pmfree[i] = dve(DVE.tensor_copy(out=mrow[:, j * 512:(j + 1) * 512], in_=pm[i][:]))
                WAIT(POOL, pmfree[0], pmfree[1]); dqs = DQ("ms")
                for s in range(nseg):
                    t = dqs.go(POOL, "dma_start", out=modd[s:s + 1, :], in_=mrow[s * half:s * half + 1, :])
                WAIT(POOL, t)
            barrier()
            if getattr(cfg, 'stop', None) == (l, 'A'): raise _Stop()

            with contextlib.ExitStack() as bufs_:
                bufs = bufs_
                wsb = sb("wsb", [128, 8, 3072], BF16); gains = sb("gains", [128, 4], F32); g1b = sb("g1b", [128, D], F32)
                Ab = sb("Ab", [128, D], F32); Bb = sb("Bb", [128, D], F32)
                xt = [sb(f"xt{i}", [128, D], F32) for i in range(2)]; junk = sb("junk", [128, D], BF16)
                st = [sb(f"st{i}", [128, 4], F32) for i in range(2)]
                hb = [sb(f"hb{i}", [128, D], BF16) for i in range(2)]; hT = [sb(f"hT{i}", [128, 8, 512], BF16) for i in range(2)]
                sq = [sb(f"sq{i}", [128, 512], BF16) for i in range(2)]; rs = [sb(f"rs{i}", [128, 512], F32) for i in range(2)]
                qk = [sb(f"qk{i}", [128, 512], BF16) for i in range(2)]; vx = [sb(f"vx{i}", [128, Ls["nvh"], 65], BF16) for i in range(2)]
                ptr = ps("ptr", [128, D], BF16); pq = [ps(f"pq{i}", [128, 512], F32) for i in range(2)]
                p2 = [ps(f"p2{i}", [128, 512], F32) for i in range(2)]; pv = [ps(f"pv{i}", [128, 512], F32) for i in range(2)]
                dqw = DQ("w"); dqx = [DQ("x0"), DQ("x1")]; dqm = DQ("m"); dqo = [DQ("qk0"), DQ("qk1")]; dqv = [DQ("v0"), DQ("v1")]
                WAIT(POOL, cast_t)
                tw = dqw.go(POOL, "dma_start", out=wsb[:], in_=winb[l].ap().rearrange("(k p) n -> p k n", p=128))
                dqg_ = DQ("gn"); tg = dqg_.go(SP, "dma_start", out=gains[:], in_=Wl["gains"][:, :])
                WAIT(DVE, tg)
                DVE.tensor_scalar(out=gains[:, 0:1], in0=gains[:, 0:1], scalar1=0.125, scalar2=None, op0=ALU.mult)
                tgain = dve(DVE.tensor_scalar(out=gains[:, 2:3], in0=gains[:, 2:3], scalar1=0.125, scalar2=None, op0=ALU.mult))
                for i in range(2):
                    tvx = dve(DVE.memset(vx[i][:], 1.0))
                xfree = [None, None]; hbfree = [None, None]; hTfree = [None, None]; ptrfree = None
                pqfree = [None, None]; p2free = [None, None]; pvfree = [None, None]; sqfree = [None, None]; rsfree = [None, None]
                qkfree = [None, None]; vxfree = [None, None]; stfree = [None, None]
                ti = 0; gi = 0; bi = 0; vi = 0
                for s in range(nseg):
                    WAIT(SP, (sDVE, sDVE.n), (sPE, sPE.n))
                    t1 = dqm.go(SP, "dma_start", out=g1b[:], in_=Wl["norm1"][0:1, :].to_broadcast([128, D]))
                    t1 = dqm.go(SP, "dma_start", out=Ab[:], in_=modd[s:s + 1, D:2 * D].to_broadcast([128, D]))
                    t1 = dqm.go(SP, "dma_start", out=Bb[:], in_=modd[s:s + 1, 0:D].to_broadcast([128, D]))
                    WAIT(DVE, t1)
                    tAB = dve(DVE.scalar_tensor_tensor(out=Ab[:], in0=Ab[:], scalar=1.0, in1=g1b[:], op0=ALU.add, op1=ALU.mult))
                    ngroups = (Rc[s] + 3) // 4
                    for g in range(ngroups):
                        tiles = list(range(g * 4, min(g * 4 + 4, Rc[s]))); ntok = len(tiles) * 128; gs = gi % 2; gi += 1
                        thT = []
                        for jt, tc in enumerate(tiles):
                            i = ti % 2; ti += 1
                            WAIT(SP, xfree[i]); tx = dqx[i].go(SP, "dma_start", out=xt[i][:], in_=xsrc[s][tc * 128:(tc + 1) * 128, :])
                            WAIT(DVE, tx, stfree[i], tAB)
                            ta_ = dve(DVE.scalar_tensor_tensor(out=junk[:], in0=xt[i][:], scalar=1.0, in1=xt[i][:], op0=ALU.mult, op1=ALU.mult, accum_out=st[i][:, 0:1]))
                            WAIT(DVE, ta_); tb_ = dve(DVE.tensor_scalar(out=st[i][:, 1:2], in0=st[i][:, 0:1], scalar1=1.0 / D, scalar2=1e-6, op0=ALU.mult, op1=ALU.add))
                            WAIT(ACT, tb_); tc_ = act(ACT.activation(out=st[i][:, 2:3], in_=st[i][:, 1:2], func=AF.Sqrt))
                            WAIT(DVE, tc_); td_ = dve(DVE.reciprocal(out=st[i][:, 3:4], in_=st[i][:, 2:3]))
                            WAIT(DVE, td_); te_ = dve(DVE.scalar_tensor_tensor(out=xt[i][:], in0=xt[i][:], scalar=st[i][:, 3:4], in1=Ab[:], op0=ALU.mult, op1=ALU.mult))
                            WAIT(DVE, te_, hbfree[i]); th = dve(DVE.tensor_tensor(out=hb[i][:], in0=xt[i][:], in1=Bb[:], op=ALU.add))
                            xfree[i] = th; stfree[i] = th
                            WAIT(PE, th, ptrfree)
                            for k in range(8):
                                tt = PE.transpose(ptr[:, k * 128:(k + 1) * 128], hb[i][:, k * 128:(k + 1) * 128], identb[:])
                            tt = pe(tt); hbfree[i] = tt
                            WAIT(DVE, tt, hTfree[gs] if jt == 0 else None)
                            ptrfree = dve(DVE.tensor_copy(out=hT[gs][:, :, jt * 128:(jt + 1) * 128], in_=ptr[:].rearrange("p (k n) -> p k n", k=8)))
                            thT.append(ptrfree)
                        qlo = q2r * 128; qhi = (q2r + Qc[s]) * 128; glo = g * 512; ghi = glo + ntok
                        olo = max(glo, qlo); ohi = min(ghi, qhi)
                        blocks = [("k", b) for b in range(Ls["nkb"])] + ([("q", b) for b in range(Ls["nqb"])] if ohi > olo else [])
                        lastmm = None
                        for (kind, b) in blocks:
                            i = bi % 2; bi += 1
                            if kind == "k":
                                c0 = Ls["kcols"][b]; gcol = Ls["kgain"][b]
                            else:
                                c0 = Ls["qcols"][b]; gcol = Ls["qgain"][b]
                            lhs = lambda k, c0=c0: wsb[:, k, c0:c0 + 128]
                            WAIT(PE, thT, tw, pqfree[i])
                            for k in range(8):
                                mm = PE.matmul(pq[i][:, 0:ntok], lhsT=lhs(k), rhs=hT[gs][:, k, 0:ntok], start=(k == 0), stop=(k == 7))
                            tmm = pe(mm); lastmm = tmm
                            WAIT(ACT, tmm, sqfree[i]); tsq = act(ACT.activation(out=sq[i][:, 0:ntok], in_=pq[i][:, 0:ntok], func=AF.Square))
                            WAIT(PE, tsq, p2free[i]); tss = pe(PE.matmul(p2[i][:, 0:ntok], lhsT=bones[:], rhs=sq[i][:, 0:ntok], start=True, stop=True))
                            sqfree[i] = tss
                            WAIT(ACT, tss, rsfree[i]); tsr = act(ACT.activation(out=rs[i][:, 0:ntok], in_=p2[i][:, 0:ntok], func=AF.Sqrt, bias=1e-6, scale=1.0 / 64))
                            p2free[i] = tsr
                            WAIT(DVE, tsr); trc = dve(DVE.reciprocal(out=rs[i][:, 0:ntok], in_=rs[i][:, 0:ntok]))
                            WAIT(DVE, trc, qkfree[i], tgain)
                            tqk = dve(DVE.scalar_tensor_tensor(out=qk[i][:, 0:ntok], in0=pq[i][:, 0:ntok], scalar=gains[:, gcol:gcol + 1], in1=rs[i][:, 0:ntok], op0=ALU.mult, op1=ALU.mult))
                            pqfree[i] = tqk; rsfree[i] = tqk
                            WAIT(POOL, tqk)
                            for hf in range(2):
                                if kind == "k":
                                    qkfree[i] = dqo[i].go(POOL, "dma_start", out=kT[l][2 * b + hf, :, (Roff[s] * 128 + glo):(Roff[s] * 128 + ghi)], in_=qk[i][hf * 64:hf * 64 + 64, 0:ntok])
                                else:
                                    qkfree[i] = dqo[i].go(POOL, "dma_start", out=qT[l][2 * b + hf, :, (Qoff[s] * 128 + olo - qlo):(Qoff[s] * 128 + ohi - qlo)], in_=qk[i][hf * 64:hf * 64 + 64, olo - glo:ohi - glo])
                        for jt, tc in enumerate(tiles):
                            i = vi % 2; vi += 1
                            WAIT(DVE, vxfree[i], tvx)
                            for (c0, ncol, vh0) in Ls["vsets"]:
                                WAIT(PE, thT, tw, pvfree[i])
                                for k in range(8):
                                    mm = PE.matmul(pv[i][:, 0:ncol], lhsT=hT[gs][:, k, jt * 128:(jt + 1) * 128], rhs=wsb[:, k, c0:c0 + ncol], start=(k == 0), stop=(k == 7))
                                tmm = pe(mm); lastmm = tmm
                                WAIT(DVE, tmm)
                                pvfree[i] = dve(DVE.tensor_copy(out=vx[i][:, vh0:vh0 + ncol // 64, 0:64], in_=pv[i][:, 0:ncol].rearrange("p (h e) -> p h e", e=64)))
                            WAIT(POOL, pvfree[i])
                            r0 = (Roff[s] + tc) * 128
                            vxfree[i] = dqv[i].go(POOL, "dma_start", out=Vd[l][r0:r0 + 128, :], in_=vx[i][:].rearrange("p h e -> p (h e)"))
                        hTfree[gs] = lastmm
                WAIT(POOL, qkfree, vxfree)
            barrier()
            if getattr(cfg, 'stop', None) == (l, 'B'): raise _Stop()

            with contextlib.ExitStack() as bufs_:
                bufs = bufs_
                quads = Ls["quads"]; nquad = len(quads); nko = Ls["nko"]
                if l == 0:
                    bl = l0_bias_list(); bidx = {k: i for i, k in enumerate(bl)}; nbias = NU0
                else:
                    bidx = {("C", qd, d): qd * 7 + d + 3 for qd in range(4) for d in range(-3, 4)}; nbias = 28
                biasr = sb("biasr", [128, nbias, 512], BF16)
                biase = [sb(f"biase{i}", [128, 28, 512], BF16) for i in range(1)] if l == 1 else None; biasefree = None
                wo = sb("wo", [128, nko, D], BF16); kv = sb("kv", [128, NRl[l]], F32); gate1 = sb("gate1", [128, D], F32)
                esink = sb("esink", [128, 8], F32)
                dmin = min(u[1] for q in quads for u in q["units"]); dmax = max(u[1] for q in quads for u in q["units"]); nwin = dmax - dmin + 1
                nqh = 2 * Ls["nqb"]; wgr = Ls["wgroups"]
                qs = [sb(f"qs{i}", [64, nqh, 128], BF16) for i in range(2)]
                ks = [[sb(f"ks{i}_{g}", [64, h1 - h0, (2 * r + 1) * 128], BF16) for (h0, h1, r) in wgr] for i in range(2)]
                vs = [[sb(f"vs{i}_{g}", [128, 2 * r + 1, (h1 - h0) * 65], BF16) for (h0, h1, r) in wgr] for i in range(2)]
                def wg_of(kh):
                    for g, (h0, h1, r) in enumerate(wgr):
                        if h0 <= kh < h1: return g, kh - h0, r
                    raise KeyError(kh)
                xq = [sb(f"xq{i}", [128, D], F32) for i in range(2)]
                Eb = [sb(f"E{i}", [128, 512], BF16) for i in range(3)]
                attn = sb("attn", [128, nko * 128], BF16); attnT = sb("attnT", [128, nko, 128], BF16); rden = sb("rden", [128, 16], F32)
                x1t = [sb(f"x1t{i}", [128, D], F32) for i in range(2)]
                pS = [ps(f"pS{i}", [128, 512], F32) for i in range(2)]; pO_ = [ps(f"pO{i}", [128, 512], F32) for i in range(nquad)]; pO = [p_[:, 0:260].rearrange("p (h e) -> p h e", e=65) for p_ in pO_]
                pT = ps("pT", [128, D], BF16); pW = ps("pW", [128, 512], F32)
                dqb = DQ("b"); dqk = [DQ("k0"), DQ("k1")]; dqe = [DQ("e0"), DQ("e1")]; dqx1 = [DQ("x10"), DQ("x11")]
                WAIT(POOL, cast_t)
                t = dqb.go(POOL, "dma_start", out=wo[:], in_=woutb[l].ap().rearrange("(k p) n -> p k n", p=128))
                bsrc = bias0_in if l == 0 else bias1_in[0]
                for u in range(nbias):
                    t = dqb.go(POOL, "dma_start", out=biasr[:, u, :], in_=bsrc[u, :, :])
                dqb2 = DQ("b2"); dqg1 = DQ("g1")
                t2 = dqb2.go(SP, "dma_start", out=kv[:], in_=kvald[:, :])
                t2 = dqb2.go(SP, "dma_start", out=esink[:], in_=sink_in[0:1, :].to_broadcast([128, 8]))
                tres = [t, t2]
                WAIT(ACT, t2); tsink = act(ACT.activation(out=esink[:], in_=esink[:], func=AF.Exp))
                slotfree = [None, None]; xqfree = [None, None]; Sfree = [None, None]; Efree = [None, None, None]
                dqzf = DQ("zf"); zf_rows = list(range(0, E * C, 128)); zf_per = -(-len(zf_rows) // sum(Qc)); zf_i = 0; WAIT(POOL, tztp)
                Ofree = [None] * nquad; pTfree = None; attnfree = None; attnTfree = None; pWfree = None; x1free = [None, None]; rdenfree = None
                qi = 0; ui = 0; ei = 0
                for s in range(nseg):
                    WAIT(SP, (sDVE, sDVE.n)); tg1 = dqg1.go(SP, "dma_start", out=gate1[:], in_=modd[s:s + 1, 2 * D:3 * D].to_broadcast([128, D]))
                    for qc in range(min(Qc[s], getattr(cfg, 'dbg', {}).get('c_blocks', 10**9))):
                        i = qi % 2; qi += 1
                        rc = qc + q2r
                        edge = None
                        if l == 1:
                            if qc < 2: edge = 1 + 4 * s + qc
                            elif qc >= Qc[s] - 2: edge = 1 + 4 * s + 2 + (qc - (Qc[s] - 2))
                        WAIT(SP, slotfree[i], xqfree[i])
                        qcol = (Qoff[s] + qc) * 128
                        dqk[i].go(SP, "dma_start", out=qs[i][:], in_=qT[l][:, :, qcol:qcol + 128].rearrange("b p t -> p b t"))
                        los = []
                        for g, (h0, h1, r) in enumerate(wgr):
                            lo = max(0, rc - r); hi = min(Rc[s] - 1, rc + r); nld = hi - lo + 1; los.append(lo)
                            kcol = (Roff[s] + lo) * 128
                            dqk[i].go(SP, "dma_start", out=ks[i][g][:, :, 0:nld * 128], in_=kT[l][h0:h1, :, kcol:kcol + nld * 128].rearrange("b p t -> p b t"))
                            dqk[i].go(SP, "dma_start", out=vs[i][g][:, 0:nld, :], in_=Vd[l][kcol:kcol + nld * 128, h0 * 65:h1 * 65].rearrange("(c p) f -> p c f", p=128))
                        xrow = (rc * 128) if l == 0 else ((cfg.Q0off[s] + rc) * 128)
                        xs_ = xe[s] if l == 0 else x2d
                        tld = dqk[i].go(SP, "dma_start", out=xq[i][:], in_=xs_[xrow:xrow + 128, :])
                        if edge is not None:
                            eb = 0
                            WAIT(POOL, slotfree[i], biasefree)
                            for u in range(28):
                                tedge = dqe[eb].go(POOL, "dma_start", out=biase[eb][:, u, :], in_=bias1_in[edge, u, :, :])
                        stage = getattr(cfg, 'dbg', {}).get('c_stage', 9)
                        if stage < 1: continue
                        for qd, quad in enumerate(quads):
                            units = quad["units"]
                            if l == 1:
                                units = [u for u in units if (edge is not None) or (-2 <= u[1] <= 2)]
                            def emit_pv(pv):
                                un_, e3_, cidx_, wg_, heads_, tE_ = pv
                                WAIT(PE, tE_, Ofree[qd] if un_ == 0 else None)
                                for h, (qh, kh) in enumerate(heads_):
                                    _, khl, _ = wg_of(kh)
                                    mm_ = PE.matmul(pO[qd][:, h, :], lhsT=Eb[e3_][:, h * 128:(h + 1) * 128], rhs=vs[i][wg_][:, cidx_, khl * 65:(khl + 1) * 65],
                                                    start=(un_ == 0 and h == 0), stop=(un_ == len(units) - 1), skip_group_check=True)
                                Efree[e3_] = pe(mm_); return Efree[e3_]
                            prev = None; tlastpv = None
                            for un, (heads, d, bkey) in enumerate(units):
                                si = ui % 2; e3 = ui % 3; ui += 1
                                wg, _, _ = wg_of(heads[0][1]); lo = los[wg]
                                cidx = min(max(rc + d, 0), Rc[s] - 1) - lo
                                btile = biasr[:, bidx[bkey], :] if edge is None else biase[eb][:, bidx[bkey], :]
                                WAIT(PE, tld, tres, Sfree[si], tedge if edge is not None else None)
                                PE.matmul(pS[si][:], lhsT=identb[:], rhs=btile, start=True, stop=False)
                                for h, (qh, kh) in enumerate(heads):
                                    _, khl, _ = wg_of(kh)
                                    mm = PE.matmul(pS[si][:, h * 128:(h + 1) * 128], lhsT=ks[i][wg][:, khl, cidx * 128:(cidx + 1) * 128],
                                                   rhs=qs[i][:, qh, :], start=False, stop=(h == 3))
                                tS = pe(mm)
                                if edge is not None: biasefree = tS
                                if stage < 2: continue
                                kcolv = Roff[s] + lo + cidx
                                WAIT(ACT, tS, Efree[e3])
                                tE = act(ACT.activation(out=Eb[e3][:], in_=pS[si][:], func=AF.Exp, bias=kv[:, kcolv:kcolv + 1], scale=1.0))
                                Sfree[si] = tE
                                if prev is not None: tlastpv = emit_pv(prev)
                                prev = (un, e3, cidx, wg, heads, tE)
                            if prev is not None: tlastpv = emit_pv(prev)
                            if stage < 4: continue
                            tO = tlastpv
                            WAIT(DVE, tO, rdenfree, tsink)
                            if quad["sink"]:
                                td1 = dve(DVE.tensor_tensor(out=rden[:, qd * 4:qd * 4 + 4], in0=pO[qd][:, :, 64], in1=esink[:, qd * 4:qd * 4 + 4], op=ALU.add))
                            else:
                                td1 = dve(DVE.tensor_copy(out=rden[:, qd * 4:qd * 4 + 4], in_=pO[qd][:, :, 64]))
                            WAIT(DVE, td1); td2 = dve(DVE.reciprocal(out=rden[:, qd * 4:qd * 4 + 4], in_=rden[:, qd * 4:qd * 4 + 4]))
                            WAIT(DVE, td2, attnfree if qd == 0 else None)
                            tat = dve(DVE.tensor_tensor(out=attn[:, qd * 256:(qd + 1) * 256].rearrange("p (h e) -> p h e", e=64), in0=pO[qd][:, :, 0:64],
                                                        in1=rden[:, qd * 4:qd * 4 + 4].unsqueeze(2).to_broadcast([128, 4, 64]), op=ALU.mult))
                            Ofree[qd] = tat
                        if stage < 5: continue
                        slotfree[i] = tlastpv; rdenfree = tat
                        WAIT(PE, tat, pTfree)
                        for k in range(nko):
                            tt = PE.transpose(pT[:, k * 128:(k + 1) * 128], attn[:, k * 128:(k + 1) * 128], identb[:])
                        tt = pe(tt); attnfree = tt
                        WAIT(DVE, tt, attnTfree)
                        tcp = dve(DVE.tensor_copy(out=attnT[:], in_=pT[:, 0:nko * 128].rearrange("p (k n) -> p k n", k=nko))); pTfree = tcp
                        xi = qi % 2
                        WAIT(DVE, x1free[xi], tg1)
                        for hh in range(2):
                            WAIT(PE, tcp, pWfree)
                            for k in range(nko):
                                mm = PE.matmul(pW[:], lhsT=attnT[:, k, :], rhs=wo[:, k, hh * 512:(hh + 1) * 512], start=(k == 0), stop=(k == nko - 1))
                            tw_ = pe(mm)
                            WAIT(DVE, tw_)
                            tm1 = dve(DVE.tensor_tensor(out=x1t[xi][:, hh * 512:(hh + 1) * 512], in0=pW[:], in1=gate1[:, hh * 512:(hh + 1) * 512], op=ALU.mult)); pWfree = tm1
                            WAIT(DVE, tm1)
                            tx1 = dve(DVE.tensor_tensor(out=x1t[xi][:, hh * 512:(hh + 1) * 512], in0=x1t[xi][:, hh * 512:(hh + 1) * 512], in1=xq[i][:, hh * 512:(hh + 1) * 512], op=ALU.add))
                        attnTfree = tw_; xqfree[i] = tx1
                        for _ in range(zf_per):
                            if zf_i < len(zf_rows):
                                tzf[0] = dqzf.go(POOL, "dma_start", out=Xg[l][zf_rows[zf_i]:zf_rows[zf_i] + 128, :], in_=ztp[:]); zf_i += 1
                        WAIT(POOL, tx1)
                        r0 = (Qoff[s] + qc) * 128
                        x1free[xi] = dqx1[xi].go(POOL, "dma_start", out=x1d[l][r0:r0 + 128, :], in_=x1t[xi][:])
                while zf_i < len(zf_rows):
                    tzf[0] = dqzf.go(POOL, "dma_start", out=Xg[l][zf_rows[zf_i]:zf_rows[zf_i] + 128, :], in_=ztp[:]); zf_i += 1
                WAIT(POOL, x1free)
            barrier()
            if getattr(cfg, 'stop', None) == (l, 'C'): raise _Stop()

            NT = NQl[l]; bc_reg = POOL.to_reg(E * C - 1); bch_reg = POOL.to_reg(EH * C - 1)
            with contextlib.ExitStack() as bufs_:
                bufs = bufs_
                g2b = sb("g2b", [128, D], F32); A2 = sb("A2", [128, D], F32); B2 = sb("B2", [128, D], F32)
                wr = sb("wr", [128, 8, E], F32); br = sb("br", [1, E], F32); ecol = sb("ecol", [128, E], F32); tv = sb("tv", [128, cfg.NQ0], F32)
                cnt = sb("cnt", [128, E], F32)
                xt = [sb(f"dxt{i}", [128, D], F32) for i in range(2)]; junk = sb("djunk", [128, D], BF16); st = [sb(f"dst{i}", [128, 4], F32) for i in range(2)]
                h2 = [sb(f"h2{i}", [128, D], F32) for i in range(2)]; h2b = [sb(f"h2b{i}", [128, D], BF16) for i in range(2)]
                h2T = sb("h2T", [128, 8, 128], F32)
                sm = [sb(f"sm{i}", [128, 8, E], F32) for i in range(2)]
                m8 = [sb(f"m8{i}", [128, 4, 8], F32) for i in range(2)]
                Mb = [sb(f"Mb{i}", [128, E], BF16) for i in range(2)]
                zt = sb("zt", [128, D], BF16)
                pTa = ps("dpTa", [128, 512], F32); pTb = ps("dpTb", [128, 512], F32); pL_ = ps("pL", [128, 512], F32); pL = pL_[:, 0:E]; pR_ = ps("pR", [128, 512], F32); pR = pR_[:, 0:2 * E].rearrange("p (a e) -> p a e", a=2)
                dql = DQ("dl"); dql2 = DQ("dl2"); dqx = [DQ("dx0"), DQ("dx1")]; dqsc = [DQ("sc0"), DQ("sc1")]; dqz = DQ("z")
                t = dql.go(SP, "dma_start", out=wr[:], in_=Wl["w_router"].ap().rearrange("(k p) e -> p k e", p=128))
                t = dql.go(SP, "dma_start", out=br[:], in_=Wl["b_router"][:, :])
                t = dql.go(SP, "dma_start", out=ecol[:], in_=ecol_in[l][:, :])
                if l == 0: t = dql.go(SP, "dma_start", out=tv[:], in_=tval0[:, :])
                tld0 = t
                tz = dve(DVE.memset(zt[:], 0.0)); tcnt = dve(DVE.memset(cnt[:], 0.0))
                if l == 1: tcnt = dve(DVE.memset(tv[:], 1.0))
                WAIT(POOL, tzf[0])
                xfree = [None, None]; stfree = [None, None]; h2free = [None, None]; h2bfree = [None, None]; smfree = [None, None]
                pTfree = None; h2Tfree = None; pLfree = None; pRfree = None; cntT = tcnt
                ti = 0
                for s in range(nseg):
                    WAIT(SP, (sDVE, sDVE.n))
                    t1 = dql2.go(SP, "dma_start", out=g2b[:], in_=Wl["norm2"][0:1, :].to_broadcast([128, D]))
                    t1 = dql2.go(SP, "dma_start", out=A2[:], in_=modd[s:s + 1, 4 * D:5 * D].to_broadcast([128, D]))
                    t1 = dql2.go(SP, "dma_start", out=B2[:], in_=modd[s:s + 1, 3 * D:4 * D].to_broadcast([128, D]))
                    WAIT(DVE, t1); tAB = dve(DVE.scalar_tensor_tensor(out=A2[:], in0=A2[:], scalar=1.0, in1=g2b[:], op0=ALU.add, op1=ALU.mult))
                    for qc in range(Qc[s]):
                        i = ti % 2; ti += 1; tg = Qoff[s] + qc
                        WAIT(SP, xfree[i]); tx = dqx[i].go(SP, "dma_start", out=xt[i][:], in_=x1d[l][tg * 128:(tg + 1) * 128, :])
                        WAIT(DVE, tx, stfree[i], tAB)
                        ta_ = dve(DVE.scalar_tensor_tensor(out=junk[:], in0=xt[i][:], scalar=1.0, in1=xt[i][:], op0=ALU.mult, op1=ALU.mult, accum_out=st[i][:, 0:1]))
                        WAIT(DVE, ta_); tb_ = dve(DVE.tensor_scalar(out=st[i][:, 1:2], in0=st[i][:, 0:1], scalar1=1.0 / D, scalar2=1e-6, op0=ALU.mult, op1=ALU.add))
                        WAIT(ACT, tb_); tc_ = act(ACT.activation(out=st[i][:, 2:3], in_=st[i][:, 1:2], func=AF.Sqrt))
                        WAIT(DVE, tc_); td_ = dve(DVE.reciprocal(out=st[i][:, 3:4], in_=st[i][:, 2:3]))
                        WAIT(DVE, td_); te_ = dve(DVE.scalar_tensor_tensor(out=xt[i][:], in0=xt[i][:], scalar=st[i][:, 3:4], in1=A2[:], op0=ALU.mult, op1=ALU.mult))
                        WAIT(DVE, te_, h2free[i]); th = dve(DVE.tensor_tensor(out=h2[i][:], in0=xt[i][:], in1=B2[:], op=ALU.add))
                        xfree[i] = th; stfree[i] = th
                        WAIT(ACT, th, h2bfree[i]); thb = act(ACT.copy(out=h2b[i][:], in_=h2[i][:]))
                        WAIT(PE, th, pTfree)
                        for k in range(8):
                            tt = PE.transpose((pTa if k < 4 else pTb)[:, (k % 4) * 128:(k % 4 + 1) * 128], h2[i][:, k * 128:(k + 1) * 128], ident[:])
                        tt = pe(tt)
                        WAIT(DVE, tt, h2Tfree)
                        DVE.tensor_copy(out=h2T[:, 0:4, :], in_=pTa[:].rearrange("p (k n) -> p k n", k=4))
                        tcp = dve(DVE.tensor_copy(out=h2T[:, 4:8, :], in_=pTb[:].rearrange("p (k n) -> p k n", k=4))); pTfree = tcp
                        WAIT(PE, tcp, tld0, pLfree, tk_const)
                        for k in range(8):
                            PE.matmul(pL, lhsT=h2T[:, k, :], rhs=wr[:, k, :], start=(k == 0), stop=False)
                        tlg = pe(PE.matmul(pL, lhsT=ones1[:], rhs=br[:], start=False, stop=True)); h2Tfree = tlg
                        S = sm[i]; M8 = m8[i]
                        WAIT(DVE, tlg, smfree[i], tld0)
                        t_ = dve(DVE.tensor_copy(out=S[:, 0, :], in_=pL)); pLfree = t_
                        WAIT(DVE, t_); t_ = dve(DVE.max(out=M8[:, 0, :], in_=S[:, 0, :]))
                        WAIT(DVE, t_); tM = dve(DVE.tensor_scalar(out=S[:, 1, :], in0=S[:, 0, :], scalar1=M8[:, 0, 3:4], scalar2=None, op0=ALU.is_ge))
                        tn = dve(DVE.tensor_scalar(out=M8[:, 2, 0:1], in0=M8[:, 0, 0:1], scalar1=-1.0, scalar2=None, op0=ALU.mult))
                        WAIT(ACT, tn); tex = act(ACT.activation(out=S[:, 2, :], in_=S[:, 0, :], func=AF.Exp, bias=M8[:, 2, 0:1], scale=1.0))
                        WAIT(DVE, tex, tM); t_ = dve(DVE.scalar_tensor_tensor(out=S[:, 3, :], in0=S[:, 2, :], scalar=1.0, in1=S[:, 1, :], op0=ALU.mult, op1=ALU.mult, accum_out=M8[:, 2, 1:2]))
                        WAIT(DVE, t_); t_ = dve(DVE.reciprocal(out=M8[:, 2, 2:3], in_=M8[:, 2, 1:2]))
                        WAIT(DVE, t_); tG = dve(DVE.tensor_scalar(out=S[:, 4, :], in0=S[:, 3, :], scalar1=M8[:, 2, 2:3], scalar2=None, op0=ALU.mult))
                        tMv = dve(DVE.tensor_scalar(out=S[:, 7, :], in0=S[:, 1, :], scalar1=tv[:, tg:tg + 1], scalar2=None, op0=ALU.mult))
                        WAIT(DVE, tMv); tMb = dve(DVE.tensor_copy(out=Mb[i][:], in_=S[:, 7, :]))
                        WAIT(PE, tMb, pRfree)
                        PE.matmul(pR[:, 0, :], lhsT=ustrict[:], rhs=Mb[i][:], start=True, stop=True)
                        tR = pe(PE.matmul(pR[:, 1, :], lhsT=onesb[:], rhs=Mb[i][:], start=True, stop=True, skip_group_check=True))
                        WAIT(DVE, tR, cntT)
                        t_ = dve(DVE.tensor_tensor(out=S[:, 5, :], in0=pR[:, 0, :], in1=cnt[:], op=ALU.add))
                        WAIT(DVE, t_); cntT = dve(DVE.tensor_tensor(out=cnt[:], in0=pR[:, 1, :], in1=cnt[:], op=ALU.add)); pRfree = cntT
                        WAIT(DVE, t_); tok_ = dve(DVE.tensor_scalar(out=S[:, 6, :], in0=S[:, 5, :], scalar1=float(C), scalar2=None, op0=ALU.is_lt))
                        WAIT(DVE, tok_, tMb); tok_ = dve(DVE.tensor_tensor(out=S[:, 7, :], in0=S[:, 7, :], in1=S[:, 6, :], op=ALU.mult))
                        WAIT(DVE, tok_); t_ = dve(DVE.tensor_tensor(out=S[:, 5, :], in0=S[:, 5, :], in1=ecol[:], op=ALU.add))
                        WAIT(DVE, t_); tsv = dve(DVE.tensor_tensor(out=S[:, 5, :], in0=S[:, 5, :], in1=S[:, 7, :], op=ALU.mult))
                        WAIT(DVE, tsv); t_ = dve(DVE.max(out=M8[:, 1, :], in_=S[:, 5, :]))
                        WAIT(DVE, t_)
                        t_ = dve(DVE.tensor_scalar(out=M8[:, 3, 0:4], in0=M8[:, 1, 0:4], scalar1=0.5, scalar2=float(E * C + 8), op0=ALU.is_lt, op1=ALU.mult))
                        WAIT(DVE, t_); t_ = dve(DVE.scalar_tensor_tensor(out=M8[:, 3, 0:4], in0=M8[:, 1, 0:4], scalar=-1.0, in1=M8[:, 3, 0:4], op0=ALU.add, op1=ALU.add))
                        WAIT(DVE, t_); tidx = dve(DVE.tensor_copy(out=S4[:, tg, :], in_=M8[:, 3, 0:4]))
                        tflag = dve(DVE.tensor_scalar(out=M8[:, 3, 4:8], in0=M8[:, 3, 0:4], scalar1=float(EH * C), scalar2=None, op0=ALU.is_lt))
                        tlow = dve(DVE.tensor_scalar(out=M8[:, 2, 4:8], in0=M8[:, 3, 0:4], scalar1=float(EH * C), scalar2=float(2 * E * C), op0=ALU.is_lt, op1=ALU.mult))
                        WAIT(DVE, tlow); tlow = dve(DVE.scalar_tensor_tensor(out=M8[:, 2, 4:8], in0=M8[:, 3, 0:4], scalar=-float(EH * C), in1=M8[:, 2, 4:8], op0=ALU.add, op1=ALU.add))
                        WAIT(DVE, tlow); tidxb = dve(DVE.tensor_copy(out=S4b[:, tg, :], in_=M8[:, 2, 4:8]))
                        tgk = None
                        for k in range(4):
                            WAIT(DVE, tsv, tG, tgk)
                            t_ = dve(DVE.tensor_scalar(out=S[:, 6, :], in0=S[:, 5, :], scalar1=M8[:, 1, k:k + 1], scalar2=None, op0=ALU.is_equal))
                            WAIT(DVE, t_); tgk = dve(DVE.scalar_tensor_tensor(out=S[:, 7, :] if False else S[:, 6, :], in0=S[:, 6, :], scalar=1.0, in1=S[:, 4, :], op0=ALU.mult, op1=ALU.mult, accum_out=G4[:, tg, k:k + 1]))
                        WAIT(DVE, tgk); tgv = dve(DVE.tensor_scalar(out=G4B[:, tg, :], in0=G4[:, tg, :], scalar1=tv[:, tg:tg + 1], scalar2=None, op0=ALU.mult))
                        WAIT(DVE, tgv, tflag); tgv = dve(DVE.tensor_tensor(out=G4[:, tg, :], in0=G4B[:, tg, :], in1=M8[:, 3, 4:8], op=ALU.mult))
                        WAIT(DVE, tgv); tgv = dve(DVE.tensor_tensor(out=G4B[:, tg, :], in0=G4B[:, tg, :], in1=G4[:, tg, :], op=ALU.subtract))
                        smfree[i] = [tgv, tidxb]; h2free[i] = tlg
                        WAIT(POOL, tidx, thb)
                        for k in range(4):
                            tsc = dqsc[i].go(POOL, "indirect_dma_start", out=Xg[l][:, :], out_offset=bass.IndirectOffsetOnAxis(ap=S4[:, tg, k:k + 1], axis=0),
                                                                  in_=h2b[i][:, :], in_offset=None, bounds_check=bc_reg, oob_is_err=False)
                        h2bfree[i] = tsc
                WAIT(POOL, h2bfree)
                WAIT(DVE, cntT); dve(DVE.tensor_copy(out=cnt_i[0:1, :], in_=cnt[0:1, :]))
                if idx_out is not None and l == 0:
                    dqix = DQ("ix"); WAIT(POOL, (sDVE, sDVE.n))
                    dqix.go(POOL, "dma_start", out=idx_out[0, :, :], in_=S4[:].rearrange("p t k -> p (t k)"))
                    tix = dqix.go(POOL, "dma_start", out=idx_out[1, :, :], in_=S4b[:].rearrange("p t k -> p (t k)")); WAIT(POOL, tix)
                if cnt_out is not None:
                    dqcn = DQ("cn"); WAIT(POOL, cntT)
                    tcn = dqcn.go(POOL, "dma_start", out=cnt_out[l:l + 1, :], in_=cnt[0:1, :]); WAIT(POOL, tcn)
            barrier()
            if getattr(cfg, 'stop', None) == (l, 'D'): raise _Stop()

            with contextlib.ExitStack() as bufs_:
                bufs = bufs_
                wgu = [sb(f"wgu{i}", [128, 8, 2048], BF16) for i in range(2)]; wd = [sb(f"wd{i}", [128, 8, D], BF16) for i in range(2)]
                bgu = [sb(f"bgu{i}", [128, 16], F32) for i in range(2)]; bd = [sb(f"bd{i}", [1, D], F32) for i in range(2)]
                xg = [sb(f"xg{i}", [128, 4, D], BF16) for i in range(2)]; xT = sb("xT", [128, 8, 512], BF16); aT = sb("aT", [128, 8, 512], BF16)
                gt = [sb(f"gt{i}", [128, 512], F32) for i in range(2)]; sg = [sb(f"sg{i}", [128, 512], F32) for i in range(2)]; ut = [sb(f"ut{i}", [128, 512], F32) for i in range(2)]
                yo = [sb(f"yo{i}", [128, D], F32) for i in range(2)]; yzero = sb("yzero", [128, D], F32)
                tyz = dve(DVE.memset(yzero[:], 0.0)); WAIT(POOL, tyz)
                pT = ps("epT", [128, D], BF16); pG = [ps(f"pG{i}", [128, 512], F32) for i in range(2)]; pU = [ps(f"pU{i}", [128, 512], F32) for i in range(2)]
                pY = [ps(f"pY{i}", [128, 512], F32) for i in range(2)]
                dqw_ = [DQ("ew0"), DQ("ew1")]; dqx = [DQ("ex0"), DQ("ex1")]; dqy = [DQ("ey0"), DQ("ey1")]
                wfree = [None, None]; xgfree = [None, None]; pTfree = None; xTfree = None; aTfree = None
                pGfree = [None, None]; pUfree = [None, None]; gtfree = [None, None]; sgfree = [None, None]; utfree = [None, None]
                pYfree = [None, None]; yofree = [None, None]
                chunks = [(c0, min(512, C - c0)) for c0 in range(0, C, 512)]
                stE = dict(xi=0, fi=0, yi=0, yoi=0, pTfree=None, xTfree=None, aTfree=None)
                def chunk_body(PE, ACT, DVE, POOL, SP, e, wi, c0, nsl, twl):
                    nb = nsl // 128; j = stE["xi"] % 2; stE["xi"] += 1
                    WAIT(SP, xgfree[j])
                    r0 = e * C + c0
                    txg = dqx[j].go(SP, "dma_start", out=xg[j][:, 0:nb, :], in_=Xg[l][r0:r0 + nsl, :].rearrange("(b p) d -> p b d", p=128))
                    for b in range(nb):
                        WAIT(PE, txg, stE["pTfree"])
                        for k in range(8):
                            tt = PE.transpose(pT[:, k * 128:(k + 1) * 128], xg[j][:, b, k * 128:(k + 1) * 128], identb[:])
                        tt = pe(tt)
                        WAIT(DVE, tt, stE["xTfree"] if b == 0 else None)
                        stE["pTfree"] = dve(DVE.tensor_copy(out=xT[:, :, b * 128:(b + 1) * 128], in_=pT[:].rearrange("p (k n) -> p k n", k=8)))
                    xgfree[j] = tt; txT = stE["pTfree"]
                    for fp in range(8):
                        f = stE["fi"] % 2; stE["fi"] += 1
                        WAIT(PE, txT, twl, pGfree[f], pUfree[f])
                        for k in range(8):
                            mm = PE.matmul(pG[f][:, 0:nsl], lhsT=wgu[wi][:, k, fp * 128:(fp + 1) * 128], rhs=xT[:, k, 0:nsl], start=(k == 0), stop=(k == 7))
                        tg_ = pe(mm)
                        for k in range(8):
                            mm = PE.matmul(pU[f][:, 0:nsl], lhsT=wgu[wi][:, k, 1024 + fp * 128:1024 + (fp + 1) * 128], rhs=xT[:, k, 0:nsl], start=(k == 0), stop=(k == 7))
                        tu_ = pe(mm)
                        WAIT(DVE, tg_, gtfree[f])
                        t1 = dve(DVE.tensor_scalar(out=gt[f][:, 0:nsl], in0=pG[f][:, 0:nsl], scalar1=bgu[wi][:, fp:fp + 1], scalar2=7.0, op0=ALU.add, op1=ALU.min)); pGfree[f] = t1
                        WAIT(ACT, t1, sgfree[f]); t2 = act(ACT.activation(out=sg[f][:, 0:nsl], in_=gt[f][:, 0:nsl], func=AF.Sigmoid, scale=1.702))
                        WAIT(DVE, tu_, utfree[f])
                        t3 = dve(DVE.tensor_scalar(out=ut[f][:, 0:nsl], in0=pU[f][:, 0:nsl], scalar1=bgu[wi][:, 8 + fp:9 + fp], scalar2=7.0, op0=ALU.add, op1=ALU.min)); pUfree[f] = t3
                        WAIT(DVE, t3); t4 = dve(DVE.tensor_scalar(out=ut[f][:, 0:nsl], in0=ut[f][:, 0:nsl], scalar1=-7.0, scalar2=1.0, op0=ALU.max, op1=ALU.add))
                        WAIT(DVE, t4, t2); t5 = dve(DVE.tensor_tensor(out=gt[f][:, 0:nsl], in0=gt[f][:, 0:nsl], in1=sg[f][:, 0:nsl], op=ALU.mult)); sgfree[f] = t5
                        WAIT(DVE, t5, stE["aTfree"] if fp == 0 else None)
                        t6 = dve(DVE.tensor_tensor(out=aT[:, fp, 0:nsl], in0=gt[f][:, 0:nsl], in1=ut[f][:, 0:nsl], op=ALU.mult)); gtfree[f] = t6; utfree[f] = t6
                    stE["xTfree"] = tu_
                    for b in range(nb):
                        yb = stE["yoi"] % 2; stE["yoi"] += 1
                        for hh in range(2):
                            y = stE["yi"] % 2; stE["yi"] += 1
                            WAIT(PE, t6, pYfree[y])
                            for k in range(8):
                                PE.matmul(pY[y][:], lhsT=aT[:, k, b * 128:(b + 1) * 128], rhs=wd[wi][:, k, hh * 512:(hh + 1) * 512], start=(k == 0), stop=False)
                            ty = pe(PE.matmul(pY[y][:], lhsT=ones1[:], rhs=bd[wi][:, hh * 512:(hh + 1) * 512], start=False, stop=True))
                            WAIT(ACT, ty, yofree[yb]); pYfree[y] = act(ACT.copy(out=yo[yb][:, hh * 512:(hh + 1) * 512], in_=pY[y][:]))
                        WAIT(POOL, pYfree[y])
                        rr = (e % EH) * C + c0 + b * 128
                        yofree[yb] = dqy[yb].go(POOL, "dma_start", out=YgH[l][e // EH][rr:rr + 128, :], in_=yo[yb][:])
                    stE["aTfree"] = ty
                    return ty
                dyn = getattr(cfg, "dyn", True)
                for e in range(E):
                    wi = e % 2
                    WAIT(POOL, wfree[wi])
                    for k in range(8):
                        dqw_[wi].go(POOL, "dma_start", out=wgu[wi][:, k, :], in_=Wl["w_gu"][e, k * 128:(k + 1) * 128, :], max_dma_last_dim=4096)
                        dqw_[wi].go(POOL, "dma_start", out=wd[wi][:, k, :], in_=Wl["w_d"][e, k * 128:(k + 1) * 128, :], max_dma_last_dim=4096)
                    dqw_[wi].go(POOL, "dma_start", out=bgu[wi][:], in_=Wl["b_gu"][e:e + 1, :].rearrange("o (f p) -> p (o f)", p=128), allow_slow_non_contiguous=True)
                    twl = dqw_[wi].go(POOL, "dma_start", out=bd[wi][:], in_=Wl["b_d"][e:e + 1, :])
                    for (c0, nsl) in chunks:
                        if c0 == 0 or not dyn:
                            ty = chunk_body(PE, ACT, DVE, POOL, SP, e, wi, c0, nsl, twl)
                        else:
                            ty = guarded(c0 + 1, cnt_i[0:1, e:e + 1], lambda PE_, ACT_, DVE_, POOL_, SP_: chunk_body(PE_, ACT_, DVE_, POOL_, SP_, e, wi, c0, nsl, twl), zero_src=yzero[:])
                    wfree[wi] = ty
                WAIT(POOL, yofree)
            barrier()
            if getattr(cfg, 'stop', None) == (l, 'E'): raise _Stop()

            with contextlib.ExitStack() as bufs_:
                bufs = bufs_
                gate2 = sb("gate2", [128, D], F32)
                yk = [sb(f"yk{i}", [128, 4, D], F32) for i in range(2)]; ykB = [sb(f"ykB{i}", [128, 4, D], F32) for i in range(2)]; xt = [sb(f"fx{i}", [128, D], F32) for i in range(2)]
                acc = [sb(f"acc{i}", [128, D], F32) for i in range(2)]
                dqg = [DQ("g0"), DQ("g1")]; dqx = [DQ("fx0"), DQ("fx1")]; dqo_ = [DQ("fo0"), DQ("fo1")]; dql = DQ("fl")
                for i in range(2):
                    DVE.memset(ykB[i][:], 0.0); tms = dve(DVE.memset(yk[i][:], 0.0))
                ykfree = [tms, tms]; xfree = [None, None]; accfree = [None, None]
                ti = 0
                for s in range(nseg):
                    WAIT(SP, (sDVE, sDVE.n)); tg2 = dql.go(SP, "dma_start", out=gate2[:], in_=modd[s:s + 1, 5 * D:6 * D].to_broadcast([128, D]))
                    for qc in range(Qc[s]):
                        i = ti % 2; ti += 1; tg = Qoff[s] + qc
                        WAIT(POOL, ykfree[i])
                        for hh, ixt, dst_ in ((0, S4, yk[i]), (1, S4b, ykB[i])):
                            for k in range(4):
                                tgt = dqg[i].go(POOL, "indirect_dma_start", out=dst_[:, k, :], out_offset=None, in_=YgH[l][hh][:, :],
                                                in_offset=bass.IndirectOffsetOnAxis(ap=ixt[:, tg, k:k + 1], axis=0), bounds_check=bch_reg, oob_is_err=False)
                        WAIT(SP, xfree[i]); tx = dqx[i].go(SP, "dma_start", out=xt[i][:], in_=x1d[l][tg * 128:(tg + 1) * 128, :])
                        WAIT(DVE, tgt, accfree[i], tg2)
                        t_ = dve(DVE.tensor_scalar(out=acc[i][:], in0=yk[i][:, 0, :], scalar1=G4[:, tg, 0:1], scalar2=None, op0=ALU.mult))
                        for k in range(1, 4):
                            WAIT(DVE, t_); t_ = dve(DVE.scalar_tensor_tensor(out=acc[i][:], in0=yk[i][:, k, :], scalar=G4[:, tg, k:k + 1], in1=acc[i][:], op0=ALU.mult, op1=ALU.add))
                        for k in range(4):
                            WAIT(DVE, t_); t_ = dve(DVE.scalar_tensor_tensor(out=acc[i][:], in0=ykB[i][:, k, :], scalar=G4B[:, tg, k:k + 1], in1=acc[i][:], op0=ALU.mult, op1=ALU.add))
                        ykfree[i] = t_
                        WAIT(DVE, t_); t_ = dve(DVE.tensor_tensor(out=acc[i][:], in0=acc[i][:], in1=gate2[:], op=ALU.mult))
                        WAIT(DVE, t_, tx); to = dve(DVE.tensor_tensor(out=acc[i][:], in0=acc[i][:], in1=xt[i][:], op=ALU.add)); xfree[i] = to
                        WAIT(POOL, to)
                        if l == 0:
                            accfree[i] = dqo_[i].go(POOL, "dma_start", out=x2d[tg * 128:(tg + 1) * 128, :], in_=acc[i][:])
                        else:
                            accfree[i] = dqo_[i].go(POOL, "dma_start", out=youts[s][qc * 128:(qc + 1) * 128, :], in_=acc[i][:])
                WAIT(POOL, accfree)
            barrier()
            if getattr(cfg, 'stop', None) == (l, 'F'): raise _Stop()
        pst.close()
    return nc


def seg_layout(cfg, core, seqlens):
    raise NotImplementedError


def make_inputs(cfg, core_segs, xs, cs, inp):
    E = cfg.E; m = {}
    kval0 = np.zeros((128, cfg.NR0), np.float32); kval1 = np.zeros((128, cfg.NQ0), np.float32); tval0 = np.zeros((128, cfg.NQ0), np.float32)
    cc = np.zeros((len(core_segs), D), np.float32)
    rpb = inp["l1_rpb_c"]
    b1 = np.empty((1 + 4 * len(core_segs), 28, 128, 512), np.float32); b1[0] = host_bias_l1(rpb, None, 0)
    for s, (xf, crow, start, L) in enumerate(core_segs):
        own = cfg.own[s] * 128
        pos = np.arange(start - H0C * 128, start + own + H0C * 128)
        ok = (pos >= 0) & (pos < L)
        xe = np.zeros((len(pos), D), np.float32); xe[ok] = xf[pos[ok]]
        m[f"xe{s}"] = xe; cc[s] = crow
        kval0[:, cfg.R0off[s]:cfg.R0off[s] + cfg.R0[s]] = np.where(ok, 0.0, NEG).reshape(cfg.R0[s], 128).T
        posq = np.arange(start - HQC * 128, start + own + HQC * 128); okq = (posq >= 0) & (posq < L)
        kval1[:, cfg.Q0off[s]:cfg.Q0off[s] + cfg.Q0[s]] = np.where(okq, 0.0, NEG).reshape(cfg.Q0[s], 128).T
        tval0[:, cfg.Q0off[s]:cfg.Q0off[s] + cfg.Q0[s]] = okq.astype(np.float32).reshape(cfg.Q0[s], 128).T
        nrows = L // 64; r_first = start // 64; nblk = cfg.own[s]
        for j, qc in enumerate([0, 1, nblk - 2, nblk - 1]):
            b1[1 + 4 * s + j] = host_bias_l1(rpb, r_first + 2 * qc, nrows)
    m["c"] = cc; m["kval0"] = kval0; m["kval1"] = kval1; m["tval0"] = tval0
    m["ident"] = np.eye(128, dtype=np.float32)
    m["ustrict"] = np.triu(np.ones((128, 128), np.float32), 1)
    bo = np.zeros((128, 128), np.float32); bo[:64, :64] = 1; bo[64:, 64:] = 1; m["bones"] = bo
    for l in range(2):
        m[f"ecol{l}"] = np.broadcast_to((np.arange(E) * cfg.C[l] + 1).astype(np.float32), (128, E)).copy()
    m["bias0"] = host_bias_l0(); m["bias1"] = b1
    m["sink"] = inp["l0_sink_a"].reshape(1, 8)
    def g2(v): return np.concatenate([v, v]).astype(np.float32)
    m["l0_gains"] = np.stack([g2(inp["l0_q_norm_a"]), g2(inp["l0_k_norm_a"]), g2(inp["l0_q_norm_b"]), g2(inp["l0_k_norm_b"])], 1)
    m["l1_gains"] = np.stack([g2(inp["l1_q_norm_c"]), g2(inp["l1_k_norm_c"]), g2(inp["l1_q_norm_c"]), g2(inp["l1_k_norm_c"])], 1)
    for l in range(2):
        p = f"l{l}_"
        m[p + "ada_w"] = inp[p + "ada_w"]; m[p + "ada_b"] = inp[p + "ada_b"].reshape(1, -1); m[p + "norm1"] = inp[p + "norm1"].reshape(1, -1)
        m[p + "w_in"] = inp[p + "w_in"]; m[p + "w_out"] = inp[p + "w_out"]; m[p + "norm2"] = inp[p + "norm2"].reshape(1, -1)
        m[p + "w_router"] = inp[p + "w_router"]; m[p + "b_router"] = inp[p + "b_router"].reshape(1, -1)
        m[p + "w_gate_up"] = inp[p + "w_gate_up"]; m[p + "b_gate_up"] = inp[p + "b_gate_up"]
        m[p + "w_down"] = inp[p + "w_down"]; m[p + "b_down"] = inp[p + "b_down"]
    return m


def kernel(**inputs):
    inp = {k: np.asarray(v) for k, v in inputs.items()}
    cfg = full_cfg()
    xp = inp["x_prompt"]; xs = inp["x_sample"]; cp = inp["c_prompt"]; cs = inp["c_sample"]
    in_maps = []
    for c in range(8):
        segs = [(xp[0], cp[0], c * 2048, xp.shape[1]), (xs[c // 2], cs[c // 2], (c % 2) * 4096, xs.shape[1])]
        in_maps.append(make_inputs(cfg, segs, None, None, inp))
    nc = build(cfg)
    res = run_bass_kernel_spmd(nc, in_maps, core_ids=list(range(8)))
    yp = np.empty_like(xp); ys = np.empty_like(xs)
    for c in range(8):
        yp[0, c * 2048:(c + 1) * 2048] = res.results[c]["y0"]
        ys[c // 2, (c % 2) * 4096:(c % 2 + 1) * 4096] = res.results[c]["y1"]
    return (yp, ys)
```
